# Optimizing a Trainium2 kernel written in Bass

```python
import math
import jax
import jax.numpy as jnp
from jax import lax
import numpy as np

D_MODEL = 1024
BATCH = 4
SEQ = 8192
DEPTH = 4

GRID_W = 64
CTX_LEN = 256
N_EVEN = (DEPTH + 1) // 2
N_ODD = DEPTH // 2
EPS = 1e-6
F32 = jnp.float32

A_HEADS = 4
A_DK = 128
A_DV = 128
A_CONV = 4
A_CHUNK = 64
A_QKV = 2 * A_HEADS * A_DK + A_HEADS * A_DV
B_HEADS = 4
B_DK = 128
B_DV = 128
B_CHUNK = 64
C_HEADS = 8
C_KV_HEADS = 2
C_HD = 64
C_WIN = 128
C_BLOCK = 128
ROPE_THETA = 10000.0
D_WIDTH = 512
D_BLOCKS = 8
D_BW = D_WIDTH // D_BLOCKS
D_CONV = 4
D_C = 8.0
N_EXPERTS = 16
EXPERT_FF = 1024
EC_FACTOR = 2

EVEN_SIZES = (A_QKV, A_HEADS * A_DV, 2 * A_HEADS, 2 * A_HEADS,
              B_HEADS * B_DK, B_HEADS * B_DV, 2 * B_HEADS * B_DK, B_HEADS * B_DV)
EVEN_OUT = A_HEADS * A_DV + B_HEADS * B_DV
ODD_SIZES = (C_HEADS * C_HD, C_KV_HEADS * C_HD, C_KV_HEADS * C_HD, D_WIDTH, D_WIDTH)
ODD_OUT = C_HEADS * C_HD + D_WIDTH

kernel_name = "hybrid_deltanet_hgrn2_swa_rglru_ec_moe_dit"


def _cuts(sizes):
    out, acc = [], 0
    for s in sizes[:-1]:
        acc += s
        out.append(acc)
    return out


def rmsnorm(x, w):
    xf = x.astype(F32)
    y = xf * lax.rsqrt(jnp.mean(xf * xf, axis=-1, keepdims=True) + EPS)
    return (y * w).astype(x.dtype)


def _modulate(h, shift, scale):
    return h * (1 + scale) + shift


def _l2norm(t):
    return t * lax.rsqrt(jnp.sum(t * t, axis=-1, keepdims=True) + EPS)


def _flip(t):
    return jnp.flip(t, axis=1)


def dwconv(x, w):
    k = w.shape[0]
    return lax.conv_general_dilated(x, w[:, None, :].astype(x.dtype), (1,), [((k - 1) // 2, k // 2)],
                                    dimension_numbers=('NWC', 'WIO', 'NWC'),
                                    feature_group_count=x.shape[-1])


def _to_chunks(t, length):
    b_, n, h = t.shape[:3]
    rest = t.shape[3:]
    t = t.reshape((b_, n // length, length, h) + rest)
    return t.transpose((1, 0, 3, 2) + tuple(range(4, t.ndim)))


def _from_chunks(t):
    nc, b_, h, length, d = t.shape
    return t.transpose(1, 0, 3, 2, 4).reshape(b_, nc * length, h, d)


def gated_delta_chunked(q, k, v, g, beta, s0):
    length = A_CHUNK
    dv = v.shape[-1]
    q, k, v = _to_chunks(q, length), _to_chunks(k, length), _to_chunks(v, length)
    g, beta = _to_chunks(g, length), _to_chunks(beta, length)
    cum = jnp.cumsum(g, axis=-1)
    incl = jnp.tril(jnp.ones((length, length), bool))
    strict = jnp.tril(jnp.ones((length, length), bool), -1)
    decay = jnp.exp(jnp.where(incl, cum[..., :, None] - cum[..., None, :], -jnp.inf))
    kb = k * beta[..., None]
    m = jnp.where(strict, jnp.einsum('...id,...jd->...ij', kb, k) * decay, 0.0)
    rhs = jnp.concatenate([v * beta[..., None], kb * jnp.exp(cum)[..., None]], axis=-1)
    sol = lax.linalg.triangular_solve(m + jnp.eye(length, dtype=m.dtype), rhs, left_side=True, lower=True)
    u, w = sol[..., :dv], sol[..., dv:]
    qk = jnp.einsum('...id,...jd->...ij', q, k) * decay
    q_dec = q * jnp.exp(cum)[..., None]
    k_dec = k * jnp.exp(cum[..., -1:] - cum)[..., None]
    g_last = jnp.exp(cum[..., -1])

    def step(s, inp):
        u_c, w_c, qk_c, qd_c, kd_c, gl_c = inp
        v_new = u_c - jnp.einsum('bhld,bhdv->bhlv', w_c, s)
        o = jnp.einsum('bhld,bhdv->bhlv', qd_c, s) + jnp.einsum('bhlm,bhmv->bhlv', qk_c, v_new)
        s = s * gl_c[..., None, None] + jnp.einsum('bhld,bhlv->bhdv', kd_c, v_new)
        return s, o

    s_fin, o = lax.scan(step, s0, (u, w, qk, q_dec, k_dec, g_last))
    return _from_chunks(o), s_fin


def hgrn2_chunked(q, k, v, lf, s0):
    length = B_CHUNK
    q, k, v, lf = (_to_chunks(t, length) for t in (q, k, v, lf))
    cum = jnp.cumsum(lf, axis=-2)
    incl = jnp.tril(jnp.ones((length, length), bool))[:, :, None]

    def step(s, inp):
        q_c, k_c, v_c, b_c = inp
        dec = jnp.exp(jnp.where(incl, b_c[..., :, None, :] - b_c[..., None, :, :], -jnp.inf))
        att = jnp.einsum('bhid,bhjd,bhijd->bhij', q_c, k_c, dec)
        b_last = b_c[..., -1:, :]
        o = jnp.einsum('bhij,bhjv->bhiv', att, v_c) + jnp.einsum('bhid,bhdv->bhiv', q_c * jnp.exp(b_c), s)
        s = s * jnp.exp(b_last)[..., 0, :, None] + jnp.einsum('bhjd,bhjv->bhdv', k_c * jnp.exp(b_last - b_c), v_c)
        return s, o

    s_fin, o = lax.scan(step, s0, (q, k, v, cum))
    return _from_chunks(o), s_fin


def delta_group(qkv, gate, alpha, beta, conv_w, a_log, dt_bias, norm_w, s0):
    b_, n, _ = qkv.shape
    qkv = jax.nn.silu(dwconv(qkv, conv_w)).astype(F32)
    q, k, v = jnp.split(qkv, [A_HEADS * A_DK, 2 * A_HEADS * A_DK], axis=-1)
    q = _l2norm(q.reshape(b_, n, A_HEADS, A_DK)) * (A_DK ** -0.5)
    k = _l2norm(k.reshape(b_, n, A_HEADS, A_DK))
    v = v.reshape(b_, n, A_HEADS, A_DV)
    g = -jnp.exp(a_log) * jax.nn.softplus(alpha.astype(F32).reshape(b_, n, 2, A_HEADS) + dt_bias)
    bt = jax.nn.sigmoid(beta.astype(F32).reshape(b_, n, 2, A_HEADS))
    o_f, s_f = gated_delta_chunked(q, k, v, g[:, :, 0], bt[:, :, 0], s0[0])
    o_b, s_b = gated_delta_chunked(_flip(q), _flip(k), _flip(v), _flip(g[:, :, 1]), _flip(bt[:, :, 1]), s0[1])
    o = o_f + _flip(o_b)
    o = rmsnorm(o, norm_w) * jax.nn.silu(gate.astype(F32).reshape(b_, n, A_HEADS, A_DV))
    return o.reshape(b_, n, A_HEADS * A_DV), (s_f, s_b)


def hgrn2_group(q, i, f, gate, lb, norm_w, s0):
    b_, n, _ = q.shape
    q = jax.nn.silu(q.astype(F32)).reshape(b_, n, B_HEADS, B_DK)
    v = i.astype(F32).reshape(b_, n, B_HEADS, B_DV)
    fg = lb + (1.0 - lb) * jax.nn.sigmoid(f.astype(F32).reshape(b_, n, 2, B_HEADS, B_DK))
    lf = jnp.log(fg)
    k = 1.0 - fg
    o_f, s_f = hgrn2_chunked(q, k[:, :, 0], v, lf[:, :, 0], s0[0])
    o_b, s_b = hgrn2_chunked(_flip(q), _flip(k[:, :, 1]), _flip(v), _flip(lf[:, :, 1]), s0[1])
    o = o_f + _flip(o_b)
    o = rmsnorm(o, norm_w) * jax.nn.silu(gate.astype(F32).reshape(b_, n, B_HEADS, B_DV))
    return o.reshape(b_, n, B_HEADS * B_DV), (s_f, s_b)


def even_mixer(h_c, h_l, w_in, w_out, conv_w, a_log, dt_bias, a_norm_w, lb, b_norm_w, ctx_out):
    b_ = h_l.shape[0]
    zero_a = jnp.zeros((b_, A_HEADS, A_DK, A_DV), F32)
    zero_b = jnp.zeros((b_, B_HEADS, B_DK, B_DV), F32)

    def run(h, st_a, st_b):
        qkv, ga, al, be, qb, ib, fb, gb = jnp.split(h @ w_in, _cuts(EVEN_SIZES), axis=-1)
        oa, st_a = delta_group(qkv, ga, al, be, conv_w, a_log, dt_bias, a_norm_w, st_a)
        ob, st_b = hgrn2_group(qb, ib, fb, gb, lb, b_norm_w, st_b)
        return jnp.concatenate([oa, ob], axis=-1).astype(h.dtype), st_a, st_b

    o_c, st_a, st_b = run(h_c, (zero_a, zero_a), (zero_b, zero_b))
    o_l, _, _ = run(h_l, st_a, st_b)
    y_c = o_c @ w_out if ctx_out else None
    return y_c, o_l @ w_out


def rope_2d(t, row, col):
    half = t.shape[-1] // 2
    nf = half // 2
    inv = ROPE_THETA ** (-jnp.arange(nf, dtype=F32) / nf)

    def rot(u, pos):
        ang = pos.astype(F32)[:, None] * inv
        cos, sin = jnp.cos(ang)[None, :, None, :], jnp.sin(ang)[None, :, None, :]
        u1, u2 = u[..., :nf], u[..., nf:]
        return jnp.concatenate([u1 * cos - u2 * sin, u2 * cos + u1 * sin], axis=-1)

    return jnp.concatenate([rot(t[..., :half], row), rot(t[..., half:], col)], axis=-1)


def _sink_softmax(s, sink):
    m = jnp.maximum(jnp.max(s, axis=-1, keepdims=True), sink)
    e = jnp.exp(s - m)
    return e / (jnp.sum(e, axis=-1, keepdims=True) + jnp.exp(sink - m))


def window_attention(q_l, k_l, v_l, k_c, v_c, sink):
    b_, n, h, hd = q_l.shape
    grp = h // C_KV_HEADS
    nb = n // C_BLOCK
    qb = q_l.reshape(b_, nb, C_BLOCK, C_KV_HEADS, grp, hd) * (hd ** -0.5)

    def band(t):
        tp = jnp.pad(t, ((0, 0), (C_BLOCK, C_BLOCK), (0, 0), (0, 0))).reshape(b_, nb + 2, C_BLOCK, C_KV_HEADS, hd)
        return jnp.concatenate([tp[:, :-2], tp[:, 1:-1], tp[:, 2:]], axis=2)

    kb, vb = band(k_l), band(v_l)
    s_lat = jnp.einsum('bnqkgd,bnskd->bnkgqs', qb, kb)
    s_ctx = jnp.einsum('bnqkgd,bskd->bnkgqs', qb, k_c)
    qpos = jnp.arange(C_BLOCK)[:, None]
    kpos = jnp.arange(3 * C_BLOCK)[None, :] - C_BLOCK
    s_abs = jnp.arange(nb)[:, None, None] * C_BLOCK + kpos[None]
    mask = (jnp.abs(kpos - qpos) <= C_WIN)[None] & (s_abs >= 0) & (s_abs < n)
    s_lat = jnp.where(mask[None, :, None, None], s_lat, -jnp.inf)
    sink_b = sink.astype(F32).reshape(C_KV_HEADS, grp)[None, None, :, :, None, None]
    p = _sink_softmax(jnp.concatenate([s_lat, s_ctx], axis=-1), sink_b)
    o = (jnp.einsum('bnkgqs,bnskd->bnqkgd', p[..., :3 * C_BLOCK], vb)
         + jnp.einsum('bnkgqs,bskd->bnqkgd', p[..., 3 * C_BLOCK:], v_c))
    return o.reshape(b_, n, h * hd)


def context_attention(q_c, k_c, v_c, sink):
    b_, lc, h, hd = q_c.shape
    grp = h // C_KV_HEADS
    qg = q_c.reshape(b_, lc, C_KV_HEADS, grp, hd) * (hd ** -0.5)
    s = jnp.einsum('bqkgd,bskd->bkgqs', qg, k_c)
    p = _sink_softmax(s, sink.astype(F32).reshape(C_KV_HEADS, grp)[None, :, :, None, None])
    return jnp.einsum('bkgqs,bskd->bqkgd', p, v_c).reshape(b_, lc, h * hd)


def _linear_scan(a, u, h0):
    u = u.at[:, 0].add(a[:, 0] * h0)
    _, h = lax.associative_scan(lambda l, r: (l[0] * r[0], r[0] * l[1] + r[1]), (a, u), axis=1)
    return h, h[:, -1]


def rglru_group(xb, gb, conv_w, conv_b, w_r, b_r, w_i, b_i, lam, s0):
    b_, n, _ = xb.shape
    xc = (dwconv(xb, conv_w) + conv_b).astype(F32)
    xblk = xc.reshape(b_, n, D_BLOCKS, D_BW)
    r = jax.nn.sigmoid(jnp.einsum('bnhi,zhij->bnzhj', xblk, w_r.astype(F32)).reshape(b_, n, 2, D_WIDTH) + b_r)
    gi = jax.nn.sigmoid(jnp.einsum('bnhi,zhij->bnzhj', xblk, w_i.astype(F32)).reshape(b_, n, 2, D_WIDTH) + b_i)
    log_a = -D_C * jax.nn.softplus(-lam) * r
    a = jnp.exp(log_a)
    u = jnp.sqrt(-jnp.expm1(2.0 * log_a)) * gi * xc[:, :, None, :]
    h_f, s_f = _linear_scan(a[:, :, 0], u[:, :, 0], s0[0])
    h_b, s_b = _linear_scan(_flip(a[:, :, 1]), _flip(u[:, :, 1]), s0[1])
    y = (h_f + _flip(h_b)) * jax.nn.gelu(gb.astype(F32))
    return y, (s_f, s_b)


def odd_mixer(h_c, h_l, w_in, w_out, sink, d_conv_w, d_conv_b, d_w_r, d_b_r, d_w_i, d_b_i, d_lambda,
              row, col, ctx_out):
    b_ = h_l.shape[0]
    qc, kc, vc, xdc, gdc = jnp.split(h_c @ w_in, _cuts(ODD_SIZES), axis=-1)
    ql, kl, vl, xdl, gdl = jnp.split(h_l @ w_in, _cuts(ODD_SIZES), axis=-1)

    def heads(t, nh):
        return t.astype(F32).reshape(t.shape[0], t.shape[1], nh, C_HD)

    kc, vc = heads(kc, C_KV_HEADS), heads(vc, C_KV_HEADS)
    att_l = window_attention(rope_2d(heads(ql, C_HEADS), row, col), rope_2d(heads(kl, C_KV_HEADS), row, col),
                             heads(vl, C_KV_HEADS), kc, vc, sink)
    zero = jnp.zeros((b_, D_WIDTH), F32)
    rg_c, st = rglru_group(xdc, gdc, d_conv_w, d_conv_b, d_w_r, d_b_r, d_w_i, d_b_i, d_lambda, (zero, zero))
    rg_l, _ = rglru_group(xdl, gdl, d_conv_w, d_conv_b, d_w_r, d_b_r, d_w_i, d_b_i, d_lambda, st)
    y_l = jnp.concatenate([att_l, rg_l], axis=-1).astype(h_l.dtype) @ w_out
    y_c = None
    if ctx_out:
        att_c = context_attention(heads(qc, C_HEADS), kc, vc, sink)
        y_c = jnp.concatenate([att_c, rg_c], axis=-1).astype(h_c.dtype) @ w_out
    return y_c, y_l


def expert_choice_ffn(h, router, w1, w3, w2):
    b_, n, d = h.shape
    cap = max(1, EC_FACTOR * n // N_EXPERTS)
    aff = jax.nn.softmax(jnp.einsum('bnd,de->bne', h, router).astype(F32), axis=-1)
    gate, idx = lax.top_k(jnp.swapaxes(aff, 1, 2), cap)
    xs = jax.vmap(lambda hb, ib: hb[ib])(h, idx)
    hid = jax.nn.silu(jnp.einsum('becd,edf->becf', xs, w1)) * jnp.einsum('becd,edf->becf', xs, w3)
    out = jnp.einsum('becf,efd->becd', hid, w2) * gate[..., None].astype(h.dtype)
    return jax.vmap(lambda ob, ib: jnp.zeros((n, d), ob.dtype).at[ib.reshape(-1)].add(ob.reshape(-1, d)))(out, idx)


def setup_inputs(seed: int = 0) -> dict:
    key = jax.random.key(seed)
    ks = iter(jax.random.split(key, 32))

    def nrm(shape, scale):
        return jax.random.normal(next(ks), shape, F32) * scale

    def unif(shape, lo, hi):
        return jax.random.uniform(next(ks), shape, F32, lo, hi)

    dt = jnp.exp(unif((N_EVEN, 2, A_HEADS), math.log(1e-3), math.log(1e-1)))
    a_pow = unif((N_ODD, 2, D_WIDTH), 0.9, 0.999) ** (1.0 / D_C)
    return {
        'x': nrm((BATCH, SEQ, D_MODEL), 1.0),
        'c': nrm((BATCH, D_MODEL), 1.0),
        'ctx': nrm((BATCH, CTX_LEN, D_MODEL), 1.0),
        'c_ctx': nrm((D_MODEL,), 1.0),
        'w_mod': nrm((DEPTH, D_MODEL, 6 * D_MODEL), 0.5 * D_MODEL ** -0.5),
        'b_mod': nrm((DEPTH, 6 * D_MODEL), 0.02),
        'norm1_w': 1.0 + nrm((DEPTH, D_MODEL), 0.02),
        'norm2_w': 1.0 + nrm((DEPTH, D_MODEL), 0.02),
        'final_norm_w': 1.0 + nrm((D_MODEL,), 0.02),
        'ev_w_in': nrm((N_EVEN, D_MODEL, sum(EVEN_SIZES)), D_MODEL ** -0.5),
        'ev_w_out': nrm((N_EVEN, EVEN_OUT, D_MODEL), EVEN_OUT ** -0.5),
        'a_conv_w': nrm((N_EVEN, A_CONV, A_QKV), A_CONV ** -0.5),
        'a_log': jnp.log(unif((N_EVEN, 2, A_HEADS), 1.0, 16.0)),
        'a_dt_bias': dt + jnp.log(-jnp.expm1(-dt)),
        'a_norm_w': 1.0 + nrm((N_EVEN, A_DV), 0.02),
        'b_lb_logits': nrm((N_EVEN, 2, B_HEADS, B_DK), 1.0),
        'b_norm_w': 1.0 + nrm((N_EVEN, B_DV), 0.02),
        'od_w_in': nrm((N_ODD, D_MODEL, sum(ODD_SIZES)), D_MODEL ** -0.5),
        'od_w_out': nrm((N_ODD, ODD_OUT, D_MODEL), ODD_OUT ** -0.5),
        'c_sink': nrm((N_ODD, C_HEADS), 0.5),
        'd_conv_w': nrm((N_ODD, D_CONV, D_WIDTH), D_CONV ** -0.5),
        'd_conv_b': nrm((N_ODD, D_WIDTH), 0.01),
        'd_w_r': nrm((N_ODD, 2, D_BLOCKS, D_BW, D_BW), D_BW ** -0.5),
        'd_b_r': nrm((N_ODD, 2, D_WIDTH), 0.01),
        'd_w_i': nrm((N_ODD, 2, D_BLOCKS, D_BW, D_BW), D_BW ** -0.5),
        'd_b_i': nrm((N_ODD, 2, D_WIDTH), 0.01),
        'd_lambda': jnp.log(a_pow) - jnp.log1p(-a_pow),
        'moe_router': nrm((DEPTH, D_MODEL, N_EXPERTS), D_MODEL ** -0.5),
        'moe_w1': nrm((DEPTH, N_EXPERTS, D_MODEL, EXPERT_FF), D_MODEL ** -0.5),
        'moe_w3': nrm((DEPTH, N_EXPERTS, D_MODEL, EXPERT_FF), D_MODEL ** -0.5),
        'moe_w2': nrm((DEPTH, N_EXPERTS, EXPERT_FF, D_MODEL), EXPERT_FF ** -0.5),
    }


def reference(x, c, ctx, c_ctx, w_mod, b_mod, norm1_w, norm2_w, final_norm_w,
              ev_w_in, ev_w_out, a_conv_w, a_log, a_dt_bias, a_norm_w, b_lb_logits, b_norm_w,
              od_w_in, od_w_out, c_sink, d_conv_w, d_conv_b, d_w_r, d_b_r, d_w_i, d_b_i, d_lambda,
              moe_router, moe_w1, moe_w3, moe_w2):
    n = x.shape[1]
    rows = n // GRID_W
    row = jnp.repeat(jnp.arange(rows, dtype=jnp.int32), GRID_W)
    col = jnp.tile(jnp.arange(GRID_W, dtype=jnp.int32), rows)
    lb_all = jnp.cumsum(jax.nn.softmax(b_lb_logits.astype(F32), axis=0), axis=0)
    lb_all = lb_all - lb_all[0:1]
    for l in range(DEPTH):
        last = l == DEPTH - 1
        j = l // 2
        mod = (jax.nn.silu(c) @ w_mod[l] + b_mod[l])[:, None, :]
        mod_c = jax.nn.silu(c_ctx) @ w_mod[l] + b_mod[l]
        sh1, sc1, g1, sh2, sc2, g2 = jnp.split(mod, 6, axis=-1)
        csh1, csc1, cg1, csh2, csc2, cg2 = jnp.split(mod_c, 6, axis=-1)
        h_l = _modulate(rmsnorm(x, norm1_w[l]), sh1, sc1)
        h_c = _modulate(rmsnorm(ctx, norm1_w[l]), csh1, csc1)
        if l % 2 == 0:
            y_c, y_l = even_mixer(h_c, h_l, ev_w_in[j], ev_w_out[j], a_conv_w[j], a_log[j], a_dt_bias[j],
                                  a_norm_w[j], lb_all[j], b_norm_w[j], not last)
        else:
            y_c, y_l = odd_mixer(h_c, h_l, od_w_in[j], od_w_out[j], c_sink[j], d_conv_w[j], d_conv_b[j],
                                 d_w_r[j], d_b_r[j], d_w_i[j], d_b_i[j], d_lambda[j], row, col, not last)
        x = x + g1 * y_l
        h_l = _modulate(rmsnorm(x, norm2_w[l]), sh2, sc2)
        x = x + g2 * expert_choice_ffn(h_l, moe_router[l], moe_w1[l], moe_w3[l], moe_w2[l])
        if not last:
            ctx = ctx + cg1 * y_c
            h_c = _modulate(rmsnorm(ctx, norm2_w[l]), csh2, csc2)
            ctx = ctx + cg2 * expert_choice_ffn(h_c, moe_router[l], moe_w1[l], moe_w3[l], moe_w2[l])
    return rmsnorm(x, final_norm_w)
```

```python
import numpy as np
import concourse.bass as bass
import concourse.mybir as mybir
from concourse.bass_utils import run_bass_kernel_spmd
from contextlib import ExitStack

F32 = mybir.dt.float32
BF16 = mybir.dt.bfloat16
I32 = mybir.dt.int32
AF = mybir.ActivationFunctionType
ALU = mybir.AluOpType

D = 1024
EPS = 1e-6
NEG = -1.0e30


class V:
    def __init__(self, key, ap):
        self.key = key
        self.ap = ap

    def __getitem__(self, k):
        return V(self.key, self.ap[k])

    def bc(self, shape):
        return V(self.key, self.ap.broadcast_to(list(shape)))

    def re(self, pat, **kw):
        return V(self.key, self.ap.rearrange(pat, **kw))

    def k(self, suffix):
        return V(self.key + ':' + str(suffix), self.ap)

    def pb(self, n):
        return V(self.key, self.ap.partition_broadcast(n))

    def bitcast(self, dt):
        return V(self.key, self.ap.bitcast(dt))


class Prog:
    def __init__(self, nc, es, n_dma_sems=24):
        self.nc = nc
        self.es = es
        self.eng = {'pe': nc.tensor, 'dve': nc.vector, 'act': nc.scalar, 'pool': nc.gpsimd, 'sp': nc.sync}
        self.sem = {k: es.enter_context(nc.semaphore('s_' + k)) for k in self.eng}
        self.cnt = {k: 0 for k in self.eng}
        self.dsem = [es.enter_context(nc.semaphore('d%d' % i)) for i in range(n_dma_sems)]
        self.dval = [0] * n_dma_sems
        self.dnext = 0
        self.seen = {k: {} for k in self.eng}
        self.last_w = {}
        self.reads = {}
        self.nins = 0

    def _need(self, e, deps):
        for key, val in deps.items():
            if self.seen[e].get(key, 0) >= val:
                continue
            sem = self.sem[key[1]] if key[0] == 'e' else self.dsem[key[1]]
            self.eng[e].wait_ge(sem, val)
            self.seen[e][key] = val

    def _deps(self, R, W):
        deps = {}

        def add(k, v):
            if deps.get(k, 0) < v:
                deps[k] = v
        for b in R:
            if b in self.last_w:
                add(*self.last_w[b])
        for b in W:
            if b in self.last_w:
                add(*self.last_w[b])
            for k, v in self.reads.get(b, {}).items():
                add(k, v)
        return deps

    def _commit(self, R, W, key, val):
        for b in R:
            self.reads.setdefault(b, {})[key] = val
        for b in W:
            self.last_w[b] = (key, val)
            self.reads[b] = {}

    def op(self, e, R, W, fn):
        R = [v.key for v in R if isinstance(v, V)]
        W = [v.key for v in W]
        deps = self._deps(R, W)
        if e == 'pe':
            deps.pop(('e', 'pe'), None)
        self._need(e, deps)
        ins = fn()
        self.cnt[e] += 1
        ins.then_inc(self.sem[e], 1)
        self._commit(R, W, ('e', e), self.cnt[e])
        self.nins += 1
        return ins

    def dma(self, out, in_, q='sp'):
        R, W = [in_.key], [out.key]
        deps = self._deps(R, W)
        i = self.dnext
        self.dnext = (self.dnext + 1) % len(self.dsem)
        if self.dval[i] > 0:
            deps[('d', i)] = max(deps.get(('d', i), 0), self.dval[i])
        self._need(q, deps)
        ins = self.eng[q].dma_start(out=out.ap, in_=in_.ap)
        self.dval[i] += 16
        ins.then_inc(self.dsem[i], 16)
        self._commit(R, W, ('d', i), self.dval[i])
        self.nins += 1

    def barrier(self):
        deps = {('e', k): c for k, c in self.cnt.items() if c > 0}
        for i, v in enumerate(self.dval):
            if v > 0:
                deps[('d', i)] = v
        for e in self.eng:
            d = dict(deps)
            d.pop(('e', e), None)
            self._need(e, d)
        self.last_w = {}
        self.reads = {}

    def sb(self, st, name, shape, dt=F32):
        self.uid = getattr(self, 'uid', 0) + 1
        name = '%s_u%d' % (name, self.uid)
        t = st.enter_context(self.nc.sbuf_tensor(name, list(shape), dt))
        return V(name, t[:])

    def dram(self, name, shape, dt=F32, kind="Internal"):
        t = self.nc.dram_tensor(name, list(shape), dt, kind=kind)
        return V(name, t.ap())

    def act(self, out, in_, func, bias=None, scale=None, accum=None, eng=None):
        kw = {}
        if bias is not None:
            kw['bias'] = bias.ap if isinstance(bias, V) else bias
        if scale is not None:
            kw['scale'] = scale.ap if isinstance(scale, V) else scale
        W = [out]
        if accum is not None:
            kw['accum_out'] = accum.ap
            W.append(accum)
        return self.op('act', [in_, bias, scale], W,
                       lambda: self.nc.scalar.activation(out=out.ap, in_=in_.ap, func=func, **kw))

    def tt(self, out, a, b, op, e='dve'):
        en = self.eng[e]
        return self.op(e, [a, b], [out], lambda: en.tensor_tensor(out=out.ap, in0=a.ap, in1=b.ap, op=op))

    def ts(self, out, a, s1, s2=None, op0=ALU.mult, op1=None, accum=None, e='dve'):
        en = self.eng[e]
        kw = {}
        if op1 is not None:
            kw['op1'] = op1
        W = [out]
        if accum is not None:
            kw['accum_out'] = accum.ap
            W.append(accum)
        g = lambda s: s.ap if isinstance(s, V) else s
        return self.op(e, [a, s1, s2], W,
                       lambda: en.tensor_scalar(out=out.ap, in0=a.ap, scalar1=g(s1), scalar2=g(s2), op0=op0, **kw))

    def stt(self, out, a, s, b, op0, op1):
        g = s.ap if isinstance(s, V) else s
        return self.op('dve', [a, s, b], [out],
                       lambda: self.nc.vector.scalar_tensor_tensor(out=out.ap, in0=a.ap, scalar=g, in1=b.ap, op0=op0, op1=op1))

    def cp(self, out, in_, e='dve'):
        if e == 'act':
            return self.op('act', [in_], [out], lambda: self.nc.scalar.activation(out=out.ap, in_=in_.ap, func=AF.Copy))
        en = self.eng[e]
        return self.op(e, [in_], [out], lambda: en.tensor_copy(out=out.ap, in_=in_.ap))

    def recip(self, out, in_):
        return self.op('dve', [in_], [out], lambda: self.nc.vector.reciprocal(out=out.ap, in_=in_.ap))

    def memset(self, out, val, e='pool'):
        en = self.eng[e]
        return self.op(e, [], [out], lambda: en.memset(out.ap, val))

    def mm(self, out, lhsT, rhs, start=True, stop=True):
        return self.op('pe', [lhsT, rhs], [out],
                       lambda: self.nc.tensor.matmul(out.ap, lhsT=lhsT.ap, rhs=rhs.ap, start=start, stop=stop))

    def tr(self, out, in_, ident):
        return self.op('pe', [in_, ident], [out],
                       lambda: self.nc.tensor.transpose(out=out.ap, in_=in_.ap, identity=ident.ap))

    def scan(self, out, d0, d1, init, op0=ALU.mult, op1=ALU.add):
        g = init.ap if isinstance(init, V) else init
        return self.op('dve', [d0, d1, init], [out],
                       lambda: self.nc.vector.tensor_tensor_scan(out=out.ap, data0=d0.ap, data1=d1.ap, initial=g, op0=op0, op1=op1))

    def reduce(self, out, in_, op, axis=mybir.AxisListType.X):
        return self.op('dve', [in_], [out], lambda: self.nc.vector.tensor_reduce(out=out.ap, in_=in_.ap, axis=axis, op=op))

    def aselect(self, out, in_, pattern, cmp, fill, base, cm):
        return self.op('pool', [in_], [out],
                       lambda: self.nc.gpsimd.affine_select(out=out.ap, in_=in_.ap, pattern=pattern, compare_op=cmp,
                                                            fill=fill, base=base, channel_multiplier=cm))

    def iota(self, out, pattern, base, cm):
        return self.op('pool', [], [out],
                       lambda: self.nc.gpsimd.iota(out.ap, pattern=pattern, base=base, channel_multiplier=cm))


EV_FM = [('qkv', c, c * 128) for c in range(12)] + [('qb', c, 2064 + c * 128) for c in range(4)] + \
        [('fb', c, 3088 + c * 128) for c in range(8)]
EV_TM = [('ga', 1536, 512), ('ab', 2048, 16), ('ib', 2576, 512), ('gb', 4112, 512)]


class Net:
    def __init__(self, N, LC, depth, dbg=()):
        self.N, self.LC, self.NT, self.depth = N, LC, N + LC, depth
        self.dbg = set(dbg)
        self.nc = nc = bass.Bass("TRN2", target_bir_lowering=False)
        self.es = ExitStack()
        self.P = P = Prog(nc, self.es)
        NT = self.NT
        n_ev, n_od = (depth + 1) // 2, depth // 2
        I = lambda name, shape: P.dram(name, shape, F32, kind="ExternalInput")
        self.xin = I("xin", [NT, D])
        self.cvec = I("cvec", [128, 16])
        self.w_mod = I("w_mod", [depth, D, 6 * D])
        self.b_mod = I("b_mod", [depth, 6 * D])
        self.norm1_w = I("norm1_w", [depth, D])
        self.norm2_w = I("norm2_w", [depth, D])
        self.final_norm_w = I("final_norm_w", [1, D])
        self.ev_w_in = I("ev_w_in", [n_ev, D, 4624])
        self.ev_w_out = I("ev_w_out", [n_ev, D, D])
        self.a_conv = I("a_conv", [n_ev, 128, 12, 4])
        self.a_log = I("a_log", [n_ev, 8])
        self.a_dtb = I("a_dtb", [n_ev, 8])
        self.a_norm_w = I("a_norm_w", [n_ev, 128])
        self.b_lbl = I("b_lbl", [128, 2, 8])
        self.b_norm_w = I("b_norm_w", [n_ev, 128])
        if n_od:
            self.od_w_in = I("od_w_in", [n_od, D, 1792])
            self.od_w_out = I("od_w_out", [n_od, D, D])
            self.c_sink = I("c_sink", [n_od, 8])
            self.d_conv = I("d_conv", [n_od, 128, 4, 4])
            self.d_convb = I("d_convb", [n_od, 128, 4])
            self.d_wr = I("d_wr", [n_od, 2, 8, 64, 64])
            self.d_wi = I("d_wi", [n_od, 2, 8, 64, 64])
            self.d_br = I("d_br", [n_od, 128, 2, 4])
            self.d_bi = I("d_bi", [n_od, 128, 2, 4])
            self.d_lam = I("d_lam", [n_od, 128, 2, 4])
        self.moe_router = I("moe_router", [depth, D, 16])
        self.moe_w1 = I("moe_w1", [depth, 16, D, D])
        self.moe_w3 = I("moe_w3", [depth, 16, D, D])
        self.moe_w2 = I("moe_w2", [depth, 16, D, D])
        self.out = P.dram("out", [N, D], F32, kind="ExternalOutput")
        self.scr = {}
        self.xs = self.S("xs", [NT, D])
        self.vecs = self.S("vecs", [depth, 2, 6, D])
        self.groups = []
        for seg, (a, b) in enumerate([(0, LC), (LC, NT)]):
            t = a
            while t < b:
                ln = min(512, b - t)
                self.groups.append((seg, t, ln))
                t += ln
        self.segs = [(0, LC), (LC, NT)]
        self.ps = []
        for i in range(8):
            t = self.es.enter_context(nc.psum_tensor("ps%d" % i, [128, 512], F32))
            self.ps.append(V("ps%d" % i, t[:]))
        self.psi = 0
        self.consts()

    def S(self, name, shape, dt=F32):
        kind = "ExternalOutput" if name in self.dbg else "Internal"
        v = self.P.dram(name, shape, dt, kind=kind)
        self.scr[name] = v
        return v

    def psn(self):
        p = self.ps[self.psi]
        self.psi = (self.psi + 1) % 8
        return p

    def consts(self):
        P, es = self.P, self.es
        self.ident = P.sb(es, "ident", [128, 128])
        P.memset(self.ident, 1.0)
        P.aselect(self.ident, self.ident, [[-1, 128]], ALU.is_equal, 0.0, 0, 1)
        self.identb = P.sb(es, "identb", [128, 128], BF16)
        P.cp(self.identb, self.ident)
        self.ones = P.sb(es, "ones", [128, 128])
        P.memset(self.ones, 1.0)
        self.epsc = P.sb(es, "epsc", [128, 1])
        P.memset(self.epsc, EPS)
        self.onec = P.sb(es, "onec", [128, 1])
        P.memset(self.onec, 1.0)
        cv = P.sb(es, "cv", [128, 16])
        P.dma(cv, self.cvec)
        self.csil = P.sb(es, "csil", [128, 16])
        P.act(self.csil, cv, AF.Silu)
        lbl = P.sb(es, "lbl", [128, 2, 8])
        P.dma(lbl, self.b_lbl)
        self.lb = P.sb(es, "lb", [128, 2, 8])
        self.oml = P.sb(es, "oml", [128, 2, 8])
        P.memset(self.lb, 0.0)
        P.tt(self.lb[:, 1, :], lbl[:, 1, :], lbl[:, 0, :], ALU.subtract)
        P.act(self.lb[:, 1, :], self.lb[:, 1, :], AF.Sigmoid)
        P.ts(self.oml, self.lb, -1.0, 1.0, ALU.mult, ALU.add)

    def vec(self, l, seg, i):
        r = 0 if seg == 1 else 1
        return self.vecs[l, r:r + 1, i, :].pb(128)

    def mod_prep(self, l):
        P = self.P
        with ExitStack() as st:
            wm = [P.sb(st, 'wm%d' % i, [128, 3072]) for i in range(2)]
            modsb = P.sb(st, 'modsb', [2, 6144])
            bm = P.sb(st, 'bm', [2, 6144])
            P.dma(bm, self.b_mod[l:l + 1, :].pb(2))
            cnt = 0
            for half in range(2):
                banks = [self.psn() for _ in range(6)]
                for k in range(8):
                    w = wm[cnt % 2]
                    cnt += 1
                    P.dma(w, self.w_mod[l, k * 128:(k + 1) * 128, half * 3072:(half + 1) * 3072])
                    for j in range(6):
                        P.mm(banks[j][0:2, :], self.csil[:, k:16:8], w[:, j * 512:(j + 1) * 512],
                             start=(k == 0), stop=(k == 7))
                for j in range(6):
                    c0 = half * 3072 + j * 512
                    P.tt(modsb[:, c0:c0 + 512], banks[j][0:2, :], bm[:, c0:c0 + 512], ALU.add)
            n1 = P.sb(st, 'n1', [2, D])
            n2 = P.sb(st, 'n2', [2, D])
            P.dma(n1, self.norm1_w[l:l + 1, :].pb(2))
            P.dma(n2, self.norm2_w[l:l + 1, :].pb(2))
            vv = P.sb(st, 'vv', [2, 6, D])
            m = lambda i: modsb[:, i * D:(i + 1) * D]
            P.stt(vv[:, 0, :], m(1), 1.0, n1, ALU.add, ALU.mult)
            P.cp(vv[:, 1, :], m(0))
            P.cp(vv[:, 2, :], m(2))
            P.stt(vv[:, 3, :], m(4), 1.0, n2, ALU.add, ALU.mult)
            P.cp(vv[:, 4, :], m(3))
            P.cp(vv[:, 5, :], m(5))
            P.dma(self.vecs[l], vv, q='pool')
        P.barrier()

    def norm_mod_T(self, st, src, t0, ln, A, B, hT, tag, hf_out=None):
        P = self.P
        for ti in range(ln // 128):
            xt = self.rot(st, tag + 'xt', [128, D], F32, 2)
            P.dma(xt, src[t0 + ti * 128:t0 + (ti + 1) * 128, :])
            junk = self.rot(st, tag + 'junk', [128, D], F32, 1)
            ss = self.rot(st, tag + 'ss', [128, 1], F32, 2)
            P.act(junk, xt, AF.Square, accum=ss)
            P.act(ss, ss, AF.Sqrt, scale=1.0 / D, bias=self.epsc)
            P.recip(ss, ss)
            P.stt(junk, xt, ss, A, ALU.mult, ALU.mult)
            if hf_out is not None:
                hf = hf_out(ti)
                P.tt(hf, junk, B, ALU.add)
                src_h = hf
            hb = self.rot(st, tag + 'hb', [128, D], BF16, 2)
            P.tt(hb, junk, B, ALU.add)
            pt = self.psn()
            ptb = pt.bitcast(BF16)
            for k in range(8):
                P.tr(ptb[:, k * 128:(k + 1) * 128], hb[:, k * 128:(k + 1) * 128], self.identb)
            P.cp(hT[:, :, ti * 128:(ti + 1) * 128], ptb.re("p (k t) -> p k t", k=8), e='act')

    def rot(self, st, name, shape, dt, n):
        d = st.__dict__.setdefault('_rot', {})
        if name not in d:
            d[name] = [[self.P.sb(st, '%s_%d' % (name, i), shape, dt) for i in range(n)], 0]
        bufs, i = d[name]
        d[name][1] = i + 1
        return bufs[i % n]

    def load_w_bf16(self, dst, src, ncols, q='pool'):
        K = dst.ap.shape[1]
        for k in range(K):
            c = 0
            while c < ncols:
                w = min(2048, ncols - c)
                self.P.dma(dst[:, k, c:c + w], src[k * 128:(k + 1) * 128, c:c + w], q=q)
                c += w

    def stageA_even(self, l):
        P, j = self.P, l // 2
        NT = self.NT
        S = self.scr
        if 'qkvT' not in S:
            self.S('qkvT', [12, 128, NT]); self.S('hqT', [4, 128, NT]); self.S('lfT', [8, 128, NT])
            self.S('hkT', [8, 128, NT]); self.S('tm_ga', [NT, 512]); self.S('tm_gb', [NT, 512])
            self.S('tm_ib', [NT, 512]); self.S('tm_g', [NT, 8]); self.S('tm_bt', [NT, 8])
        src = self.xin if l == 0 else self.xs
        with ExitStack() as st:
            w = P.sb(st, 'wA', [128, 8, 4624], BF16)
            self.load_w_bf16(w, self.ev_w_in[j], 4624)
            AB = {}
            for seg in (0, 1):
                AB[seg] = (P.sb(st, 'A1_%d' % seg, [128, D]), P.sb(st, 'B1_%d' % seg, [128, D]))
                P.dma(AB[seg][0], self.vec(l, seg, 0))
                P.dma(AB[seg][1], self.vec(l, seg, 1))
            al = P.sb(st, 'alog', [128, 8]); dtb = P.sb(st, 'dtb', [128, 8])
            P.dma(al, self.a_log[j:j + 1, :].pb(128))
            P.dma(dtb, self.a_dtb[j:j + 1, :].pb(128))
            negA = P.sb(st, 'negA', [128, 8])
            P.act(negA, al, AF.Exp)
            P.ts(negA, negA, -1.0, None, ALU.mult)
            hT = P.sb(st, 'hT', [128, 8, 512], BF16)
            for gi, (seg, t0, ln) in enumerate(self.groups):
                self.norm_mod_T(st, src, t0, ln, AB[seg][0], AB[seg][1], hT, 'A')
                for kind, c, col0 in EV_FM:
                    ps = self.psn()
                    for k in range(8):
                        P.mm(ps[:, :ln], w[:, k, col0:col0 + 128], hT[:, k, :ln], start=(k == 0), stop=(k == 7))
                    o = self.rot(st, 'Ao', [128, 512], F32, 4)
                    if kind == 'qkv':
                        P.cp(o[:, :ln], ps[:, :ln])
                        P.dma(S['qkvT'][c, :, t0:t0 + ln].k(gi), o[:, :ln], q='pool')
                    elif kind == 'qb':
                        P.act(o[:, :ln], ps[:, :ln], AF.Silu)
                        P.dma(S['hqT'][c, :, t0:t0 + ln].k(gi), o[:, :ln], q='pool')
                    else:
                        o2 = self.rot(st, 'Ao2', [128, 512], F32, 2)
                        P.act(o[:, :ln], ps[:, :ln], AF.Sigmoid)
                        P.ts(o[:, :ln], o[:, :ln], self.oml[:, j, c:c + 1], self.lb[:, j, c:c + 1], ALU.mult, ALU.add)
                        P.act(o2[:, :ln], o[:, :ln], AF.Ln)
                        P.dma(S['lfT'][c, :, t0:t0 + ln].k(gi), o2[:, :ln], q='pool')
                        o3 = self.rot(st, 'Ao3', [128, 512], F32, 2)
                        P.ts(o3[:, :ln], o[:, :ln], -1.0, 1.0, ALU.mult, ALU.add)
                        P.dma(S['hkT'][c, :, t0:t0 + ln].k(gi), o3[:, :ln], q='pool')
                for ti in range(ln // 128):
                    ta = t0 + ti * 128
                    for kind, col0, ncol in EV_TM:
                        ps = self.psn()
                        for k in range(8):
                            P.mm(ps[:, :ncol], hT[:, k, ti * 128:(ti + 1) * 128], w[:, k, col0:col0 + ncol],
                                 start=(k == 0), stop=(k == 7))
                        o = self.rot(st, 'Ao', [128, 512], F32, 4)
                        if kind in ('ga', 'gb'):
                            P.act(o, ps, AF.Silu)
                            P.dma(S['tm_' + kind][ta:ta + 128, :].k(gi), o, q='pool')
                        elif kind == 'ib':
                            P.cp(o, ps)
                            P.dma(S['tm_ib'][ta:ta + 128, :].k(gi), o, q='pool')
                        else:
                            P.tt(o[:, 0:8], ps[:, 0:8], dtb, ALU.add)
                            P.act(o[:, 0:8], o[:, 0:8], AF.Exp)
                            P.act(o[:, 0:8], o[:, 0:8], AF.Ln, bias=self.onec)
                            P.tt(o[:, 0:8], o[:, 0:8], negA, ALU.mult)
                            P.act(o[:, 8:16], ps[:, 8:16], AF.Sigmoid)
                            P.dma(S['tm_g'][ta:ta + 128, :].k(gi), o[:, 0:8], q='pool')
                            P.dma(S['tm_bt'][ta:ta + 128, :].k(gi), o[:, 8:16], q='pool')
        P.barrier()

    def stageB0_delta(self, l):
        P, j = self.P, l // 2
        NT = self.NT
        S = self.scr
        if 'dqT' not in S:
            self.S('dqT', [4, 128, NT]); self.S('dkT', [4, 128, NT])
            self.S('dk_tm', [NT, 512]); self.S('dv_tm', [NT, 512])
        with ExitStack() as st:
            cw = P.sb(st, 'cw', [128, 12, 4])
            P.dma(cw, self.a_conv[j])
            for gi, (seg, t0, ln) in enumerate(self.groups):
                a, b = self.segs[seg]
                cin = self.rot(st, 'cin', [128, 12, 515], F32, 2)
                lo, hi = max(a, t0 - 1), min(b, t0 + ln + 2)
                if lo > t0 - 1 or hi < t0 + ln + 2:
                    P.memset(cin, 0.0)
                P.dma(cin[:, :, lo - (t0 - 1):hi - (t0 - 1)], S['qkvT'][:, :, lo:hi].re("c p t -> p c t"))
                for c in range(12):
                    acc = self.rot(st, 'acc', [128, 512], F32, 3)
                    P.ts(acc[:, :ln], cin[:, c, 0:ln], cw[:, c, 0:1], None, ALU.mult)
                    for tap in range(1, 4):
                        P.stt(acc[:, :ln], cin[:, c, tap:tap + ln], cw[:, c, tap:tap + 1], acc[:, :ln], ALU.mult, ALU.add)
                    sl = self.rot(st, 'sl', [128, 512], F32, 3)
                    P.act(sl[:, :ln], acc[:, :ln], AF.Silu)
                    if c < 8:
                        sq = self.rot(st, 'sq', [128, 512], F32, 2)
                        P.act(sq[:, :ln], sl[:, :ln], AF.Square)
                        ps = self.psn()
                        P.mm(ps[:, :ln], self.ones, sq[:, :ln])
                        P.act(sq[:, :ln], ps[:, :ln], AF.Sqrt, bias=self.epsc)
                        P.recip(sq[:, :ln], sq[:, :ln])
                        qn = self.rot(st, 'qn', [128, 512], F32, 3)
                        P.stt(qn[:, :ln], sl[:, :ln], (128 ** -0.5) if c < 4 else 1.0, sq[:, :ln], ALU.mult, ALU.mult)
                        if c < 4:
                            P.dma(S['dqT'][c, :, t0:t0 + ln].k(gi), qn[:, :ln], q='pool')
                        else:
                            P.dma(S['dkT'][c - 4, :, t0:t0 + ln].k(gi), qn[:, :ln], q='pool')
                        srcT = qn
                    else:
                        srcT = sl
                    if c >= 4:
                        dst = S['dk_tm'] if c < 8 else S['dv_tm']
                        h = c % 4
                        ps = self.psn()
                        for ti in range(ln // 128):
                            P.tr(ps[:, ti * 128:(ti + 1) * 128], srcT[:, ti * 128:(ti + 1) * 128], self.ident)
                        tmo = self.rot(st, 'tmo', [128, 512], F32, 3)
                        P.cp(tmo[:, :ln], ps[:, :ln], e='act')
                        P.dma(dst[t0:t0 + ln, h * 128:(h + 1) * 128].re("(n p) d -> p n d", p=128).k(gi),
                              tmo[:, :ln].re("p (n d) -> p n d", d=128), q='pool')
        P.barrier()

    def chunk_consts(self):
        if hasattr(self, 'tri'):
            return
        P, es = self.P, self.es
        self.tri, self.maskL = [], []
        for z in range(2):
            sgn = 1 if z == 0 else -1
            t = P.sb(es, "tri%d" % z, [64, 64])
            P.memset(t, 1.0)
            P.aselect(t, t, [[sgn, 64]], ALU.is_ge, 0.0, 0, -sgn)
            self.tri.append(t)
            m = P.sb(es, "maskL%d" % z, [64, 4, 64])
            P.memset(m, 0.0)
            P.aselect(m, m, [[0, 4], [-sgn, 64]], ALU.is_gt, NEG, 0, sgn)
            self.maskL.append(m)
        self.ident4 = P.sb(es, "ident4", [64, 4, 64])
        P.memset(self.ident4, 1.0)
        P.aselect(self.ident4, self.ident4, [[0, 4], [-1, 64]], ALU.is_equal, 0.0, 0, 1)

    def chunk_order(self, z):
        out = []
        for a, b in self.segs:
            cs = list(range(a, b, 64))
            out += cs if z == 0 else cs[::-1]
        return out

    def stageB_delta(self, l):
        P = self.P
        S = self.scr
        NT = self.NT
        self.chunk_consts()
        if 'do_0' not in S:
            self.S('do_0', [NT, 512]); self.S('do_1', [NT, 512])
        with ExitStack() as st:
            St = [P.sb(st, 'dS%d' % z, [128, 4, 128]) for z in range(2)]
            for z in range(2):
                P.memset(St[z], 0.0)
            orders = [self.chunk_order(0), self.chunk_order(1)]
            nch = len(orders[0])
            DEP = 1
            hold = {}
            for step in range(nch + DEP):
                for z in range(2):
                    if step < nch:
                        hold[(z, step)] = self.delta_b1(st, z, orders[z][step])
                    if step >= DEP:
                        self.delta_b2(st, z, orders[z][step - DEP], St[z], hold.pop((z, step - DEP)))
        P.barrier()

    def delta_b1(self, st, z, t0):
        P, S = self.P, self.scr
        R = lambda nm, shape, n=2: self.rot(st, 'd%d%s' % (z, nm), shape, F32, n)
        g4 = R('g4', [64, 4]); bt4 = R('bt4', [64, 4])
        kT = R('kT', [128, 4, 64]); qT = R('qT', [128, 4, 64])
        ktm = R('ktm', [64, 4, 128]); vtm = R('vtm', [64, 4, 128])
        P.dma(g4, S['tm_g'][t0:t0 + 64, z * 4:(z + 1) * 4])
        P.dma(bt4, S['tm_bt'][t0:t0 + 64, z * 4:(z + 1) * 4])
        P.dma(kT, S['dkT'][:, :, t0:t0 + 64].re("h p t -> p h t"))
        P.dma(qT, S['dqT'][:, :, t0:t0 + 64].re("h p t -> p h t"))
        P.dma(ktm, S['dk_tm'][t0:t0 + 64, :].re("t (h d) -> t h d", h=4))
        P.dma(vtm, S['dv_tm'][t0:t0 + 64, :].re("t (h d) -> t h d", h=4))
        tri, maskL, I4 = self.tri[z], self.maskL[z], self.ident4
        gb = R('gb', [64, 4, 128])
        P.cp(gb, g4[:, :, None].bc([64, 4, 128]), e='pool')
        pc = self.psn()
        P.mm(pc[0:64, 0:4], tri, g4)
        cumc = R('cumc', [64, 4])
        P.cp(cumc, pc[0:64, 0:4])
        pt = self.psn()
        P.mm(pt[:, 0:4], self.ones[0:64, :], g4)
        prow = self.psn()
        for h in range(4):
            P.mm(prow[:, h * 64:(h + 1) * 64], gb[:, h, :], tri)
        prow3 = prow[:, 0:256].re("p (h f) -> p h f", h=4)
        X = R('X', [64, 4, 64])
        P.tt(X, prow3[0:64], cumc[:, :, None].bc([64, 4, 64]), ALU.subtract)
        E = R('E', [64, 4, 64])
        P.stt(E, X, -1.0, maskL, ALU.mult, ALU.add)
        P.act(E, E, AF.Exp)
        ecr = R('ecr', [128, 4, 64])
        P.act(ecr, prow3, AF.Exp)
        qdT = R('qdT', [128, 4, 64], 3)
        P.tt(qdT, qT, ecr, ALU.mult)
        glast = R('glast', [128, 4], 3)
        P.act(glast, pt[:, 0:4], AF.Exp)
        ekd = R('ekd', [64, 4])
        P.tt(ekd, pt[0:64, 0:4], cumc, ALU.subtract)
        P.act(ekd, ekd, AF.Exp)
        kdec = R('kdec', [64, 4, 128], 3)
        P.tt(kdec, ktm, ekd[:, :, None].bc([64, 4, 128]), ALU.mult, e='pool')
        ec = R('ec', [64, 4])
        P.act(ec, cumc, AF.Exp)
        P.tt(ec, ec, bt4, ALU.mult)
        Ru = R('Ru', [64, 4, 128]); Rw = R('Rw', [64, 4, 128])
        P.tt(Ru, vtm, bt4[:, :, None].bc([64, 4, 128]), ALU.mult, e='pool')
        P.tt(Rw, ktm, ec[:, :, None].bc([64, 4, 128]), ALU.mult, e='pool')
        pk = self.psn(); pq = self.psn()
        for h in range(4):
            P.mm(pk[0:64, h * 64:(h + 1) * 64], kT[:, h, :], kT[:, h, :])
        for h in range(4):
            P.mm(pq[0:64, h * 64:(h + 1) * 64], qT[:, h, :], kT[:, h, :])
        v3 = lambda p: p[0:64, 0:256].re("p (h f) -> p h f", h=4)
        Pm = R('Pm', [64, 4, 64]); Qm = R('Qm', [64, 4, 64])
        P.tt(Pm, v3(pk), bt4[:, :, None].bc([64, 4, 64]), ALU.mult)
        P.tt(Pm, Pm, E, ALU.mult)
        QK = R('QK', [64, 4, 64])
        P.tt(E, E, I4, ALU.add)
        P.tt(QK, v3(pq), E, ALU.mult)
        pn = self.psn(); pqt = self.psn()
        for h in range(4):
            P.tr(pn[0:64, h * 64:(h + 1) * 64], Pm[:, h, :], self.ident[0:64, 0:64])
        for h in range(4):
            P.tr(pqt[0:64, h * 64:(h + 1) * 64], QK[:, h, :], self.ident[0:64, 0:64])
        P.cp(Qm, v3(pn), e='act')
        QKT = R('QKT', [64, 4, 64], 3)
        P.cp(QKT, v3(pqt), e='act')
        Z = R('Z', [64, 4, 64]); Y = R('Y', [64, 4, 64])
        P.tt(Z, I4, Pm, ALU.subtract)
        P.tt(Y, I4, Qm, ALU.subtract)
        for lev in range(1, 6):
            last = lev == 5
            Pn = R('Pm', [64, 4, 64]); Qn = R('Qm', [64, 4, 64])
            if not last:
                p1 = self.psn()
                for h in range(4):
                    P.mm(p1[0:64, h * 64:(h + 1) * 64], Qm[:, h, :], Pm[:, h, :])
            p2 = self.psn()
            for h in range(4):
                P.mm(p2[0:64, h * 64:(h + 1) * 64], Pm[:, h, :], Qm[:, h, :])
            if not last:
                P.cp(Pn, v3(p1), e='act')
            P.cp(Qn, v3(p2))
            p3 = self.psn()
            for h in range(4):
                P.mm(p3[0:64, h * 64:(h + 1) * 64], Z[:, h, :], Qn[:, h, :])
            if not last:
                p4 = self.psn()
                for h in range(4):
                    P.mm(p4[0:64, h * 64:(h + 1) * 64], Y[:, h, :], Pn[:, h, :])
            Yn = R('Y', [64, 4, 64])
            P.tt(Yn, Y, v3(p3), ALU.add)
            if not last:
                Zn = R('Z', [64, 4, 64])
                P.tt(Zn, Z, v3(p4), ALU.add)
                Z = Zn
            Y, Pm, Qm = Yn, Pn, Qn
        pu = self.psn(); pw = self.psn()
        for h in range(4):
            P.mm(pu[0:64, h * 128:(h + 1) * 128], Y[:, h, :], Ru[:, h, :])
        for h in range(4):
            P.mm(pw[:, h * 64:(h + 1) * 64], Rw[:, h, :], Y[:, h, :])
        u = R('u', [64, 4, 128], 3); wT = R('wT', [128, 4, 64], 3)
        P.cp(u, pu[0:64, :].re("p (h d) -> p h d", h=4))
        P.cp(wT, pw[:, 0:256].re("p (h f) -> p h f", h=4), e='act')
        return dict(u=u, wT=wT, QKT=QKT, qdT=qdT, kdec=kdec, glast=glast)

    def delta_b2(self, st, z, t0, St, b):
        P, S = self.P, self.scr
        R = lambda nm, shape, n=2: self.rot(st, 'd%d%s' % (z, nm), shape, F32, n)
        pw = self.psn()
        for h in range(4):
            P.mm(pw[0:64, h * 128:(h + 1) * 128], b['wT'][:, h, :], St[:, h, :])
        vn = R('vn', [64, 4, 128])
        P.tt(vn, b['u'], pw[0:64, :].re("p (h d) -> p h d", h=4), ALU.subtract)
        po = self.psn()
        for h in range(4):
            P.mm(po[0:64, h * 128:(h + 1) * 128], b['qdT'][:, h, :], St[:, h, :], start=True, stop=False)
            P.mm(po[0:64, h * 128:(h + 1) * 128], b['QKT'][:, h, :], vn[:, h, :], start=False, stop=True)
        pS = self.psn()
        for h in range(4):
            P.mm(pS[:, h * 128:(h + 1) * 128], b['kdec'][:, h, :], vn[:, h, :])
        o = R('o', [64, 512])
        P.cp(o, po[0:64, :], e='act')
        P.dma(S['do_%d' % z][t0:t0 + 64, :].k(t0), o, q='pool')
        P.tt(St, St, b['glast'][:, :, None].bc([128, 4, 128]), ALU.mult)
        P.tt(St, St, pS.re("p (h d) -> p h d", h=4), ALU.add)

    def stageB_hgrn(self, l):
        P = self.P
        S = self.scr
        NT = self.NT
        self.chunk_consts()
        if 'ho_0' not in S:
            self.S('ho_0', [NT, 512]); self.S('ho_1', [NT, 512])
        with ExitStack() as st:
            self.hmask, self.hrm = [], []
            for z in range(2):
                sgn = 1 if z == 0 else -1
                m = P.sb(st, "hmask%d" % z, [64, 4, 64])
                P.memset(m, 1.0)
                P.aselect(m, m, [[0, 4], [sgn, 64]], ALU.is_ge, 0.0, 0, -sgn)
                self.hmask.append(m)
                rm = P.sb(st, "hrm%d" % z, [128, 4, 64])
                P.memset(rm, 1.0)
                e0 = 0 if z == 0 else 63
                P.memset(rm[:, :, e0:e0 + 1], 0.0)
                self.hrm.append(rm)
            St = [P.sb(st, 'hS%d' % z, [128, 4, 128]) for z in range(2)]
            for z in range(2):
                P.memset(St[z], 0.0)
            orders = [self.chunk_order(0), self.chunk_order(1)]
            nch = len(orders[0])
            DEP = 1
            hold = {}
            for step in range(nch + DEP):
                for z in range(2):
                    if step < nch:
                        hold[(z, step)] = self.hgrn_b1(st, z, orders[z][step])
                    if step >= DEP:
                        self.hgrn_b2(st, z, orders[z][step - DEP], St[z], hold.pop((z, step - DEP)))
        P.barrier()

    def hgrn_b1(self, st, z, t0):
        P, S = self.P, self.scr
        R = lambda nm, shape, n=2: self.rot(st, 'h%d%s' % (z, nm), shape, F32, n)
        lf = R('lf', [128, 4, 64]); hk = R('hk', [128, 4, 64]); q = R('q', [128, 4, 64])
        vtm = R('vtm', [64, 4, 128], 3)
        P.dma(lf, S['lfT'][z * 4:(z + 1) * 4, :, t0:t0 + 64].re("h p t -> p h t"))
        P.dma(hk, S['hkT'][z * 4:(z + 1) * 4, :, t0:t0 + 64].re("h p t -> p h t"))
        P.dma(q, S['hqT'][:, :, t0:t0 + 64].re("h p t -> p h t"))
        P.dma(vtm, S['tm_ib'][t0:t0 + 64, :].re("t (h d) -> t h d", h=4))
        b = R('b', [128, 4, 64])
        fl = lambda v: v.re("p h t -> p (h t)")
        rv = (lambda v: v) if z == 0 else (lambda v: v[:, ::-1])
        P.scan(rv(fl(b)), rv(fl(self.hrm[z])), rv(fl(lf)), 0.0)
        last = 63 if z == 0 else 0
        db = R('db', [128, 4, 64])
        P.tt(db, b, b[:, :, 32:33].bc([128, 4, 64]), ALU.subtract)
        eq = R('eq', [128, 4, 64]); ek = R('ek', [128, 4, 64])
        P.act(eq, db, AF.Exp)
        P.act(ek, db, AF.Exp, scale=-1.0)
        P.tt(eq, eq, q, ALU.mult)
        P.tt(ek, ek, hk, ALU.mult, e='pool')
        pa = self.psn()
        for h in range(4):
            P.mm(pa[0:64, h * 64:(h + 1) * 64], ek[:, h, :], eq[:, h, :])
        attT = R('attT', [64, 4, 64], 3)
        P.ts(attT, pa[0:64, 0:256].re("p (h f) -> p h f", h=4), 1.0e30, -1.0e30, ALU.min, ALU.max)
        P.tt(attT, attT, self.hmask[z], ALU.mult)
        eb = R('eb', [128, 4, 64])
        P.act(eb, b, AF.Exp)
        qeT = R('qeT', [128, 4, 64], 3)
        P.tt(qeT, eb, q, ALU.mult)
        kd = R('kd', [128, 4, 64])
        P.tt(kd, b, b[:, :, last:last + 1].bc([128, 4, 64]), ALU.subtract)
        P.act(kd, kd, AF.Exp, scale=-1.0)
        P.tt(kd, kd, hk, ALU.mult, e='pool')
        pk = self.psn()
        for h in range(4):
            P.tr(pk[0:64, h * 128:(h + 1) * 128], kd[:, h, :], self.ident)
        kdtm = R('kdtm', [64, 4, 128], 3)
        P.cp(kdtm, pk[0:64, :].re("p (h d) -> p h d", h=4), e='act')
        ebl = R('ebl', [128, 4, 1], 3)
        P.act(ebl, b[:, :, last:last + 1], AF.Exp)
        return dict(attT=attT, qeT=qeT, kdtm=kdtm, ebl=ebl, vtm=vtm)

    def hgrn_b2(self, st, z, t0, St, b):
        P, S = self.P, self.scr
        R = lambda nm, shape, n=2: self.rot(st, 'h%d%s' % (z, nm), shape, F32, n)
        po = self.psn()
        for h in range(4):
            P.mm(po[0:64, h * 128:(h + 1) * 128], b['attT'][:, h, :], b['vtm'][:, h, :], start=True, stop=False)
            P.mm(po[0:64, h * 128:(h + 1) * 128], b['qeT'][:, h, :], St[:, h, :], start=False, stop=True)
        pS = self.psn()
        for h in range(4):
            P.mm(pS[:, h * 128:(h + 1) * 128], b['kdtm'][:, h, :], b['vtm'][:, h, :])
        o = R('o', [64, 512])
        P.cp(o, po[0:64, :], e='act')
        P.dma(S['ho_%d' % z][t0:t0 + 64, :].k(t0), o, q='pool')
        P.tt(St, St, b['ebl'].bc([128, 4, 128]), ALU.mult)
        P.tt(St, St, pS.re("p (h d) -> p h d", h=4), ALU.add)

    def stageC(self, l, even):
        P, S = self.P, self.scr
        NT, j = self.NT, l // 2
        last = l == self.depth - 1
        if 'h2T' not in S:
            self.S('h2T', [8, 128, NT], BF16); self.S('affT', [16, NT]); self.S('gateT', [16, NT])
        src = self.xin if l == 0 else self.xs
        with ExitStack() as st:
            wo = P.sb(st, 'wo', [128, 8, D], BF16)
            self.load_w_bf16(wo, (self.ev_w_out if even else self.od_w_out)[j], D)
            rt = P.sb(st, 'rt', [128, 8, 16])
            P.dma(rt, self.moe_router[l].re("(k p) e -> p k e", p=128))
            vt = {}
            for seg in (0, 1):
                vt[seg] = [P.sb(st, 'C%d_%d' % (i, seg), [128, D]) for i in (2, 3, 4)]
                for t, i in zip(vt[seg], (2, 3, 4)):
                    P.dma(t, self.vec(l, seg, i))
            if even:
                nwa = P.sb(st, 'nwa', [128, 128]); nwb = P.sb(st, 'nwb', [128, 128])
                P.dma(nwa, self.a_norm_w[j:j + 1, :].pb(128))
                P.dma(nwb, self.b_norm_w[j:j + 1, :].pb(128))
            oT = P.sb(st, 'oT', [128, 8, 512], BF16)
            for gi, (seg, t0, ln) in enumerate(self.groups):
                if last and seg == 0:
                    continue
                G1, A2, B2 = vt[seg]
                nt = ln // 128
                if even:
                    for ti in range(nt):
                        ta = t0 + ti * 128
                        oc = self.rot(st, 'oc', [128, D], BF16, 2)
                        for mi, (pre, gname, nw) in enumerate((('do', 'tm_ga', nwa), ('ho', 'tm_gb', nwb))):
                            o0 = self.rot(st, 'o0', [128, 4, 128], F32, 2)
                            o1 = self.rot(st, 'o1', [128, 4, 128], F32, 2)
                            gt = self.rot(st, 'gt', [128, 4, 128], F32, 2)
                            P.dma(o0, S[pre + '_0'][ta:ta + 128, :].re("t (h d) -> t h d", h=4))
                            P.dma(o1, S[pre + '_1'][ta:ta + 128, :].re("t (h d) -> t h d", h=4))
                            P.dma(gt, S[gname][ta:ta + 128, :].re("t (h d) -> t h d", h=4))
                            P.tt(o0, o0, o1, ALU.add)
                            P.tt(o1, o0, o0, ALU.mult, e='pool')
                            ss = self.rot(st, 'ss4', [128, 4], F32, 2)
                            P.reduce(ss, o1, ALU.add)
                            P.act(ss, ss, AF.Sqrt, scale=1.0 / 128, bias=self.epsc)
                            P.recip(ss, ss)
                            P.tt(o0, o0, ss[:, :, None].bc([128, 4, 128]), ALU.mult)
                            P.tt(o0, o0, nw[:, None, :].bc([128, 4, 128]), ALU.mult, e='pool')
                            P.tt(oc[:, mi * 512:(mi + 1) * 512].re("t (h d) -> t h d", h=4), o0, gt, ALU.mult)
                        pt = self.psn()
                        ptb = pt.bitcast(BF16)
                        for k in range(8):
                            P.tr(ptb[:, k * 128:(k + 1) * 128], oc[:, k * 128:(k + 1) * 128], self.identb)
                        P.cp(oT[:, :, ti * 128:(ti + 1) * 128], ptb.re("p (k t) -> p k t", k=8), e='act')
                else:
                    self.odd_oT(st, oT, t0, ln)
                affs = self.rot(st, 'affs', [16, 512], F32, 2)
                h2s = self.rot(st, 'h2s', [128, 8, 512], BF16, 2)
                for ti in range(nt):
                    ta = t0 + ti * 128
                    xt = self.rot(st, 'Cxt', [128, D], F32, 2)
                    P.dma(xt, src[ta:ta + 128, :])
                    xn = self.rot(st, 'Cxn', [128, D], F32, 2)
                    for half in range(2):
                        ps = self.psn()
                        for k in range(8):
                            P.mm(ps, oT[:, k, ti * 128:(ti + 1) * 128], wo[:, k, half * 512:(half + 1) * 512],
                                 start=(k == 0), stop=(k == 7))
                        hs = slice(half * 512, (half + 1) * 512)
                        P.tt(xn[:, hs], ps, G1[:, hs], ALU.mult)
                        P.tt(xn[:, hs], xn[:, hs], xt[:, hs], ALU.add, e='pool')
                    P.dma(self.xs[ta:ta + 128, :].k(gi), xn, q='pool')
                    junk = self.rot(st, 'Cjunk', [128, D], F32, 1)
                    ss = self.rot(st, 'Css', [128, 1], F32, 2)
                    P.act(junk, xn, AF.Square, accum=ss)
                    P.act(ss, ss, AF.Sqrt, scale=1.0 / D, bias=self.epsc)
                    P.recip(ss, ss)
                    h2 = self.rot(st, 'Ch2', [128, D], F32, 2)
                    P.stt(h2, xn, ss, A2, ALU.mult, ALU.mult)
                    P.tt(h2, h2, B2, ALU.add, e='pool')
                    pa = self.psn(); pb = self.psn()
                    for k in range(8):
                        pp = pa if k < 4 else pb
                        P.tr(pp[:, (k % 4) * 128:(k % 4 + 1) * 128], h2[:, k * 128:(k + 1) * 128], self.ident)
                    h2T = self.rot(st, 'Ch2T', [128, 8, 128], F32, 2)
                    P.cp(h2T[:, 0:4, :], pa.re("p (k t) -> p k t", k=4), e='act')
                    P.cp(h2T[:, 4:8, :], pb.re("p (k t) -> p k t", k=4))
                    P.cp(h2s[:, :, ti * 128:(ti + 1) * 128], h2T, e='pool')
                    pl = self.psn()
                    for k in range(8):
                        P.mm(pl[:, 0:16], h2T[:, k, :], rt[:, k, :], start=(k == 0), stop=(k == 7))
                    mx = self.rot(st, 'Cmx', [128, 1], F32, 2)
                    P.reduce(mx, pl[:, 0:16], ALU.max)
                    P.ts(mx, mx, -1.0, None, ALU.mult)
                    ex = self.rot(st, 'Cex', [128, 16], F32, 2)
                    sm = self.rot(st, 'Csm', [128, 1], F32, 2)
                    P.act(ex, pl[:, 0:16], AF.Exp, bias=mx, accum=sm)
                    P.recip(sm, sm)
                    P.ts(ex, ex, sm, None, ALU.mult)
                    pT = self.psn()
                    P.tr(pT[0:16, 0:128], ex, self.ident)
                    P.cp(affs[:, ti * 128:(ti + 1) * 128], pT[0:16, 0:128], e='act')
                P.dma(S['affT'][:, t0:t0 + ln].k(gi), affs[:, :ln], q='pool')
                P.dma(S['h2T'][:, :, t0:t0 + ln].re("k p t -> p k t").k(gi), h2s[:, :, :ln], q='pool')
        P.barrier()

    def stage_topk(self, l):
        P, S = self.P, self.scr
        last = l == self.depth - 1
        with ExitStack() as st:
            for seg, (a, b) in enumerate(self.segs):
                if last and seg == 0:
                    continue
                n = b - a
                kk = max(1, 2 * n // 16)
                af = P.sb(st, 'af%d' % seg, [16, n])
                jk = P.sb(st, 'jk%d' % seg, [16, n])
                P.dma(af, S['affT'][:, a:b])
                lo = P.sb(st, 'lo%d' % seg, [16, 1]); hi = P.sb(st, 'hi%d' % seg, [16, 1])
                mid = P.sb(st, 'mid%d' % seg, [16, 1]); cnt = P.sb(st, 'cnt%d' % seg, [16, 1])
                fl = P.sb(st, 'fl%d' % seg, [16, 1]); d1 = P.sb(st, 'd1%d' % seg, [16, 1]); d2 = P.sb(st, 'd2%d' % seg, [16, 1])
                P.memset(lo, 0.0, e='dve'); P.memset(hi, 2.0, e='dve')
                for it in range(36):
                    P.ts(mid, lo, hi, 0.5, ALU.add, ALU.mult)
                    P.ts(jk, af, mid, None, ALU.is_ge, ALU.add, accum=cnt)
                    P.ts(fl, cnt, float(kk), None, ALU.is_ge)
                    P.tt(d1, mid, lo, ALU.subtract)
                    P.tt(d2, hi, mid, ALU.subtract)
                    P.stt(lo, d1, fl, lo, ALU.mult, ALU.add)
                    P.stt(hi, d2, fl, mid, ALU.mult, ALU.add)
                P.stt(jk, af, lo, af, ALU.is_ge, ALU.mult)
                P.dma(S['gateT'][:, a:b], jk, q='pool')
        P.barrier()

    def stage_moe_dense(self, l):
        P, S = self.P, self.scr
        last = l == self.depth - 1
        if 'wbf' not in S:
            self.S('wbf', [16, 3, D, D], BF16)
        for e in range(16):
            for i, wsrc in enumerate((self.moe_w1, self.moe_w3, self.moe_w2)):
                P.dma(S['wbf'][e, i].k('%d_%d' % (e, i)), wsrc[l, e], q='pool')
        with ExitStack() as st:
            sel = P.sb(st, 'sel', [16, 16, 128])
            P.memset(sel, 1.0)
            P.aselect(sel, sel, [[-1, 16], [0, 128]], ALU.is_equal, 0.0, 0, 1)
            G2 = {}
            for seg in (0, 1):
                G2[seg] = P.sb(st, 'G2_%d' % seg, [128, D])
                P.dma(G2[seg], self.vec(l, seg, 5))
            acc = P.sb(st, 'macc', [128, 8, D])
            hb = P.sb(st, 'mhb', [128, 8, 1024], BF16)
            gT = P.sb(st, 'mgT', [16, 1024])
            hid = P.sb(st, 'mhid', [128, 8, 512], BF16)
            blocks = []
            for seg, (a, b) in enumerate(self.segs):
                if last and seg == 0:
                    continue
                t = a
                while t < b:
                    bl = min(1024, b - t)
                    blocks.append((seg, t, bl))
                    t += bl
            for bi, (seg, t0, bl) in enumerate(blocks):
                P.memset(acc, 0.0)
                P.dma(hb[:, :, :bl], S['h2T'][:, :, t0:t0 + bl].re("k p t -> p k t"))
                P.dma(gT[:, :bl], S['gateT'][:, t0:t0 + bl])
                for e in range(16):
                    ws = []
                    for i in range(3):
                        wt = self.rot(st, 'mw%d' % i, [128, 8, D], BF16, 2)
                        P.dma(wt, S['wbf'][e, i].re("(k p) f -> p k f", p=128).k('%d_%d' % (e, i)))
                        ws.append(wt)
                    w1, w3, w2 = ws
                    for s0 in range(0, bl, 512):
                        ln = min(512, bl - s0)
                        pg = self.psn()
                        P.mm(pg[:, :ln], sel[:, e, :], gT[:, s0:s0 + ln])
                        gbc = self.rot(st, 'mgbc', [128, 512], F32, 2)
                        P.cp(gbc[:, :ln], pg[:, :ln], e='act')
                        for fc in range(8):
                            p1 = self.psn(); p3 = self.psn()
                            for k in range(8):
                                P.mm(p1[:, :ln], w1[:, k, fc * 128:(fc + 1) * 128], hb[:, k, s0:s0 + ln], start=(k == 0), stop=(k == 7))
                            for k in range(8):
                                P.mm(p3[:, :ln], w3[:, k, fc * 128:(fc + 1) * 128], hb[:, k, s0:s0 + ln], start=(k == 0), stop=(k == 7))
                            sg = self.rot(st, 'msg', [128, 512], F32, 2)
                            P.act(sg[:, :ln], p1[:, :ln], AF.Silu)
                            P.tt(sg[:, :ln], sg[:, :ln], p3[:, :ln], ALU.mult)
                            P.tt(hid[:, fc, :ln], sg[:, :ln], gbc[:, :ln], ALU.mult, e='pool')
                        for ti in range(ln // 128):
                            at = (s0 + ti * 128) // 128
                            for half in range(2):
                                po = self.psn()
                                for fc in range(8):
                                    P.mm(po, hid[:, fc, ti * 128:(ti + 1) * 128], w2[:, fc, half * 512:(half + 1) * 512],
                                         start=(fc == 0), stop=(fc == 7))
                                hs = slice(half * 512, (half + 1) * 512)
                                P.tt(acc[:, at, hs], acc[:, at, hs], po, ALU.add)
                for ti in range(bl // 128):
                    ta = t0 + ti * 128
                    xt = self.rot(st, 'mxt', [128, D], F32, 2)
                    P.dma(xt, self.xs[ta:ta + 128, :].k('m%d' % bi))
                    P.tt(acc[:, ti, :], acc[:, ti, :], G2[seg], ALU.mult)
                    P.tt(xt, xt, acc[:, ti, :], ALU.add, e='pool')
                    P.dma(self.xs[ta:ta + 128, :].k('m%d' % bi), xt, q='pool')
        P.barrier()

    def final_norm(self):
        P = self.P
        with ExitStack() as st:
            fw = P.sb(st, 'fw', [128, D])
            P.dma(fw, self.final_norm_w.pb(128))
            for ti in range(self.N // 128):
                ta = self.LC + ti * 128
                xt = self.rot(st, 'Fxt', [128, D], F32, 3)
                P.dma(xt, self.xs[ta:ta + 128, :])
                junk = self.rot(st, 'Fjunk', [128, D], F32, 2)
                ss = self.rot(st, 'Fss', [128, 1], F32, 2)
                P.act(junk, xt, AF.Square, accum=ss)
                P.act(ss, ss, AF.Sqrt, scale=1.0 / D, bias=self.epsc)
                P.recip(ss, ss)
                P.stt(junk, xt, ss, fw, ALU.mult, ALU.mult)
                P.dma(self.out[ti * 128:(ti + 1) * 128, :].k(ti), junk, q='pool')
        P.barrier()

    def layer(self, l):
        self.mod_prep(l)
        if l % 2 == 0:
            self.stageA_even(l)
            self.stageB0_delta(l)
            self.stageB_delta(l)
            self.stageB_hgrn(l)
            self.stageC(l, True)
        else:
            self.stageA_odd(l)
            self.stageB_attn(l)
            self.stageB_rglru(l)
            self.stageC(l, False)
        self.stage_topk(l)
        self.stage_moe_dense(l)

    def rope_tables(self):
        P, S = self.P, self.scr
        if 'ropeC' in S:
            return
        N = self.N
        self.S('ropeC', [128, N]); self.S('ropeS', [128, N])
        import math
        with ExitStack() as st:
            pi_ = P.sb(st, 'r_pi', [128, 1], I32)
            P.iota(pi_, [[0, 1]], 0, 1)
            t1 = P.sb(st, 'r_t1', [128, 1], I32); t2 = P.sb(st, 'r_t2', [128, 1], I32)
            P.ts(t1, pi_, 4, 4, ALU.arith_shift_right, ALU.logical_shift_left)
            P.tt(t1, pi_, t1, ALU.subtract)
            f16 = P.sb(st, 'r_f16', [128, 1])
            P.cp(f16, t1)
            inv = P.sb(st, 'r_inv', [128, 1])
            P.act(inv, f16, AF.Exp, scale=-math.log(10000.0) / 16.0)
            P.ts(inv, inv, 1.0 / (2 * math.pi), None, ALU.mult)
            P.ts(t2, pi_, 5, 1, ALU.arith_shift_right, ALU.bitwise_and)
            selc = P.sb(st, 'r_sel', [128, 1])
            P.cp(selc, t2)
            CW = min(N, 2048)
            ri = P.sb(st, 'r_ri', [128, CW], I32); ci = P.sb(st, 'r_ci', [128, CW], I32)
            rf = P.sb(st, 'r_rf', [128, CW]); cf = P.sb(st, 'r_cf', [128, CW])
            ys = {nm: P.sb(st, 'r_y' + nm, [128, CW]) for nm in ('ropeS', 'ropeC')}
            yi = P.sb(st, 'r_yi', [128, CW], I32)
            tm = P.sb(st, 'r_tm', [128, CW])
            for c0 in range(0, N, CW):
                P.iota(ri, [[1, CW // 64], [0, 64]], c0 // 64, 0)
                P.iota(ci, [[0, CW // 64], [1, 64]], 0, 0)
                P.cp(rf, ri); P.cp(cf, ci)
                P.tt(cf, cf, rf, ALU.subtract)
                P.stt(rf, cf, selc, rf, ALU.mult, ALU.add)
                P.ts(rf, rf, inv, None, ALU.mult)
                for name, off in (('ropeS', 0.0), ('ropeC', 0.25)):
                    y = ys[name]
                    P.ts(y, rf, off, None, ALU.add)
                    P.cp(yi, y)
                    P.cp(tm, yi)
                    P.tt(y, y, tm, ALU.subtract)
                    P.ts(tm, y, 0.5, None, ALU.is_gt)
                    P.tt(y, y, tm, ALU.subtract)
                    P.ts(tm, y, -0.5, None, ALU.is_lt)
                    P.tt(y, y, tm, ALU.add)
                    P.act(y, y, AF.Sin, scale=2 * math.pi)
                    P.dma(S[name][:, c0:c0 + CW].k(c0), y, q='pool')
        P.barrier()

    def stageA_odd(self, l):
        P, j = self.P, l // 2
        NT, N, LC = self.NT, self.N, self.LC
        S = self.scr
        self.rope_tables()
        if 'aqT' not in S:
            self.S('aqT', [4, 128, NT]); self.S('akT', [128, NT]); self.S('av_tm', [NT, 128])
            self.S('xdT', [4, 128, NT]); self.S('ggT', [4, 128, NT]); self.S('m0', [128, 1])
        src = self.xs
        with ExitStack() as st:
            w = P.sb(st, 'wAo', [128, 8, 1792], BF16)
            for k in range(8):
                rows = self.od_w_in[j, k * 128:(k + 1) * 128, :]
                for g in range(2):
                    P.dma(w[:, k, 0:512].re("p (i g d) -> p g i d", i=4, g=2)[:, g], rows[:, g * 256:(g + 1) * 256].re("p (i d) -> p i d", i=4), q='pool')
                P.dma(w[:, k, 512:1792], rows[:, 512:1792], q='pool')
            AB = {}
            for seg in (0, 1):
                AB[seg] = (P.sb(st, 'A1_%d' % seg, [128, D]), P.sb(st, 'B1_%d' % seg, [128, D]))
                P.dma(AB[seg][0], self.vec(l, seg, 0))
                P.dma(AB[seg][1], self.vec(l, seg, 1))
            piT = P.sb(st, 'piT', [128, 128])
            P.memset(piT, 0.0)
            pv = piT.re("p (b h j) -> p b h j", b=4, h=2)
            m1 = P.sb(st, 'pm1', [128, 4, 16]); p1 = P.sb(st, 'pp1', [128, 4, 16])
            P.memset(m1, -1.0); P.memset(p1, 1.0)
            P.aselect(pv[:, :, 0, :], m1, [[-32, 4], [-1, 16]], ALU.is_equal, 0.0, -16, 1)
            P.aselect(pv[:, :, 1, :], p1, [[-32, 4], [-1, 16]], ALU.is_equal, 0.0, 0, 1)
            qmax = P.sb(st, 'qmax', [128, 512]); kmax = P.sb(st, 'kmax', [128, 512])
            P.memset(qmax, 0.0); P.memset(kmax, 0.0)
            hT = P.sb(st, 'hT', [128, 8, 512], BF16)
            for gi, (seg, t0, ln) in enumerate(self.groups):
                self.norm_mod_T(st, src, t0, ln, AB[seg][0], AB[seg][1], hT, 'A')
                if seg == 1:
                    rc = self.rot(st, 'rC', [128, 512], F32, 2); rs = self.rot(st, 'rS', [128, 512], F32, 2)
                    P.dma(rc[:, :ln], S['ropeC'][:, t0 - LC:t0 - LC + ln])
                    P.dma(rs[:, :ln], S['ropeS'][:, t0 - LC:t0 - LC + ln])
                for c in range(5):
                    ps = self.psn()
                    col0 = c * 128
                    for k in range(8):
                        P.mm(ps[:, :ln], w[:, k, col0:col0 + 128], hT[:, k, :ln], start=(k == 0), stop=(k == 7))
                    x0 = self.rot(st, 'ox0', [128, 512], F32, 3)
                    if c < 4:
                        P.act(x0[:, :ln], ps[:, :ln], AF.Copy, scale=0.125)
                    else:
                        P.cp(x0[:, :ln], ps[:, :ln])
                    if seg == 1:
                        pr = self.psn()
                        P.mm(pr[:, :ln], piT, x0[:, :ln])
                        x1 = self.rot(st, 'ox1', [128, 512], F32, 2)
                        P.tt(x1[:, :ln], pr[:, :ln], rs[:, :ln], ALU.mult)
                        P.tt(x0[:, :ln], x0[:, :ln], rc[:, :ln], ALU.mult, e='pool')
                        P.tt(x0[:, :ln], x0[:, :ln], x1[:, :ln], ALU.add)
                    dst = S['aqT'][c, :, t0:t0 + ln] if c < 4 else S['akT'][:, t0:t0 + ln]
                    P.dma(dst.k(gi), x0[:, :ln], q='pool')
                    sq = self.rot(st, 'osq', [128, 512], F32, 2)
                    P.act(sq[:, :ln], x0[:, :ln], AF.Square)
                    pn = self.psn()
                    P.mm(pn[:, :ln], self.ones, sq[:, :ln])
                    mx = qmax if c < 4 else kmax
                    P.tt(mx[:, :ln], mx[:, :ln], pn[:, :ln], ALU.max)
                for c in range(8):
                    ps = self.psn()
                    col0 = 768 + c * 128
                    for k in range(8):
                        P.mm(ps[:, :ln], w[:, k, col0:col0 + 128], hT[:, k, :ln], start=(k == 0), stop=(k == 7))
                    o = self.rot(st, 'Ao', [128, 512], F32, 4)
                    if c < 4:
                        P.cp(o[:, :ln], ps[:, :ln])
                        P.dma(S['xdT'][c, :, t0:t0 + ln].k(gi), o[:, :ln], q='pool')
                    else:
                        t = self.rot(st, 'Ao2', [128, 512], F32, 2)
                        P.cp(o[:, :ln], ps[:, :ln], e='act')
                        P.tt(t[:, :ln], o[:, :ln], o[:, :ln], ALU.mult)
                        P.ts(t[:, :ln], t[:, :ln], 0.044715, 1.0, ALU.mult, ALU.add)
                        P.tt(t[:, :ln], t[:, :ln], o[:, :ln], ALU.mult)
                        P.act(t[:, :ln], t[:, :ln], AF.Sigmoid, scale=1.5957691216057308)
                        P.tt(o[:, :ln], o[:, :ln], t[:, :ln], ALU.mult)
                        P.dma(S['ggT'][c - 4, :, t0:t0 + ln].k(gi), o[:, :ln], q='pool')
                for ti in range(ln // 128):
                    ta = t0 + ti * 128
                    ps = self.psn()
                    for k in range(8):
                        P.mm(ps[:, 0:128], hT[:, k, ti * 128:(ti + 1) * 128], w[:, k, 640:768], start=(k == 0), stop=(k == 7))
                    o = self.rot(st, 'Av', [128, 128], F32, 3)
                    P.cp(o, ps[:, 0:128])
                    P.dma(S['av_tm'][ta:ta + 128, :].k(gi), o, q='pool')
            qm = P.sb(st, 'qm', [128, 1]); km = P.sb(st, 'km', [128, 1])
            P.reduce(qm, qmax, ALU.max); P.reduce(km, kmax, ALU.max)
            P.tt(qm, qm, km, ALU.mult)
            P.act(qm, qm, AF.Sqrt)
            sk = P.sb(st, 'sk', [128, 8])
            P.dma(sk, self.c_sink[j:j + 1, :].pb(128))
            P.reduce(km, sk, ALU.max)
            P.tt(qm, qm, km, ALU.max)
            P.ts(qm, qm, -1.0, None, ALU.mult)
            P.dma(S['m0'], qm, q='pool')
        P.barrier()

    def stageB_attn(self, l):
        P, j = self.P, l // 2
        NT, N, LC = self.NT, self.N, self.LC
        S = self.scr
        last = l == self.depth - 1
        if 'aoT' not in S:
            self.S('aoT', [512, NT])
        with ExitStack() as st:
            nm0 = P.sb(st, 'nm0', [128, 1])
            P.dma(nm0, S['m0'])
            mprev = P.sb(st, 'mprev', [128, 4, 128]); mnext = P.sb(st, 'mnext', [128, 4, 128])
            P.memset(mprev, 1.0); P.memset(mnext, 1.0)
            P.aselect(mprev, mprev, [[0, 4], [-1, 128]], ALU.is_ge, 0.0, 0, 1)
            P.aselect(mnext, mnext, [[0, 4], [1, 128]], ALU.is_ge, 0.0, 0, -1)
            e64 = P.sb(st, 'e64', [65, 64])
            P.memset(e64, 1.0)
            P.aselect(e64, e64, [[0, 64]], ALU.is_equal, 0.0, -64, 1)
            sk = P.sb(st, 'sk1', [1, 8])
            P.dma(sk, self.c_sink[j:j + 1, :])
            P.act(sk, sk, AF.Exp, bias=nm0[0:1, :])
            crow = P.sb(st, 'crow', [1, 8, 128])
            P.cp(crow, sk[:, :, None].bc([1, 8, 128]))
            kT = P.sb(st, 'akTs', [128, NT])
            P.dma(kT, S['akT'])
            va = P.sb(st, 'vaug', [128, NT // 128, 2, 65])
            P.memset(va, 1.0)
            for g in range(2):
                for n0 in range(0, NT // 128, 8):
                    n1 = min(NT // 128, n0 + 8)
                    P.dma(va[:, n0:n1, g, 0:64], S['av_tm'][n0 * 128:n1 * 128, g * 64:(g + 1) * 64].re("(n p) d -> p n d", p=128))
            nctx = LC // 128
            blocks = []
            if not last:
                blocks += [(b, list(range(nctx)), {}) for b in range(nctx)]
            nlat = N // 128
            for b in range(nlat):
                ch, mk = [], {}
                if b > 0:
                    ch.append(nctx + b - 1); mk[nctx + b - 1] = mprev
                ch.append(nctx + b)
                if b < nlat - 1:
                    ch.append(nctx + b + 1); mk[nctx + b + 1] = mnext
                blocks.append((nctx + b, ch + list(range(nctx)), mk))
            for (qb, chunks, masks) in blocks:
                qa = qb * 128
                qT = self.rot(st, 'aq', [128, 4, 128], F32, 2)
                P.dma(qT, S['aqT'][:, :, qa:qa + 128].re("c p t -> p c t"))
                for g in range(2):
                    pr = slice(g * 64, (g + 1) * 64)
                    po = self.psn()
                    for ci, ck in enumerate(chunks):
                        ps = self.psn()
                        P.mm(ps, kT[pr, ck * 128:(ck + 1) * 128], qT[pr, :, :])
                        pe = self.rot(st, 'ape', [128, 512], F32, 3)
                        P.act(pe, ps, AF.Exp, bias=nm0)
                        if ck in masks:
                            P.tt(pe.re("p (h q) -> p h q", h=4), pe.re("p (h q) -> p h q", h=4), masks[ck], ALU.mult)
                        P.mm(po[0:65, :], va[:, ck, g, :], pe, start=(ci == 0), stop=(ci == len(chunks) - 1))
                    oa = self.rot(st, 'aoa', [65, 512], F32, 2)
                    P.cp(oa, po[0:65, :], e='act')
                    pd = self.psn()
                    P.mm(pd[0:64, :], e64, oa, start=True, stop=False)
                    P.mm(pd[0:64, :], self.ones[0:1, 0:64], crow[:, g * 4:(g + 1) * 4, :], start=False, stop=True)
                    rd = self.rot(st, 'ard', [64, 512], F32, 2)
                    P.recip(rd, pd[0:64, :])
                    P.tt(rd, rd, oa[0:64, :], ALU.mult)
                    P.dma(S['aoT'][g * 256:(g + 1) * 256, qa:qa + 128].re("(h d) t -> d h t", h=4).k(qb),
                          rd.re("d (h t) -> d h t", h=4), q='pool')
        P.barrier()

    def stageB_rglru(self, l):
        P, j = self.P, l // 2
        NT = self.NT
        S = self.scr
        if 'rhf' not in S:
            self.S('rhf', [4, 128, NT]); self.S('rgT', [4, 128, NT])
        with ExitStack() as st:
            cw = P.sb(st, 'rcw', [128, 4, 4]); cb = P.sb(st, 'rcb', [128, 4])
            P.dma(cw, self.d_conv[j]); P.dma(cb, self.d_convb[j])
            br = P.sb(st, 'rbr', [128, 2, 4]); bi = P.sb(st, 'rbi', [128, 2, 4]); lam = P.sb(st, 'rlam', [128, 2, 4])
            P.dma(br, self.d_br[j]); P.dma(bi, self.d_bi[j]); P.dma(lam, self.d_lam[j])
            coef = P.sb(st, 'rcoef', [128, 2, 4]); coef2 = P.sb(st, 'rcoef2', [128, 2, 4])
            P.act(coef, lam, AF.Exp, scale=-1.0)
            P.act(coef, coef, AF.Ln, bias=self.onec)
            P.ts(coef2, coef, -16.0, None, ALU.mult)
            P.ts(coef, coef, -8.0, None, ALU.mult)
            Wr = P.sb(st, 'rWr', [128, 2, 4, 128]); Wi = P.sb(st, 'rWi', [128, 2, 4, 128])
            P.memset(Wr, 0.0); P.memset(Wi, 0.0)
            for z in range(2):
                for c in range(4):
                    for hh in range(2):
                        sl = slice(hh * 64, (hh + 1) * 64)
                        P.dma(Wr[sl, z, c, sl], self.d_wr[j, z, 2 * c + hh])
                        P.dma(Wi[sl, z, c, sl], self.d_wi[j, z, 2 * c + hh])
            hst = P.sb(st, 'rhst', [128, 2, 4])
            P.memset(hst, 0.0)
            for z in range(2):
                order = []
                for seg in (0, 1):
                    gs = [g for g in self.groups if g[0] == seg]
                    order += gs if z == 0 else gs[::-1]
                rv = (lambda v: v) if z == 0 else (lambda v: v[:, ::-1])
                for (seg, t0, ln) in order:
                    a, b = self.segs[seg]
                    cin = self.rot(st, 'rcin', [128, 4, 515], F32, 2)
                    lo, hi = max(a, t0 - 1), min(b, t0 + ln + 2)
                    if lo > t0 - 1 or hi < t0 + ln + 2:
                        P.memset(cin, 0.0)
                    P.dma(cin[:, :, lo - (t0 - 1):hi - (t0 - 1)], S['xdT'][:, :, lo:hi].re("c p t -> p c t"))
                    if z == 1:
                        hf = self.rot(st, 'rhfl', [128, 4, 512], F32, 2)
                        gg = self.rot(st, 'rggl', [128, 4, 512], F32, 2)
                        P.dma(hf[:, :, :ln], S['rhf'][:, :, t0:t0 + ln].re("c p t -> p c t"))
                        P.dma(gg[:, :, :ln], S['ggT'][:, :, t0:t0 + ln].re("c p t -> p c t"))
                    for c in range(4):
                        xc = self.rot(st, 'rxc', [128, 512], F32, 3)
                        P.ts(xc[:, :ln], cin[:, c, 0:ln], cw[:, c, 0:1], cb[:, c:c + 1], ALU.mult, ALU.add)
                        for tap in range(1, 4):
                            P.stt(xc[:, :ln], cin[:, c, tap:tap + ln], cw[:, c, tap:tap + 1], xc[:, :ln], ALU.mult, ALU.add)
                        p_r = self.psn(); p_i = self.psn()
                        P.mm(p_r[:, :ln], Wr[:, z, c, :], xc[:, :ln])
                        P.mm(p_i[:, :ln], Wi[:, z, c, :], xc[:, :ln])
                        r = self.rot(st, 'rr', [128, 512], F32, 2); gi_ = self.rot(st, 'rgi', [128, 512], F32, 2)
                        P.act(r[:, :ln], p_r[:, :ln], AF.Sigmoid, bias=br[:, z, c:c + 1])
                        P.act(gi_[:, :ln], p_i[:, :ln], AF.Sigmoid, bias=bi[:, z, c:c + 1])
                        aa = self.rot(st, 'raa', [128, 512], F32, 2); a2 = self.rot(st, 'ra2', [128, 512], F32, 2)
                        P.act(aa[:, :ln], r[:, :ln], AF.Exp, scale=coef[:, z, c:c + 1])
                        P.act(a2[:, :ln], r[:, :ln], AF.Exp, scale=coef2[:, z, c:c + 1])
                        P.ts(a2[:, :ln], a2[:, :ln], -1.0, 1.0, ALU.mult, ALU.add)
                        P.act(a2[:, :ln], a2[:, :ln], AF.Sqrt)
                        P.tt(a2[:, :ln], a2[:, :ln], gi_[:, :ln], ALU.mult)
                        P.tt(a2[:, :ln], a2[:, :ln], xc[:, :ln], ALU.mult, e='pool')
                        hh_ = self.rot(st, 'rhh', [128, 512], F32, 3)
                        P.scan(rv(hh_[:, :ln]), rv(aa[:, :ln]), rv(a2[:, :ln]), hst[:, z, c:c + 1])
                        e_ = ln - 1 if z == 0 else 0
                        P.cp(hst[:, z, c:c + 1], hh_[:, e_:e_ + 1])
                        if z == 0:
                            P.dma(S['rhf'][c, :, t0:t0 + ln].k('%d_%d' % (t0, c)), hh_[:, :ln], q='pool')
                        else:
                            P.tt(hh_[:, :ln], hh_[:, :ln], hf[:, c, :ln], ALU.add)
                            P.tt(hh_[:, :ln], hh_[:, :ln], gg[:, c, :ln], ALU.mult, e='pool')
                            P.dma(S['rgT'][c, :, t0:t0 + ln].k('%d_%d' % (t0, c)), hh_[:, :ln], q='pool')
        P.barrier()

    def odd_oT(self, st, oT, t0, ln):
        P, S = self.P, self.scr
        a = self.rot(st, 'ooa', [128, 4, 512], F32, 2)
        r = self.rot(st, 'oor', [128, 4, 512], F32, 2)
        P.dma(a[:, :, :ln], S['aoT'][:, t0:t0 + ln].re("(c p) t -> p c t", p=128))
        P.dma(r[:, :, :ln], S['rgT'][:, :, t0:t0 + ln].re("c p t -> p c t"))
        P.cp(oT[:, 0:4, :ln], a[:, :, :ln], e='act')
        P.cp(oT[:, 4:8, :ln], r[:, :, :ln])


def host_maps(inp, nb, depth):
    f = lambda a: np.ascontiguousarray(np.asarray(a, dtype=np.float32))
    n_ev, n_od = (depth + 1) // 2, depth // 2
    sh = {}
    for k in ('w_mod', 'b_mod', 'norm1_w', 'norm2_w', 'moe_router', 'moe_w1', 'moe_w3', 'moe_w2'):
        sh[k] = f(inp[k][:depth])
    sh['final_norm_w'] = f(inp['final_norm_w']).reshape(1, D)
    sh['ev_w_in'] = f(inp['ev_w_in'][:n_ev])
    sh['ev_w_out'] = f(inp['ev_w_out'][:n_ev])
    sh['a_conv'] = f(np.asarray(inp['a_conv_w'])[:n_ev].reshape(n_ev, 4, 12, 128).transpose(0, 3, 2, 1))
    sh['a_log'] = f(np.asarray(inp['a_log'])[:n_ev].reshape(n_ev, 8))
    sh['a_dtb'] = f(np.asarray(inp['a_dt_bias'])[:n_ev].reshape(n_ev, 8))
    sh['a_norm_w'] = f(inp['a_norm_w'][:n_ev])
    sh['b_lbl'] = f(np.asarray(inp['b_lb_logits']).reshape(2, 8, 128).transpose(2, 0, 1))
    sh['b_norm_w'] = f(inp['b_norm_w'][:n_ev])
    if n_od:
        sh['od_w_in'] = f(inp['od_w_in'][:n_od])
        sh['od_w_out'] = f(inp['od_w_out'][:n_od])
        sh['c_sink'] = f(inp['c_sink'][:n_od])
        sh['d_conv'] = f(np.asarray(inp['d_conv_w'])[:n_od].reshape(n_od, 4, 4, 128).transpose(0, 3, 2, 1))
        sh['d_convb'] = f(np.asarray(inp['d_conv_b'])[:n_od].reshape(n_od, 4, 128).transpose(0, 2, 1))
        sh['d_wr'] = f(inp['d_w_r'][:n_od])
        sh['d_wi'] = f(inp['d_w_i'][:n_od])
        for kk, src in (('d_br', 'd_b_r'), ('d_bi', 'd_b_i'), ('d_lam', 'd_lambda')):
            sh[kk] = f(np.asarray(inp[src])[:n_od].reshape(n_od, 2, 4, 128).transpose(0, 3, 1, 2))
    maps = []
    cc = np.asarray(inp['c_ctx'], np.float32).reshape(8, 128).T
    for b in range(nb):
        m = dict(sh)
        m['xin'] = f(np.concatenate([np.asarray(inp['ctx'][b]), np.asarray(inp['x'][b])], axis=0))
        cb = np.asarray(inp['c'][b], np.float32).reshape(8, 128).T
        m['cvec'] = f(np.concatenate([cb, cc], axis=1))
        maps.append(m)
    return maps


_CACHE = {}


def build_net(N, LC, depth):
    key = (N, LC, depth)
    if key not in _CACHE:
        net = Net(N, LC, depth)
        for l in range(depth):
            net.layer(l)
        net.final_norm()
        net.P.barrier()
        _CACHE[key] = net
    return _CACHE[key]


def kernel(**inputs):
    x = np.asarray(inputs['x'])
    B, N, _ = x.shape
    LC = np.asarray(inputs['ctx']).shape[1]
    depth = np.asarray(inputs['w_mod']).shape[0]
    net = build_net(N, LC, depth)
    maps = host_maps(inputs, B, depth)
    res = run_bass_kernel_spmd(net.nc, maps, core_ids=list(range(B)))
    return np.stack([np.asarray(r['out'], dtype=np.float32) for r in res.results], axis=0)
```

```python
import numpy as np
import concourse.bass as bass
import concourse.mybir as mybir
from concourse.bass_utils import run_bass_kernel_spmd
from contextlib import ExitStack

F32 = mybir.dt.float32
BF16 = mybir.dt.bfloat16
I32 = mybir.dt.int32
AF = mybir.ActivationFunctionType
ALU = mybir.AluOpType

D = 1024
EPS = 1e-6
NEG = -1.0e30
ATTACH = True


class V:
    def __init__(self, key, ap):
        self.key = key
        self.ap = ap

    def __getitem__(self, k):
        return V(self.key, self.ap[k])

    def bc(self, shape):
        return V(self.key, self.ap.broadcast_to(list(shape)))

    def re(self, pat, **kw):
        return V(self.key, self.ap.rearrange(pat, **kw))

    def k(self, suffix):
        return V(self.key + ':' + str(suffix), self.ap)

    def pb(self, n):
        return V(self.key, self.ap.partition_broadcast(n))

    def bitcast(self, dt):
        return V(self.key, self.ap.bitcast(dt))


class Prog:
    def __init__(self, nc, es, n_dma_sems=24):
        self.nc = nc
        self.es = es
        self.eng = {'pe': nc.tensor, 'dve': nc.vector, 'act': nc.scalar, 'pool': nc.gpsimd, 'sp': nc.sync}
        self.sem = {k: es.enter_context(nc.semaphore('s_' + k)) for k in self.eng}
        self.cnt = {k: 0 for k in self.eng}
        self.dsem = [es.enter_context(nc.semaphore('d%d' % i)) for i in range(n_dma_sems)]
        self.dval = [0] * n_dma_sems
        self.dnext = 0
        self.seen = {k: {} for k in self.eng}
        self.last_w = {}
        self.reads = {}
        self.nins = 0

    def _need(self, e, deps, attach=False):
        todo = []
        for key, val in deps.items():
            if self.seen[e].get(key, 0) >= val:
                continue
            sem = self.sem[key[1]] if key[0] == 'e' else self.dsem[key[1]]
            todo.append((sem, val))
            self.seen[e][key] = val
        keep = None
        if attach and todo:
            keep = todo.pop()
        for sem, val in todo:
            self.eng[e].wait_ge(sem, val)
            self.nwait = getattr(self, 'nwait', 0) + 1
        return keep

    def _deps(self, R, W):
        deps = {}

        def add(k, v):
            if deps.get(k, 0) < v:
                deps[k] = v
        for b in R:
            if b in self.last_w:
                add(*self.last_w[b])
        for b in W:
            if b in self.last_w:
                add(*self.last_w[b])
            for k, v in self.reads.get(b, {}).items():
                add(k, v)
        return deps

    def _commit(self, R, W, key, val):
        for b in R:
            self.reads.setdefault(b, {})[key] = val
        for b in W:
            self.last_w[b] = (key, val)
            self.reads[b] = {}

    def op(self, e, R, W, fn):
        R = [v.key for v in R if isinstance(v, V)]
        W = [v.key for v in W]
        deps = self._deps(R, W)
        if e == 'pe':
            deps.pop(('e', 'pe'), None)
        keep = self._need(e, deps, attach=ATTACH)
        ins = fn()
        if keep is not None:
            ins._wait_ge(keep[0], keep[1])
        self.cnt[e] += 1
        ins.then_inc(self.sem[e], 1)
        self._commit(R, W, ('e', e), self.cnt[e])
        self.nins += 1
        return ins

    def dma(self, out, in_, q='sp'):
        R, W = [in_.key], [out.key]
        deps = self._deps(R, W)
        i = self.dnext
        self.dnext = (self.dnext + 1) % len(self.dsem)
        if self.dval[i] > 0:
            deps[('d', i)] = max(deps.get(('d', i), 0), self.dval[i])
        keep = self._need(q, deps, attach=ATTACH)
        ins = self.eng[q].dma_start(out=out.ap, in_=in_.ap)
        if keep is not None:
            ins._wait_ge(keep[0], keep[1])
        self.dval[i] += 16
        ins.then_inc(self.dsem[i], 16)
        self._commit(R, W, ('d', i), self.dval[i])
        self.nins += 1

    def barrier(self):
        deps = {('e', k): c for k, c in self.cnt.items() if c > 0}
        for i, v in enumerate(self.dval):
            if v > 0:
                deps[('d', i)] = v
        for e in self.eng:
            d = dict(deps)
            d.pop(('e', e), None)
            self._need(e, d)
        self.last_w = {}
        self.reads = {}

    def sb(self, st, name, shape, dt=F32):
        self.uid = getattr(self, 'uid', 0) + 1
        name = '%s_u%d' % (name, self.uid)
        t = st.enter_context(self.nc.sbuf_tensor(name, list(shape), dt))
        return V(name, t[:])

    def dram(self, name, shape, dt=F32, kind="Internal"):
        t = self.nc.dram_tensor(name, list(shape), dt, kind=kind)
        return V(name, t.ap())

    def act(self, out, in_, func, bias=None, scale=None, accum=None, eng=None):
        kw = {}
        if bias is not None:
            kw['bias'] = bias.ap if isinstance(bias, V) else bias
        if scale is not None:
            kw['scale'] = scale.ap if isinstance(scale, V) else scale
        W = [out]
        if accum is not None:
            kw['accum_out'] = accum.ap
            W.append(accum)
        return self.op('act', [in_, bias, scale], W,
                       lambda: self.nc.scalar.activation(out=out.ap, in_=in_.ap, func=func, **kw))

    def tt(self, out, a, b, op, e='dve'):
        en = self.eng[e]
        return self.op(e, [a, b], [out], lambda: en.tensor_tensor(out=out.ap, in0=a.ap, in1=b.ap, op=op))

    def ts(self, out, a, s1, s2=None, op0=ALU.mult, op1=None, accum=None, e='dve'):
        en = self.eng[e]
        kw = {}
        if op1 is not None:
            kw['op1'] = op1
        W = [out]
        if accum is not None:
            kw['accum_out'] = accum.ap
            W.append(accum)
        g = lambda s: s.ap if isinstance(s, V) else s
        return self.op(e, [a, s1, s2], W,
                       lambda: en.tensor_scalar(out=out.ap, in0=a.ap, scalar1=g(s1), scalar2=g(s2), op0=op0, **kw))

    def stt(self, out, a, s, b, op0, op1):
        g = s.ap if isinstance(s, V) else s
        return self.op('dve', [a, s, b], [out],
                       lambda: self.nc.vector.scalar_tensor_tensor(out=out.ap, in0=a.ap, scalar=g, in1=b.ap, op0=op0, op1=op1))

    def cp(self, out, in_, e='dve'):
        if e == 'act':
            return self.op('act', [in_], [out], lambda: self.nc.scalar.activation(out=out.ap, in_=in_.ap, func=AF.Copy))
        en = self.eng[e]
        return self.op(e, [in_], [out], lambda: en.tensor_copy(out=out.ap, in_=in_.ap))

    def recip(self, out, in_):
        return self.op('dve', [in_], [out], lambda: self.nc.vector.reciprocal(out=out.ap, in_=in_.ap))

    def memset(self, out, val, e='pool'):
        en = self.eng[e]
        return self.op(e, [], [out], lambda: en.memset(out.ap, val))

    def mm(self, out, lhsT, rhs, start=True, stop=True):
        return self.op('pe', [lhsT, rhs], [out],
                       lambda: self.nc.tensor.matmul(out.ap, lhsT=lhsT.ap, rhs=rhs.ap, start=start, stop=stop))

    def tr(self, out, in_, ident):
        return self.op('pe', [in_, ident], [out],
                       lambda: self.nc.tensor.transpose(out=out.ap, in_=in_.ap, identity=ident.ap))

    def scan(self, out, d0, d1, init, op0=ALU.mult, op1=ALU.add):
        g = init.ap if isinstance(init, V) else init
        return self.op('dve', [d0, d1, init], [out],
                       lambda: self.nc.vector.tensor_tensor_scan(out=out.ap, data0=d0.ap, data1=d1.ap, initial=g, op0=op0, op1=op1))

    def reduce(self, out, in_, op, axis=mybir.AxisListType.X):
        return self.op('dve', [in_], [out], lambda: self.nc.vector.tensor_reduce(out=out.ap, in_=in_.ap, axis=axis, op=op))

    def aselect(self, out, in_, pattern, cmp, fill, base, cm):
        return self.op('pool', [in_], [out],
                       lambda: self.nc.gpsimd.affine_select(out=out.ap, in_=in_.ap, pattern=pattern, compare_op=cmp,
                                                            fill=fill, base=base, channel_multiplier=cm))

    def iota(self, out, pattern, base, cm):
        return self.op('pool', [], [out],
                       lambda: self.nc.gpsimd.iota(out.ap, pattern=pattern, base=base, channel_multiplier=cm))


EV_FM = [('qkv', c, c * 128) for c in range(12)] + [('qb', c, 2064 + c * 128) for c in range(4)] + \
        [('fb', c, 3088 + c * 128) for c in range(8)]
EV_TM = [('ga', 1536, 512), ('ab', 2048, 16), ('ib', 2576, 512), ('gb', 4112, 512)]


class Net:
    def __init__(self, N, LC, depth, dbg=()):
        self.N, self.LC, self.NT, self.depth = N, LC, N + LC, depth
        self.dbg = set(dbg)
        self.nc = nc = bass.Bass("TRN2", target_bir_lowering=False)
        self.es = ExitStack()
        self.P = P = Prog(nc, self.es)
        NT = self.NT
        n_ev, n_od = (depth + 1) // 2, depth // 2
        I = lambda name, shape: P.dram(name, shape, F32, kind="ExternalInput")
        self.xin = I("xin", [NT, D])
        self.cvec = I("cvec", [128, 16])
        self.w_mod = I("w_mod", [depth, D, 6 * D])
        self.b_mod = I("b_mod", [depth, 6 * D])
        self.norm1_w = I("norm1_w", [depth, D])
        self.norm2_w = I("norm2_w", [depth, D])
        self.final_norm_w = I("final_norm_w", [1, D])
        self.ev_w_in = I("ev_w_in", [n_ev, D, 4624])
        self.ev_w_out = I("ev_w_out", [n_ev, D, D])
        self.a_conv = I("a_conv", [n_ev, 128, 12, 4])
        self.a_log = I("a_log", [n_ev, 8])
        self.a_dtb = I("a_dtb", [n_ev, 8])
        self.a_norm_w = I("a_norm_w", [n_ev, 128])
        self.b_lbl = I("b_lbl", [128, 2, 8])
        self.b_norm_w = I("b_norm_w", [n_ev, 128])
        if n_od:
            self.od_w_in = I("od_w_in", [n_od, D, 1792])
            self.od_w_out = I("od_w_out", [n_od, D, D])
            self.c_sink = I("c_sink", [n_od, 8])
            self.d_conv = I("d_conv", [n_od, 128, 4, 4])
            self.d_convb = I("d_convb", [n_od, 128, 4])
            self.d_wr = I("d_wr", [n_od, 2, 8, 64, 64])
            self.d_wi = I("d_wi", [n_od, 2, 8, 64, 64])
            self.d_br = I("d_br", [n_od, 128, 2, 4])
            self.d_bi = I("d_bi", [n_od, 128, 2, 4])
            self.d_lam = I("d_lam", [n_od, 128, 2, 4])
        self.moe_router = I("moe_router", [depth, D, 16])
        self.moe_w1 = I("moe_w1", [depth, 16, D, D])
        self.moe_w3 = I("moe_w3", [depth, 16, D, D])
        self.moe_w2 = I("moe_w2", [depth, 16, D, D])
        self.out = P.dram("out", [N, D], F32, kind="ExternalOutput")
        self.scr = {}
        self.xs = self.S("xs", [NT, D])
        self.vecs = self.S("vecs", [depth, 2, 6, D])
        self.groups = []
        for seg, (a, b) in enumerate([(0, LC), (LC, NT)]):
            t = a
            while t < b:
                ln = min(512, b - t)
                self.groups.append((seg, t, ln))
                t += ln
        self.segs = [(0, LC), (LC, NT)]
        self.ps = []
        for i in range(8):
            t = self.es.enter_context(nc.psum_tensor("ps%d" % i, [128, 512], F32))
            self.ps.append(V("ps%d" % i, t[:]))
        self.psi = 0
        self.consts()

    def S(self, name, shape, dt=F32):
        kind = "ExternalOutput" if name in self.dbg else "Internal"
        v = self.P.dram(name, shape, dt, kind=kind)
        self.scr[name] = v
        return v

    def psn(self):
        p = self.ps[self.psi]
        self.psi = (self.psi + 1) % 8
        return p

    def consts(self):
        P, es = self.P, self.es
        self.ident = P.sb(es, "ident", [128, 128])
        P.memset(self.ident, 1.0)
        P.aselect(self.ident, self.ident, [[-1, 128]], ALU.is_equal, 0.0, 0, 1)
        self.identb = P.sb(es, "identb", [128, 128], BF16)
        P.cp(self.identb, self.ident)
        self.ones = P.sb(es, "ones", [128, 128])
        P.memset(self.ones, 1.0)
        self.epsc = P.sb(es, "epsc", [128, 1])
        P.memset(self.epsc, EPS)
        self.onec = P.sb(es, "onec", [128, 1])
        P.memset(self.onec, 1.0)
        cv = P.sb(es, "cv", [128, 16])
        P.dma(cv, self.cvec)
        self.csil = P.sb(es, "csil", [128, 16])
        P.act(self.csil, cv, AF.Silu)
        lbl = P.sb(es, "lbl", [128, 2, 8])
        P.dma(lbl, self.b_lbl)
        self.lb = P.sb(es, "lb", [128, 2, 8])
        self.oml = P.sb(es, "oml", [128, 2, 8])
        P.memset(self.lb, 0.0)
        P.tt(self.lb[:, 1, :], lbl[:, 1, :], lbl[:, 0, :], ALU.subtract)
        P.act(self.lb[:, 1, :], self.lb[:, 1, :], AF.Sigmoid)
        P.ts(self.oml, self.lb, -1.0, 1.0, ALU.mult, ALU.add)

    def vec(self, l, seg, i):
        r = 0 if seg == 1 else 1
        return self.vecs[l, r:r + 1, i, :].pb(128)

    def mod_prep(self, l):
        P = self.P
        with ExitStack() as st:
            wm = [P.sb(st, 'wm%d' % i, [128, 3072]) for i in range(2)]
            modsb = P.sb(st, 'modsb', [2, 6144])
            bm = P.sb(st, 'bm', [2, 6144])
            P.dma(bm, self.b_mod[l:l + 1, :].pb(2))
            cnt = 0
            for half in range(2):
                banks = [self.psn() for _ in range(6)]
                for k in range(8):
                    w = wm[cnt % 2]
                    cnt += 1
                    P.dma(w, self.w_mod[l, k * 128:(k + 1) * 128, half * 3072:(half + 1) * 3072])
                    for j in range(6):
                        P.mm(banks[j][0:2, :], self.csil[:, k:16:8], w[:, j * 512:(j + 1) * 512],
                             start=(k == 0), stop=(k == 7))
                for j in range(6):
                    c0 = half * 3072 + j * 512
                    P.tt(modsb[:, c0:c0 + 512], banks[j][0:2, :], bm[:, c0:c0 + 512], ALU.add)
            n1 = P.sb(st, 'n1', [2, D])
            n2 = P.sb(st, 'n2', [2, D])
            P.dma(n1, self.norm1_w[l:l + 1, :].pb(2))
            P.dma(n2, self.norm2_w[l:l + 1, :].pb(2))
            vv = P.sb(st, 'vv', [2, 6, D])
            m = lambda i: modsb[:, i * D:(i + 1) * D]
            P.stt(vv[:, 0, :], m(1), 1.0, n1, ALU.add, ALU.mult)
            P.cp(vv[:, 1, :], m(0))
            P.cp(vv[:, 2, :], m(2))
            P.stt(vv[:, 3, :], m(4), 1.0, n2, ALU.add, ALU.mult)
            P.cp(vv[:, 4, :], m(3))
            P.cp(vv[:, 5, :], m(5))
            P.dma(self.vecs[l], vv, q='pool')
        P.barrier()

    def norm_mod_T(self, st, src, t0, ln, A, B, hT, tag, hf_out=None):
        P = self.P
        for ti in range(ln // 128):
            xt = self.rot(st, tag + 'xt', [128, D], F32, 2)
            P.dma(xt, src[t0 + ti * 128:t0 + (ti + 1) * 128, :])
            junk = self.rot(st, tag + 'junk', [128, D], F32, 1)
            ss = self.rot(st, tag + 'ss', [128, 1], F32, 2)
            P.act(junk, xt, AF.Square, accum=ss)
            P.act(ss, ss, AF.Sqrt, scale=1.0 / D, bias=self.epsc)
            P.recip(ss, ss)
            P.stt(junk, xt, ss, A, ALU.mult, ALU.mult)
            if hf_out is not None:
                hf = hf_out(ti)
                P.tt(hf, junk, B, ALU.add)
                src_h = hf
            hb = self.rot(st, tag + 'hb', [128, D], BF16, 2)
            P.tt(hb, junk, B, ALU.add)
            pt = self.psn()
            ptb = pt.bitcast(BF16)
            for k in range(8):
                P.tr(ptb[:, k * 128:(k + 1) * 128], hb[:, k * 128:(k + 1) * 128], self.identb)
            P.cp(hT[:, :, ti * 128:(ti + 1) * 128], ptb.re("p (k t) -> p k t", k=8), e='act')

    def rot(self, st, name, shape, dt, n):
        d = st.__dict__.setdefault('_rot', {})
        if name not in d:
            d[name] = [[self.P.sb(st, '%s_%d' % (name, i), shape, dt) for i in range(n)], 0]
        bufs, i = d[name]
        d[name][1] = i + 1
        return bufs[i % n]

    def load_w_bf16(self, dst, src, ncols, q='pool'):
        K = dst.ap.shape[1]
        for k in range(K):
            c = 0
            while c < ncols:
                w = min(2048, ncols - c)
                self.P.dma(dst[:, k, c:c + w], src[k * 128:(k + 1) * 128, c:c + w], q=q)
                c += w

    def stageA_even(self, l):
        P, j = self.P, l // 2
        NT = self.NT
        S = self.scr
        if 'qkvT' not in S:
            self.S('qkvT', [12, 128, NT]); self.S('hqT', [4, 128, NT]); self.S('lfT', [8, 128, NT])
            self.S('hkT', [8, 128, NT]); self.S('tm_ga', [NT, 512]); self.S('tm_gb', [NT, 512])
            self.S('tm_ib', [NT, 512]); self.S('tm_g', [NT, 8]); self.S('tm_bt', [NT, 8])
        src = self.xin if l == 0 else self.xs
        with ExitStack() as st:
            w = P.sb(st, 'wA', [128, 8, 4624], BF16)
            self.load_w_bf16(w, self.ev_w_in[j], 4624)
            AB = {}
            for seg in (0, 1):
                AB[seg] = (P.sb(st, 'A1_%d' % seg, [128, D]), P.sb(st, 'B1_%d' % seg, [128, D]))
                P.dma(AB[seg][0], self.vec(l, seg, 0))
                P.dma(AB[seg][1], self.vec(l, seg, 1))
            al = P.sb(st, 'alog', [128, 8]); dtb = P.sb(st, 'dtb', [128, 8])
            P.dma(al, self.a_log[j:j + 1, :].pb(128))
            P.dma(dtb, self.a_dtb[j:j + 1, :].pb(128))
            negA = P.sb(st, 'negA', [128, 8])
            P.act(negA, al, AF.Exp)
            P.ts(negA, negA, -1.0, None, ALU.mult)
            hT = P.sb(st, 'hT', [128, 8, 512], BF16)
            for gi, (seg, t0, ln) in enumerate(self.groups):
                self.norm_mod_T(st, src, t0, ln, AB[seg][0], AB[seg][1], hT, 'A')
                for kind, c, col0 in EV_FM:
                    ps = self.psn()
                    for k in range(8):
                        P.mm(ps[:, :ln], w[:, k, col0:col0 + 128], hT[:, k, :ln], start=(k == 0), stop=(k == 7))
                    o = self.rot(st, 'Ao', [128, 512], F32, 4)
                    if kind == 'qkv':
                        P.cp(o[:, :ln], ps[:, :ln])
                        P.dma(S['qkvT'][c, :, t0:t0 + ln].k(gi), o[:, :ln], q='pool')
                    elif kind == 'qb':
                        P.act(o[:, :ln], ps[:, :ln], AF.Silu)
                        P.dma(S['hqT'][c, :, t0:t0 + ln].k(gi), o[:, :ln], q='pool')
                    else:
                        o2 = self.rot(st, 'Ao2', [128, 512], F32, 2)
                        P.act(o[:, :ln], ps[:, :ln], AF.Sigmoid)
                        P.ts(o[:, :ln], o[:, :ln], self.oml[:, j, c:c + 1], self.lb[:, j, c:c + 1], ALU.mult, ALU.add)
                        P.act(o2[:, :ln], o[:, :ln], AF.Ln)
                        P.dma(S['lfT'][c, :, t0:t0 + ln].k(gi), o2[:, :ln], q='pool')
                        o3 = self.rot(st, 'Ao3', [128, 512], F32, 2)
                        P.ts(o3[:, :ln], o[:, :ln], -1.0, 1.0, ALU.mult, ALU.add)
                        P.dma(S['hkT'][c, :, t0:t0 + ln].k(gi), o3[:, :ln], q='pool')
                for ti in range(ln // 128):
                    ta = t0 + ti * 128
                    for kind, col0, ncol in EV_TM:
                        ps = self.psn()
                        for k in range(8):
                            P.mm(ps[:, :ncol], hT[:, k, ti * 128:(ti + 1) * 128], w[:, k, col0:col0 + ncol],
                                 start=(k == 0), stop=(k == 7))
                        o = self.rot(st, 'Ao', [128, 512], F32, 4)
                        if kind in ('ga', 'gb'):
                            P.act(o, ps, AF.Silu)
                            P.dma(S['tm_' + kind][ta:ta + 128, :].k(gi), o, q='pool')
                        elif kind == 'ib':
                            P.cp(o, ps)
                            P.dma(S['tm_ib'][ta:ta + 128, :].k(gi), o, q='pool')
                        else:
                            P.tt(o[:, 0:8], ps[:, 0:8], dtb, ALU.add)
                            P.act(o[:, 0:8], o[:, 0:8], AF.Exp)
                            P.act(o[:, 0:8], o[:, 0:8], AF.Ln, bias=self.onec)
                            P.tt(o[:, 0:8], o[:, 0:8], negA, ALU.mult)
                            P.act(o[:, 8:16], ps[:, 8:16], AF.Sigmoid)
                            P.dma(S['tm_g'][ta:ta + 128, :].k(gi), o[:, 0:8], q='pool')
                            P.dma(S['tm_bt'][ta:ta + 128, :].k(gi), o[:, 8:16], q='pool')
        P.barrier()

    def stageB0_delta(self, l):
        P, j = self.P, l // 2
        NT = self.NT
        S = self.scr
        if 'dqT' not in S:
            self.S('dqT', [4, 128, NT]); self.S('dkT', [4, 128, NT])
            self.S('dk_tm', [NT, 512]); self.S('dv_tm', [NT, 512])
        with ExitStack() as st:
            cw = P.sb(st, 'cw', [128, 12, 4])
            P.dma(cw, self.a_conv[j])
            for gi, (seg, t0, ln) in enumerate(self.groups):
                a, b = self.segs[seg]
                cin = self.rot(st, 'cin', [128, 12, 515], F32, 2)
                lo, hi = max(a, t0 - 1), min(b, t0 + ln + 2)
                if lo > t0 - 1 or hi < t0 + ln + 2:
                    P.memset(cin, 0.0)
                P.dma(cin[:, :, lo - (t0 - 1):hi - (t0 - 1)], S['qkvT'][:, :, lo:hi].re("c p t -> p c t"))
                for c in range(12):
                    acc = self.rot(st, 'acc', [128, 512], F32, 3)
                    P.ts(acc[:, :ln], cin[:, c, 0:ln], cw[:, c, 0:1], None, ALU.mult)
                    for tap in range(1, 4):
                        P.stt(acc[:, :ln], cin[:, c, tap:tap + ln], cw[:, c, tap:tap + 1], acc[:, :ln], ALU.mult, ALU.add)
                    sl = self.rot(st, 'sl', [128, 512], F32, 3)
                    P.act(sl[:, :ln], acc[:, :ln], AF.Silu)
                    if c < 8:
                        sq = self.rot(st, 'sq', [128, 512], F32, 2)
                        P.act(sq[:, :ln], sl[:, :ln], AF.Square)
                        ps = self.psn()
                        P.mm(ps[:, :ln], self.ones, sq[:, :ln])
                        P.act(sq[:, :ln], ps[:, :ln], AF.Sqrt, bias=self.epsc)
                        P.recip(sq[:, :ln], sq[:, :ln])
                        qn = self.rot(st, 'qn', [128, 512], F32, 3)
                        P.stt(qn[:, :ln], sl[:, :ln], (128 ** -0.5) if c < 4 else 1.0, sq[:, :ln], ALU.mult, ALU.mult)
                        if c < 4:
                            P.dma(S['dqT'][c, :, t0:t0 + ln].k(gi), qn[:, :ln], q='pool')
                        else:
                            P.dma(S['dkT'][c - 4, :, t0:t0 + ln].k(gi), qn[:, :ln], q='pool')
                        srcT = qn
                    else:
                        srcT = sl
                    if c >= 4:
                        dst = S['dk_tm'] if c < 8 else S['dv_tm']
                        h = c % 4
                        ps = self.psn()
                        for ti in range(ln // 128):
                            P.tr(ps[:, ti * 128:(ti + 1) * 128], srcT[:, ti * 128:(ti + 1) * 128], self.ident)
                        tmo = self.rot(st, 'tmo', [128, 512], F32, 3)
                        P.cp(tmo[:, :ln], ps[:, :ln], e='act')
                        P.dma(dst[t0:t0 + ln, h * 128:(h + 1) * 128].re("(n p) d -> p n d", p=128).k(gi),
                              tmo[:, :ln].re("p (n d) -> p n d", d=128), q='pool')
        P.barrier()

    def chunk_consts(self):
        if hasattr(self, 'tri'):
            return
        P, es = self.P, self.es
        self.tri, self.maskL = [], []
        for z in range(2):
            sgn = 1 if z == 0 else -1
            t = P.sb(es, "tri%d" % z, [64, 64])
            P.memset(t, 1.0)
            P.aselect(t, t, [[sgn, 64]], ALU.is_ge, 0.0, 0, -sgn)
            self.tri.append(t)
            m = P.sb(es, "maskL%d" % z, [64, 4, 64])
            P.memset(m, 0.0)
            P.aselect(m, m, [[0, 4], [-sgn, 64]], ALU.is_gt, NEG, 0, sgn)
            self.maskL.append(m)
        self.ident4 = P.sb(es, "ident4", [64, 4, 64])
        P.memset(self.ident4, 1.0)
        P.aselect(self.ident4, self.ident4, [[0, 4], [-1, 64]], ALU.is_equal, 0.0, 0, 1)

    def chunk_order(self, z):
        out = []
        for a, b in self.segs:
            cs = list(range(a, b, 64))
            out += cs if z == 0 else cs[::-1]
        return out

    def stageB_delta(self, l):
        P = self.P
        S = self.scr
        NT = self.NT
        self.chunk_consts()
        if 'do_0' not in S:
            self.S('do_0', [NT, 512]); self.S('do_1', [NT, 512])
        with ExitStack() as st:
            St = [P.sb(st, 'dS%d' % z, [128, 4, 128]) for z in range(2)]
            for z in range(2):
                P.memset(St[z], 0.0)
            orders = [self.chunk_order(0), self.chunk_order(1)]
            nch = len(orders[0])
            DEP = 1
            hold = {}
            for step in range(nch + DEP):
                for z in range(2):
                    if step < nch:
                        hold[(z, step)] = self.delta_b1(st, z, orders[z][step])
                    if step >= DEP:
                        self.delta_b2(st, z, orders[z][step - DEP], St[z], hold.pop((z, step - DEP)))
        P.barrier()

    def delta_b1(self, st, z, t0):
        P, S = self.P, self.scr
        R = lambda nm, shape, n=2: self.rot(st, 'd%d%s' % (z, nm), shape, F32, n)
        g4 = R('g4', [64, 4]); bt4 = R('bt4', [64, 4])
        kT = R('kT', [128, 4, 64]); qT = R('qT', [128, 4, 64])
        ktm = R('ktm', [64, 4, 128]); vtm = R('vtm', [64, 4, 128])
        P.dma(g4, S['tm_g'][t0:t0 + 64, z * 4:(z + 1) * 4])
        P.dma(bt4, S['tm_bt'][t0:t0 + 64, z * 4:(z + 1) * 4])
        P.dma(kT, S['dkT'][:, :, t0:t0 + 64].re("h p t -> p h t"))
        P.dma(qT, S['dqT'][:, :, t0:t0 + 64].re("h p t -> p h t"))
        P.dma(ktm, S['dk_tm'][t0:t0 + 64, :].re("t (h d) -> t h d", h=4))
        P.dma(vtm, S['dv_tm'][t0:t0 + 64, :].re("t (h d) -> t h d", h=4))
        tri, maskL, I4 = self.tri[z], self.maskL[z], self.ident4
        gb = R('gb', [64, 4, 128])
        P.cp(gb, g4[:, :, None].bc([64, 4, 128]), e='pool')
        pc = self.psn()
        P.mm(pc[0:64, 0:4], tri, g4)
        cumc = R('cumc', [64, 4])
        P.cp(cumc, pc[0:64, 0:4])
        pt = self.psn()
        P.mm(pt[:, 0:4], self.ones[0:64, :], g4)
        prow = self.psn()
        for h in range(4):
            P.mm(prow[:, h * 64:(h + 1) * 64], gb[:, h, :], tri)
        prow3 = prow[:, 0:256].re("p (h f) -> p h f", h=4)
        X = R('X', [64, 4, 64])
        P.tt(X, prow3[0:64], cumc[:, :, None].bc([64, 4, 64]), ALU.subtract)
        E = R('E', [64, 4, 64])
        P.stt(E, X, -1.0, maskL, ALU.mult, ALU.add)
        P.act(E, E, AF.Exp)
        ecr = R('ecr', [128, 4, 64])
        P.act(ecr, prow3, AF.Exp)
        qdT = R('qdT', [128, 4, 64], 3)
        P.tt(qdT, qT, ecr, ALU.mult)
        glast = R('glast', [128, 4], 3)
        P.act(glast, pt[:, 0:4], AF.Exp)
        ekd = R('ekd', [64, 4])
        P.tt(ekd, pt[0:64, 0:4], cumc, ALU.subtract)
        P.act(ekd, ekd, AF.Exp)
        kdec = R('kdec', [64, 4, 128], 3)
        P.tt(kdec, ktm, ekd[:, :, None].bc([64, 4, 128]), ALU.mult, e='pool')
        ec = R('ec', [64, 4])
        P.act(ec, cumc, AF.Exp)
        P.tt(ec, ec, bt4, ALU.mult)
        Ru = R('Ru', [64, 4, 128]); Rw = R('Rw', [64, 4, 128])
        P.tt(Ru, vtm, bt4[:, :, None].bc([64, 4, 128]), ALU.mult, e='pool')
        P.tt(Rw, ktm, ec[:, :, None].bc([64, 4, 128]), ALU.mult, e='pool')
        pk = self.psn(); pq = self.psn()
        for h in range(4):
            P.mm(pk[0:64, h * 64:(h + 1) * 64], kT[:, h, :], kT[:, h, :])
        for h in range(4):
            P.mm(pq[0:64, h * 64:(h + 1) * 64], qT[:, h, :], kT[:, h, :])
        v3 = lambda p: p[0:64, 0:256].re("p (h f) -> p h f", h=4)
        Pm = R('Pm', [64, 4, 64]); Qm = R('Qm', [64, 4, 64])
        P.tt(Pm, v3(pk), bt4[:, :, None].bc([64, 4, 64]), ALU.mult)
        P.tt(Pm, Pm, E, ALU.mult)
        QK = R('QK', [64, 4, 64])
        P.tt(E, E, I4, ALU.add)
        P.tt(QK, v3(pq), E, ALU.mult)
        pn = self.psn(); pqt = self.psn()
        for h in range(4):
            P.tr(pn[0:64, h * 64:(h + 1) * 64], Pm[:, h, :], self.ident[0:64, 0:64])
        for h in range(4):
            P.tr(pqt[0:64, h * 64:(h + 1) * 64], QK[:, h, :], self.ident[0:64, 0:64])
        P.cp(Qm, v3(pn), e='act')
        QKT = R('QKT', [64, 4, 64], 3)
        P.cp(QKT, v3(pqt), e='act')
        Z = R('Z', [64, 4, 64]); Y = R('Y', [64, 4, 64])
        P.tt(Z, I4, Pm, ALU.subtract)
        P.tt(Y, I4, Qm, ALU.subtract)
        for lev in range(1, 6):
            last = lev == 5
            Pn = R('Pm', [64, 4, 64]); Qn = R('Qm', [64, 4, 64])
            if not last:
                p1 = self.psn()
                for h in range(4):
                    P.mm(p1[0:64, h * 64:(h + 1) * 64], Qm[:, h, :], Pm[:, h, :])
            p2 = self.psn()
            for h in range(4):
                P.mm(p2[0:64, h * 64:(h + 1) * 64], Pm[:, h, :], Qm[:, h, :])
            if not last:
                P.cp(Pn, v3(p1), e='act')
            P.cp(Qn, v3(p2))
            p3 = self.psn()
            for h in range(4):
                P.mm(p3[0:64, h * 64:(h + 1) * 64], Z[:, h, :], Qn[:, h, :])
            if not last:
                p4 = self.psn()
                for h in range(4):
                    P.mm(p4[0:64, h * 64:(h + 1) * 64], Y[:, h, :], Pn[:, h, :])
            Yn = R('Y', [64, 4, 64])
            P.tt(Yn, Y, v3(p3), ALU.add)
            if not last:
                Zn = R('Z', [64, 4, 64])
                P.tt(Zn, Z, v3(p4), ALU.add)
                Z = Zn
            Y, Pm, Qm = Yn, Pn, Qn
        pu = self.psn(); pw = self.psn()
        for h in range(4):
            P.mm(pu[0:64, h * 128:(h + 1) * 128], Y[:, h, :], Ru[:, h, :])
        for h in range(4):
            P.mm(pw[:, h * 64:(h + 1) * 64], Rw[:, h, :], Y[:, h, :])
        u = R('u', [64, 4, 128], 3); wT = R('wT', [128, 4, 64], 3)
        P.cp(u, pu[0:64, :].re("p (h d) -> p h d", h=4))
        P.cp(wT, pw[:, 0:256].re("p (h f) -> p h f", h=4), e='act')
        return dict(u=u, wT=wT, QKT=QKT, qdT=qdT, kdec=kdec, glast=glast)

    def delta_b2(self, st, z, t0, St, b):
        P, S = self.P, self.scr
        R = lambda nm, shape, n=2: self.rot(st, 'd%d%s' % (z, nm), shape, F32, n)
        pw = self.psn()
        for h in range(4):
            P.mm(pw[0:64, h * 128:(h + 1) * 128], b['wT'][:, h, :], St[:, h, :])
        vn = R('vn', [64, 4, 128])
        P.tt(vn, b['u'], pw[0:64, :].re("p (h d) -> p h d", h=4), ALU.subtract)
        po = self.psn()
        for h in range(4):
            P.mm(po[0:64, h * 128:(h + 1) * 128], b['qdT'][:, h, :], St[:, h, :], start=True, stop=False)
            P.mm(po[0:64, h * 128:(h + 1) * 128], b['QKT'][:, h, :], vn[:, h, :], start=False, stop=True)
        pS = self.psn()
        for h in range(4):
            P.mm(pS[:, h * 128:(h + 1) * 128], b['kdec'][:, h, :], vn[:, h, :])
        o = R('o', [64, 512])
        P.cp(o, po[0:64, :], e='act')
        P.dma(S['do_%d' % z][t0:t0 + 64, :].k(t0), o, q='pool')
        P.tt(St, St, b['glast'][:, :, None].bc([128, 4, 128]), ALU.mult)
        P.tt(St, St, pS.re("p (h d) -> p h d", h=4), ALU.add)

    def stageB_hgrn(self, l):
        P = self.P
        S = self.scr
        NT = self.NT
        self.chunk_consts()
        if 'ho_0' not in S:
            self.S('ho_0', [NT, 512]); self.S('ho_1', [NT, 512])
        with ExitStack() as st:
            self.hmask, self.hrm = [], []
            for z in range(2):
                sgn = 1 if z == 0 else -1
                m = P.sb(st, "hmask%d" % z, [64, 4, 64])
                P.memset(m, 1.0)
                P.aselect(m, m, [[0, 4], [sgn, 64]], ALU.is_ge, 0.0, 0, -sgn)
                self.hmask.append(m)
                rm = P.sb(st, "hrm%d" % z, [128, 4, 64])
                P.memset(rm, 1.0)
                e0 = 0 if z == 0 else 63
                P.memset(rm[:, :, e0:e0 + 1], 0.0)
                self.hrm.append(rm)
            St = [P.sb(st, 'hS%d' % z, [128, 4, 128]) for z in range(2)]
            for z in range(2):
                P.memset(St[z], 0.0)
            orders = [self.chunk_order(0), self.chunk_order(1)]
            nch = len(orders[0])
            DEP = 1
            hold = {}
            for step in range(nch + DEP):
                for z in range(2):
                    if step < nch:
                        hold[(z, step)] = self.hgrn_b1(st, z, orders[z][step])
                    if step >= DEP:
                        self.hgrn_b2(st, z, orders[z][step - DEP], St[z], hold.pop((z, step - DEP)))
        P.barrier()

    def hgrn_b1(self, st, z, t0):
        P, S = self.P, self.scr
        R = lambda nm, shape, n=2: self.rot(st, 'h%d%s' % (z, nm), shape, F32, n)
        lf = R('lf', [128, 4, 64]); hk = R('hk', [128, 4, 64]); q = R('q', [128, 4, 64])
        vtm = R('vtm', [64, 4, 128], 3)
        P.dma(lf, S['lfT'][z * 4:(z + 1) * 4, :, t0:t0 + 64].re("h p t -> p h t"))
        P.dma(hk, S['hkT'][z * 4:(z + 1) * 4, :, t0:t0 + 64].re("h p t -> p h t"))
        P.dma(q, S['hqT'][:, :, t0:t0 + 64].re("h p t -> p h t"))
        P.dma(vtm, S['tm_ib'][t0:t0 + 64, :].re("t (h d) -> t h d", h=4))
        b = R('b', [128, 4, 64])
        fl = lambda v: v.re("p h t -> p (h t)")
        rv = (lambda v: v) if z == 0 else (lambda v: v[:, ::-1])
        P.scan(rv(fl(b)), rv(fl(self.hrm[z])), rv(fl(lf)), 0.0)
        last = 63 if z == 0 else 0
        db = R('db', [128, 4, 64])
        P.tt(db, b, b[:, :, 32:33].bc([128, 4, 64]), ALU.subtract)
        eq = R('eq', [128, 4, 64]); ek = R('ek', [128, 4, 64])
        P.act(eq, db, AF.Exp)
        P.act(ek, db, AF.Exp, scale=-1.0)
        P.tt(eq, eq, q, ALU.mult)
        P.tt(ek, ek, hk, ALU.mult, e='pool')
        pa = self.psn()
        for h in range(4):
            P.mm(pa[0:64, h * 64:(h + 1) * 64], ek[:, h, :], eq[:, h, :])
        attT = R('attT', [64, 4, 64], 3)
        P.ts(attT, pa[0:64, 0:256].re("p (h f) -> p h f", h=4), 1.0e30, -1.0e30, ALU.min, ALU.max)
        P.tt(attT, attT, self.hmask[z], ALU.mult)
        eb = R('eb', [128, 4, 64])
        P.act(eb, b, AF.Exp)
        qeT = R('qeT', [128, 4, 64], 3)
        P.tt(qeT, eb, q, ALU.mult)
        kd = R('kd', [128, 4, 64])
        P.tt(kd, b, b[:, :, last:last + 1].bc([128, 4, 64]), ALU.subtract)
        P.act(kd, kd, AF.Exp, scale=-1.0)
        P.tt(kd, kd, hk, ALU.mult, e='pool')
        pk = self.psn()
        for h in range(4):
            P.tr(pk[0:64, h * 128:(h + 1) * 128], kd[:, h, :], self.ident)
        kdtm = R('kdtm', [64, 4, 128], 3)
        P.cp(kdtm, pk[0:64, :].re("p (h d) -> p h d", h=4), e='act')
        ebl = R('ebl', [128, 4, 1], 3)
        P.act(ebl, b[:, :, last:last + 1], AF.Exp)
        return dict(attT=attT, qeT=qeT, kdtm=kdtm, ebl=ebl, vtm=vtm)

    def hgrn_b2(self, st, z, t0, St, b):
        P, S = self.P, self.scr
        R = lambda nm, shape, n=2: self.rot(st, 'h%d%s' % (z, nm), shape, F32, n)
        po = self.psn()
        for h in range(4):
            P.mm(po[0:64, h * 128:(h + 1) * 128], b['attT'][:, h, :], b['vtm'][:, h, :], start=True, stop=False)
            P.mm(po[0:64, h * 128:(h + 1) * 128], b['qeT'][:, h, :], St[:, h, :], start=False, stop=True)
        pS = self.psn()
        for h in range(4):
            P.mm(pS[:, h * 128:(h + 1) * 128], b['kdtm'][:, h, :], b['vtm'][:, h, :])
        o = R('o', [64, 512])
        P.cp(o, po[0:64, :], e='act')
        P.dma(S['ho_%d' % z][t0:t0 + 64, :].k(t0), o, q='pool')
        P.tt(St, St, b['ebl'].bc([128, 4, 128]), ALU.mult)
        P.tt(St, St, pS.re("p (h d) -> p h d", h=4), ALU.add)

    def stageC(self, l, even):
        P, S = self.P, self.scr
        NT, j = self.NT, l // 2
        last = l == self.depth - 1
        if 'h2tm' not in S:
            self.S('h2tm', [NT, D], BF16); self.S('affT', [16, NT]); self.S('posT', [16, NT])
        src = self.xin if l == 0 else self.xs
        with ExitStack() as st:
            wo = P.sb(st, 'wo', [128, 8, D], BF16)
            self.load_w_bf16(wo, (self.ev_w_out if even else self.od_w_out)[j], D)
            rt = P.sb(st, 'rt', [128, 8, 16])
            P.dma(rt, self.moe_router[l].re("(k p) e -> p k e", p=128))
            vt = {}
            for seg in (0, 1):
                vt[seg] = [P.sb(st, 'C%d_%d' % (i, seg), [128, D]) for i in (2, 3, 4)]
                for t, i in zip(vt[seg], (2, 3, 4)):
                    P.dma(t, self.vec(l, seg, i))
            if even:
                nwa = P.sb(st, 'nwa', [128, 128]); nwb = P.sb(st, 'nwb', [128, 128])
                P.dma(nwa, self.a_norm_w[j:j + 1, :].pb(128))
                P.dma(nwb, self.b_norm_w[j:j + 1, :].pb(128))
            oT = P.sb(st, 'oT', [128, 8, 512], BF16)
            for gi, (seg, t0, ln) in enumerate(self.groups):
                if last and seg == 0:
                    continue
                G1, A2, B2 = vt[seg]
                nt = ln // 128
                if even:
                    for ti in range(nt):
                        ta = t0 + ti * 128
                        oc = self.rot(st, 'oc', [128, D], BF16, 2)
                        for mi, (pre, gname, nw) in enumerate((('do', 'tm_ga', nwa), ('ho', 'tm_gb', nwb))):
                            o0 = self.rot(st, 'o0', [128, 4, 128], F32, 2)
                            o1 = self.rot(st, 'o1', [128, 4, 128], F32, 2)
                            gt = self.rot(st, 'gt', [128, 4, 128], F32, 2)
                            P.dma(o0, S[pre + '_0'][ta:ta + 128, :].re("t (h d) -> t h d", h=4))
                            P.dma(o1, S[pre + '_1'][ta:ta + 128, :].re("t (h d) -> t h d", h=4))
                            P.dma(gt, S[gname][ta:ta + 128, :].re("t (h d) -> t h d", h=4))
                            P.tt(o0, o0, o1, ALU.add)
                            P.tt(o1, o0, o0, ALU.mult, e='pool')
                            ss = self.rot(st, 'ss4', [128, 4], F32, 2)
                            P.reduce(ss, o1, ALU.add)
                            P.act(ss, ss, AF.Sqrt, scale=1.0 / 128, bias=self.epsc)
                            P.recip(ss, ss)
                            P.tt(o0, o0, ss[:, :, None].bc([128, 4, 128]), ALU.mult)
                            P.tt(o0, o0, nw[:, None, :].bc([128, 4, 128]), ALU.mult, e='pool')
                            P.tt(oc[:, mi * 512:(mi + 1) * 512].re("t (h d) -> t h d", h=4), o0, gt, ALU.mult)
                        pt = self.psn()
                        ptb = pt.bitcast(BF16)
                        for k in range(8):
                            P.tr(ptb[:, k * 128:(k + 1) * 128], oc[:, k * 128:(k + 1) * 128], self.identb)
                        P.cp(oT[:, :, ti * 128:(ti + 1) * 128], ptb.re("p (k t) -> p k t", k=8), e='act')
                else:
                    self.odd_oT(st, oT, t0, ln)
                affs = self.rot(st, 'affs', [16, 512], F32, 2)
                for ti in range(nt):
                    ta = t0 + ti * 128
                    xt = self.rot(st, 'Cxt', [128, D], F32, 2)
                    P.dma(xt, src[ta:ta + 128, :])
                    xn = self.rot(st, 'Cxn', [128, D], F32, 2)
                    for half in range(2):
                        ps = self.psn()
                        for k in range(8):
                            P.mm(ps, oT[:, k, ti * 128:(ti + 1) * 128], wo[:, k, half * 512:(half + 1) * 512],
                                 start=(k == 0), stop=(k == 7))
                        hs = slice(half * 512, (half + 1) * 512)
                        P.tt(xn[:, hs], ps, G1[:, hs], ALU.mult)
                        P.tt(xn[:, hs], xn[:, hs], xt[:, hs], ALU.add, e='pool')
                    P.dma(self.xs[ta:ta + 128, :].k(gi), xn, q='pool')
                    junk = self.rot(st, 'Cjunk', [128, D], F32, 1)
                    ss = self.rot(st, 'Css', [128, 1], F32, 2)
                    P.act(junk, xn, AF.Square, accum=ss)
                    P.act(ss, ss, AF.Sqrt, scale=1.0 / D, bias=self.epsc)
                    P.recip(ss, ss)
                    h2 = self.rot(st, 'Ch2', [128, D], F32, 2)
                    P.stt(h2, xn, ss, A2, ALU.mult, ALU.mult)
                    P.tt(h2, h2, B2, ALU.add, e='pool')
                    pa = self.psn(); pb = self.psn()
                    for k in range(8):
                        pp = pa if k < 4 else pb
                        P.tr(pp[:, (k % 4) * 128:(k % 4 + 1) * 128], h2[:, k * 128:(k + 1) * 128], self.ident)
                    h2T = self.rot(st, 'Ch2T', [128, 8, 128], F32, 2)
                    P.cp(h2T[:, 0:4, :], pa.re("p (k t) -> p k t", k=4), e='act')
                    P.cp(h2T[:, 4:8, :], pb.re("p (k t) -> p k t", k=4))
                    h2b = self.rot(st, 'Ch2b', [128, D], BF16, 2)
                    P.cp(h2b, h2, e='pool')
                    P.dma(S['h2tm'][ta:ta + 128, :].k(gi), h2b, q='pool')
                    pl = self.psn()
                    for k in range(8):
                        P.mm(pl[:, 0:16], h2T[:, k, :], rt[:, k, :], start=(k == 0), stop=(k == 7))
                    mx = self.rot(st, 'Cmx', [128, 1], F32, 2)
                    P.reduce(mx, pl[:, 0:16], ALU.max)
                    P.ts(mx, mx, -1.0, None, ALU.mult)
                    ex = self.rot(st, 'Cex', [128, 16], F32, 2)
                    sm = self.rot(st, 'Csm', [128, 1], F32, 2)
                    P.act(ex, pl[:, 0:16], AF.Exp, bias=mx, accum=sm)
                    P.recip(sm, sm)
                    P.ts(ex, ex, sm, None, ALU.mult)
                    pT = self.psn()
                    P.tr(pT[0:16, 0:128], ex, self.ident)
                    P.cp(affs[:, ti * 128:(ti + 1) * 128], pT[0:16, 0:128], e='act')
                P.dma(S['affT'][:, t0:t0 + ln].k(gi), affs[:, :ln], q='pool')
        P.barrier()

    def stage_topk(self, l):
        P, S = self.P, self.scr
        last = l == self.depth - 1
        with ExitStack() as st:
            for seg, (a, b) in enumerate(self.segs):
                if last and seg == 0:
                    continue
                n = b - a
                kk = max(1, 2 * n // 16)
                af = P.sb(st, 'af%d' % seg, [16, n])
                jk = P.sb(st, 'jk%d' % seg, [16, n])
                P.dma(af, S['affT'][:, a:b])
                lo = P.sb(st, 'lo%d' % seg, [16, 1]); hi = P.sb(st, 'hi%d' % seg, [16, 1])
                mid = P.sb(st, 'mid%d' % seg, [16, 1]); cnt = P.sb(st, 'cnt%d' % seg, [16, 1])
                fl = P.sb(st, 'fl%d' % seg, [16, 1]); d1 = P.sb(st, 'd1%d' % seg, [16, 1]); d2 = P.sb(st, 'd2%d' % seg, [16, 1])
                P.memset(lo, 0.0, e='dve'); P.memset(hi, 2.0, e='dve')
                for it in range(36):
                    P.ts(mid, lo, hi, 0.5, ALU.add, ALU.mult)
                    P.ts(jk, af, mid, None, ALU.is_ge, ALU.add, accum=cnt)
                    P.ts(fl, cnt, float(kk), None, ALU.is_ge)
                    P.tt(d1, mid, lo, ALU.subtract)
                    P.tt(d2, hi, mid, ALU.subtract)
                    P.stt(lo, d1, fl, lo, ALU.mult, ALU.add)
                    P.stt(hi, d2, fl, mid, ALU.mult, ALU.add)
                on = P.sb(st, 'on%d' % seg, [16, n])
                P.memset(on, 1.0)
                P.ts(jk, af, lo, None, ALU.is_ge)
                P.scan(af, on, jk, 0.0)
                P.tt(af, af, jk, ALU.mult)
                P.ts(af, af, -1.0, None, ALU.add)
                P.dma(S['posT'][:, a:b], af, q='pool')
        P.barrier()

    def stage_moe_dense(self, l):
        P, S = self.P, self.scr
        last = l == self.depth - 1
        if 'wbf' not in S:
            self.S('wbf', [16, 3, D, D], BF16)
        for e in range(16):
            for i, wsrc in enumerate((self.moe_w1, self.moe_w3, self.moe_w2)):
                P.dma(S['wbf'][e, i].k('%d_%d' % (e, i)), wsrc[l, e], q='pool')
        with ExitStack() as st:
            sel = P.sb(st, 'sel', [16, 16, 128])
            P.memset(sel, 1.0)
            P.aselect(sel, sel, [[-1, 16], [0, 128]], ALU.is_equal, 0.0, 0, 1)
            G2 = {}
            for seg in (0, 1):
                G2[seg] = P.sb(st, 'G2_%d' % seg, [128, D])
                P.dma(G2[seg], self.vec(l, seg, 5))
            acc = P.sb(st, 'macc', [128, 8, D])
            hb = P.sb(st, 'mhb', [128, 8, 1024], BF16)
            gT = P.sb(st, 'mgT', [16, 1024])
            hid = P.sb(st, 'mhid', [128, 8, 512], BF16)
            blocks = []
            for seg, (a, b) in enumerate(self.segs):
                if last and seg == 0:
                    continue
                t = a
                while t < b:
                    bl = min(1024, b - t)
                    blocks.append((seg, t, bl))
                    t += bl
            for bi, (seg, t0, bl) in enumerate(blocks):
                P.memset(acc, 0.0)
                P.dma(hb[:, :, :bl], S['h2T'][:, :, t0:t0 + bl].re("k p t -> p k t"))
                P.dma(gT[:, :bl], S['gateT'][:, t0:t0 + bl])
                for e in range(16):
                    ws = []
                    for i in range(3):
                        wt = self.rot(st, 'mw%d' % i, [128, 8, D], BF16, 2)
                        P.dma(wt, S['wbf'][e, i].re("(k p) f -> p k f", p=128).k('%d_%d' % (e, i)))
                        ws.append(wt)
                    w1, w3, w2 = ws
                    for s0 in range(0, bl, 512):
                        ln = min(512, bl - s0)
                        pg = self.psn()
                        P.mm(pg[:, :ln], sel[:, e, :], gT[:, s0:s0 + ln])
                        gbc = self.rot(st, 'mgbc', [128, 512], F32, 2)
                        P.cp(gbc[:, :ln], pg[:, :ln], e='act')
                        for fc in range(8):
                            p1 = self.psn(); p3 = self.psn()
                            for k in range(8):
                                P.mm(p1[:, :ln], w1[:, k, fc * 128:(fc + 1) * 128], hb[:, k, s0:s0 + ln], start=(k == 0), stop=(k == 7))
                            for k in range(8):
                                P.mm(p3[:, :ln], w3[:, k, fc * 128:(fc + 1) * 128], hb[:, k, s0:s0 + ln], start=(k == 0), stop=(k == 7))
                            sg = self.rot(st, 'msg', [128, 512], F32, 2)
                            P.act(sg[:, :ln], p1[:, :ln], AF.Silu)
                            P.tt(sg[:, :ln], sg[:, :ln], p3[:, :ln], ALU.mult)
                            P.tt(hid[:, fc, :ln], sg[:, :ln], gbc[:, :ln], ALU.mult, e='pool')
                        for ti in range(ln // 128):
                            at = (s0 + ti * 128) // 128
                            for half in range(2):
                                po = self.psn()
                                for fc in range(8):
                                    P.mm(po, hid[:, fc, ti * 128:(ti + 1) * 128], w2[:, fc, half * 512:(half + 1) * 512],
                                         start=(fc == 0), stop=(fc == 7))
                                hs = slice(half * 512, (half + 1) * 512)
                                P.tt(acc[:, at, hs], acc[:, at, hs], po, ALU.add)
                for ti in range(bl // 128):
                    ta = t0 + ti * 128
                    xt = self.rot(st, 'mxt', [128, D], F32, 2)
                    P.dma(xt, self.xs[ta:ta + 128, :].k('m%d' % bi))
                    P.tt(acc[:, ti, :], acc[:, ti, :], G2[seg], ALU.mult)
                    P.tt(xt, xt, acc[:, ti, :], ALU.add, e='pool')
                    P.dma(self.xs[ta:ta + 128, :].k('m%d' % bi), xt, q='pool')
        P.barrier()

    def idma(self, fn, R, W):
        P = self.P
        R = [v.key for v in R]; W = [v.key for v in W]
        deps = P._deps(R, W)
        i = P.dnext
        P.dnext = (P.dnext + 1) % len(P.dsem)
        if P.dval[i] > 0:
            deps[('d', i)] = max(deps.get(('d', i), 0), P.dval[i])
        keep = P._need('pool', deps, attach=ATTACH)
        ins = fn()
        if keep is not None:
            ins._wait_ge(keep[0], keep[1])
        P.dval[i] += 16
        ins.then_inc(P.dsem[i], 16)
        P._commit(R, W, ('d', i), P.dval[i])
        P.nins += 1

    def stage_moe_gather(self, l):
        P, S, nc = self.P, self.scr, self.nc
        last = l == self.depth - 1
        NT = self.NT
        nch = NT // 128
        with ExitStack() as st:
            G2 = {}
            for seg in (0, 1):
                G2[seg] = P.sb(st, 'G2_%d' % seg, [128, D])
                P.dma(G2[seg], self.vec(l, seg, 5))
            ptm = P.sb(st, 'ptm', [128, nch, 16])
            tg = P.sb(st, 'tg', [128, nch, 16, 4], BF16)
            atm = P.sb(st, 'atm', [128, nch, 16])
            for c0 in range(0, NT, 512):
                ln = min(512, NT - c0)
                for nm, dst in (('posT', ptm), ('affT', atm)):
                    t = self.rot(st, 'mld', [16, 512], F32, 2)
                    P.dma(t[:, :ln], S[nm][:, c0:c0 + ln])
                    ps = self.psn()
                    for i in range(ln // 128):
                        P.tr(ps[:, i * 16:(i + 1) * 16], t[:, i * 128:(i + 1) * 128], self.ident[0:16, 0:16])
                    P.cp(dst[:, c0 // 128:c0 // 128 + ln // 128, :], ps[:, 0:(ln // 128) * 16].re("p (c e) -> p c e", e=16))
            ti_ = P.sb(st, 'mti', [128, nch], I32)
            P.iota(ti_, [[0, nch]], 0, 1)
            P.cp(tg[:, :, :, 0], ti_[:, :, None].bc([128, nch, 16]))
            P.iota(ti_, [[128, nch]], 0, 0)
            P.cp(tg[:, :, :, 1], ti_[:, :, None].bc([128, nch, 16]))
            P.cp(tg[:, :, :, 2], atm)
            ahi = P.sb(st, 'ahi', [128, nch, 16])
            P.cp(ahi, tg[:, :, :, 2])
            P.tt(ahi, atm, ahi, ALU.subtract)
            P.cp(tg[:, :, :, 3], ahi)
            capmax = max(1, 2 * self.N // 16)
            ii = P.sb(st, 'mii', [128, capmax], I32)
            P.iota(ii, [[1, capmax]], 0, 0)
            iof = P.sb(st, 'miof', [128, capmax])
            P.cp(iof, ii)
            xT = P.sb(st, 'mxT', [128, 8, capmax], BF16)
            hid = P.sb(st, 'mhid', [128, 8, capmax], BF16)
            rows = P.sb(st, 'mrows', [4, capmax])
            for e in range(16):
                ws = []
                for i, wsrc in enumerate((self.moe_w1, self.moe_w3, self.moe_w2)):
                    wt = self.rot(st, 'mw%d' % i, [128, 8, D], BF16, 2)
                    P.dma(wt, wsrc[l, e].re("(k p) f -> p k f", p=128), q='pool')
                    ws.append(wt)
                w1, w3, w2 = ws
                for seg, (a, b) in enumerate(self.segs):
                    if last and seg == 0:
                        continue
                    n = b - a
                    cap = max(1, 2 * n // 16)
                    halves = [(h0, min(512, cap - h0)) for h0 in range(0, cap, 512)]
                    tiles = [(s0, min(128, cap - s0)) for s0 in range(0, cap, 128)]
                    pr = [self.psn() for _ in halves]
                    c_lo, c_hi = a // 128, b // 128
                    for c in range(c_lo, c_hi):
                        oh = self.rot(st, 'moh', [128, capmax], BF16, 3)
                        P.ts(oh[:, :cap], iof[:, :cap], ptm[:, c, e:e + 1], None, ALU.is_equal)
                        for hi_, (h0, hl) in enumerate(halves):
                            P.mm(pr[hi_][0:4, :hl], tg[:, c, e, :], oh[:, h0:h0 + hl], start=(c == c_lo), stop=(c == c_hi - 1))
                    for hi_, (h0, hl) in enumerate(halves):
                        P.cp(rows[:, h0:h0 + hl], pr[hi_][0:4, :hl], e='act')
                    idxs, gcols = [], []
                    for (s0, ns) in tiles:
                        pc = self.psn()
                        P.mm(pc[0:ns, 0:4], rows[:, s0:s0 + ns], self.ident[0:4, 0:4])
                        cf = self.rot(st, 'mcf', [128, 4], F32, 10)
                        P.cp(cf[0:ns], pc[0:ns, 0:4])
                        ix = self.rot(st, 'mix', [128, 1], I32, 10)
                        P.tt(cf[0:ns, 0:1], cf[0:ns, 0:1], cf[0:ns, 1:2], ALU.add)
                        P.cp(ix[0:ns], cf[0:ns, 0:1])
                        P.tt(cf[0:ns, 2:3], cf[0:ns, 2:3], cf[0:ns, 3:4], ALU.add)
                        idxs.append(ix); gcols.append(cf)
                    for ti, (s0, ns) in enumerate(tiles):
                        xg = self.rot(st, 'mxg', [128, D], BF16, 3)
                        ix = idxs[ti]
                        self.idma(lambda: nc.gpsimd.indirect_dma_start(
                            out=xg.ap[0:ns], out_offset=None, in_=S['h2tm'].ap,
                            in_offset=bass.IndirectOffsetOnAxis(ap=ix.ap[0:ns, 0:1], axis=0)), [S['h2tm'], ix], [xg])
                        pt = self.psn()
                        ptb = pt.bitcast(BF16)
                        for k in range(8):
                            P.tr(ptb[:, k * 128:k * 128 + ns], xg[0:ns, k * 128:(k + 1) * 128], self.identb[0:ns, 0:ns])
                        P.cp(xT[:, :, s0:s0 + ns], ptb.re("p (k t) -> p k t", k=8)[:, :, 0:ns], e='act')
                    for fc in range(8):
                        for (h0, hl) in halves:
                            p1 = self.psn(); p3 = self.psn()
                            for k in range(8):
                                P.mm(p1[:, :hl], w1[:, k, fc * 128:(fc + 1) * 128], xT[:, k, h0:h0 + hl], start=(k == 0), stop=(k == 7))
                            for k in range(8):
                                P.mm(p3[:, :hl], w3[:, k, fc * 128:(fc + 1) * 128], xT[:, k, h0:h0 + hl], start=(k == 0), stop=(k == 7))
                            sg = self.rot(st, 'msg', [128, 512], F32, 2)
                            P.act(sg[:, :hl], p1[:, :hl], AF.Silu)
                            P.tt(hid[:, fc, h0:h0 + hl], sg[:, :hl], p3[:, :hl], ALU.mult)
                    for ti, (s0, ns) in enumerate(tiles):
                        y = self.rot(st, 'my', [128, D], F32, 2)
                        for half in range(2):
                            po = self.psn()
                            for fc in range(8):
                                P.mm(po[0:ns, :], hid[:, fc, s0:s0 + ns], w2[:, fc, half * 512:(half + 1) * 512],
                                     start=(fc == 0), stop=(fc == 7))
                            hs = slice(half * 512, (half + 1) * 512)
                            P.stt(y[0:ns, hs], po[0:ns, :], gcols[ti][0:ns, 2:3], G2[seg][0:ns, hs], ALU.mult, ALU.mult)
                        ix = idxs[ti]
                        self.idma(lambda: nc.gpsimd.indirect_dma_start(
                            out=self.xs.ap, out_offset=bass.IndirectOffsetOnAxis(ap=ix.ap[0:ns, 0:1], axis=0),
                            in_=y.ap[0:ns], in_offset=None, compute_op=ALU.add), [y, ix], [self.xs])
        P.barrier()

    def final_norm(self):
        P = self.P
        with ExitStack() as st:
            fw = P.sb(st, 'fw', [128, D])
            P.dma(fw, self.final_norm_w.pb(128))
            for ti in range(self.N // 128):
                ta = self.LC + ti * 128
                xt = self.rot(st, 'Fxt', [128, D], F32, 3)
                P.dma(xt, self.xs[ta:ta + 128, :])
                junk = self.rot(st, 'Fjunk', [128, D], F32, 2)
                ss = self.rot(st, 'Fss', [128, 1], F32, 2)
                P.act(junk, xt, AF.Square, accum=ss)
                P.act(ss, ss, AF.Sqrt, scale=1.0 / D, bias=self.epsc)
                P.recip(ss, ss)
                P.stt(junk, xt, ss, fw, ALU.mult, ALU.mult)
                P.dma(self.out[ti * 128:(ti + 1) * 128, :].k(ti), junk, q='pool')
        P.barrier()

    def layer(self, l):
        self.mod_prep(l)
        if l % 2 == 0:
            self.stageA_even(l)
            self.stageB0_delta(l)
            self.stageB_delta(l)
            self.stageB_hgrn(l)
            self.stageC(l, True)
        else:
            self.stageA_odd(l)
            self.stageB_attn(l)
            self.stageB_rglru(l)
            self.stageC(l, False)
        self.stage_topk(l)
        self.stage_moe_gather(l)

    def rope_tables(self):
        P, S = self.P, self.scr
        if 'ropeC' in S:
            return
        N = self.N
        self.S('ropeC', [128, N]); self.S('ropeS', [128, N])
        import math
        with ExitStack() as st:
            pi_ = P.sb(st, 'r_pi', [128, 1], I32)
            P.iota(pi_, [[0, 1]], 0, 1)
            t1 = P.sb(st, 'r_t1', [128, 1], I32); t2 = P.sb(st, 'r_t2', [128, 1], I32)
            P.ts(t1, pi_, 4, 4, ALU.arith_shift_right, ALU.logical_shift_left)
            P.tt(t1, pi_, t1, ALU.subtract)
            f16 = P.sb(st, 'r_f16', [128, 1])
            P.cp(f16, t1)
            inv = P.sb(st, 'r_inv', [128, 1])
            P.act(inv, f16, AF.Exp, scale=-math.log(10000.0) / 16.0)
            P.ts(inv, inv, 1.0 / (2 * math.pi), None, ALU.mult)
            P.ts(t2, pi_, 5, 1, ALU.arith_shift_right, ALU.bitwise_and)
            selc = P.sb(st, 'r_sel', [128, 1])
            P.cp(selc, t2)
            CW = min(N, 2048)
            ri = P.sb(st, 'r_ri', [128, CW], I32); ci = P.sb(st, 'r_ci', [128, CW], I32)
            rf = P.sb(st, 'r_rf', [128, CW]); cf = P.sb(st, 'r_cf', [128, CW])
            ys = {nm: P.sb(st, 'r_y' + nm, [128, CW]) for nm in ('ropeS', 'ropeC')}
            yi = P.sb(st, 'r_yi', [128, CW], I32)
            tm = P.sb(st, 'r_tm', [128, CW])
            for c0 in range(0, N, CW):
                P.iota(ri, [[1, CW // 64], [0, 64]], c0 // 64, 0)
                P.iota(ci, [[0, CW // 64], [1, 64]], 0, 0)
                P.cp(rf, ri); P.cp(cf, ci)
                P.tt(cf, cf, rf, ALU.subtract)
                P.stt(rf, cf, selc, rf, ALU.mult, ALU.add)
                P.ts(rf, rf, inv, None, ALU.mult)
                for name, off in (('ropeS', 0.0), ('ropeC', 0.25)):
                    y = ys[name]
                    P.ts(y, rf, off, None, ALU.add)
                    P.cp(yi, y)
                    P.cp(tm, yi)
                    P.tt(y, y, tm, ALU.subtract)
                    P.ts(tm, y, 0.5, None, ALU.is_gt)
                    P.tt(y, y, tm, ALU.subtract)
                    P.ts(tm, y, -0.5, None, ALU.is_lt)
                    P.tt(y, y, tm, ALU.add)
                    P.act(y, y, AF.Sin, scale=2 * math.pi)
                    P.dma(S[name][:, c0:c0 + CW].k(c0), y, q='pool')
        P.barrier()

    def stageA_odd(self, l):
        P, j = self.P, l // 2
        NT, N, LC = self.NT, self.N, self.LC
        S = self.scr
        self.rope_tables()
        if 'aqT' not in S:
            self.S('aqT', [4, 128, NT]); self.S('akT', [128, NT]); self.S('av_tm', [NT, 128])
            self.S('xdT', [4, 128, NT]); self.S('ggT', [4, 128, NT]); self.S('m0', [128, 1])
        src = self.xs
        with ExitStack() as st:
            w = P.sb(st, 'wAo', [128, 8, 1792], BF16)
            for k in range(8):
                rows = self.od_w_in[j, k * 128:(k + 1) * 128, :]
                for g in range(2):
                    P.dma(w[:, k, 0:512].re("p (i g d) -> p g i d", i=4, g=2)[:, g], rows[:, g * 256:(g + 1) * 256].re("p (i d) -> p i d", i=4), q='pool')
                P.dma(w[:, k, 512:1792], rows[:, 512:1792], q='pool')
            AB = {}
            for seg in (0, 1):
                AB[seg] = (P.sb(st, 'A1_%d' % seg, [128, D]), P.sb(st, 'B1_%d' % seg, [128, D]))
                P.dma(AB[seg][0], self.vec(l, seg, 0))
                P.dma(AB[seg][1], self.vec(l, seg, 1))
            piT = P.sb(st, 'piT', [128, 128])
            P.memset(piT, 0.0)
            pv = piT.re("p (b h j) -> p b h j", b=4, h=2)
            m1 = P.sb(st, 'pm1', [128, 4, 16]); p1 = P.sb(st, 'pp1', [128, 4, 16])
            P.memset(m1, -1.0); P.memset(p1, 1.0)
            P.aselect(pv[:, :, 0, :], m1, [[-32, 4], [-1, 16]], ALU.is_equal, 0.0, -16, 1)
            P.aselect(pv[:, :, 1, :], p1, [[-32, 4], [-1, 16]], ALU.is_equal, 0.0, 0, 1)
            qmax = P.sb(st, 'qmax', [128, 512]); kmax = P.sb(st, 'kmax', [128, 512])
            P.memset(qmax, 0.0); P.memset(kmax, 0.0)
            hT = P.sb(st, 'hT', [128, 8, 512], BF16)
            for gi, (seg, t0, ln) in enumerate(self.groups):
                self.norm_mod_T(st, src, t0, ln, AB[seg][0], AB[seg][1], hT, 'A')
                if seg == 1:
                    rc = self.rot(st, 'rC', [128, 512], F32, 2); rs = self.rot(st, 'rS', [128, 512], F32, 2)
                    P.dma(rc[:, :ln], S['ropeC'][:, t0 - LC:t0 - LC + ln])
                    P.dma(rs[:, :ln], S['ropeS'][:, t0 - LC:t0 - LC + ln])
                for c in range(5):
                    ps = self.psn()
                    col0 = c * 128
                    for k in range(8):
                        P.mm(ps[:, :ln], w[:, k, col0:col0 + 128], hT[:, k, :ln], start=(k == 0), stop=(k == 7))
                    x0 = self.rot(st, 'ox0', [128, 512], F32, 3)
                    if c < 4:
                        P.act(x0[:, :ln], ps[:, :ln], AF.Copy, scale=0.125)
                    else:
                        P.cp(x0[:, :ln], ps[:, :ln])
                    if seg == 1:
                        pr = self.psn()
                        P.mm(pr[:, :ln], piT, x0[:, :ln])
                        x1 = self.rot(st, 'ox1', [128, 512], F32, 2)
                        P.tt(x1[:, :ln], pr[:, :ln], rs[:, :ln], ALU.mult)
                        P.tt(x0[:, :ln], x0[:, :ln], rc[:, :ln], ALU.mult, e='pool')
                        P.tt(x0[:, :ln], x0[:, :ln], x1[:, :ln], ALU.add)
                    dst = S['aqT'][c, :, t0:t0 + ln] if c < 4 else S['akT'][:, t0:t0 + ln]
                    P.dma(dst.k(gi), x0[:, :ln], q='pool')
                    sq = self.rot(st, 'osq', [128, 512], F32, 2)
                    P.act(sq[:, :ln], x0[:, :ln], AF.Square)
                    pn = self.psn()
                    P.mm(pn[:, :ln], self.ones, sq[:, :ln])
                    mx = qmax if c < 4 else kmax
                    P.tt(mx[:, :ln], mx[:, :ln], pn[:, :ln], ALU.max)
                for c in range(8):
                    ps = self.psn()
                    col0 = 768 + c * 128
                    for k in range(8):
                        P.mm(ps[:, :ln], w[:, k, col0:col0 + 128], hT[:, k, :ln], start=(k == 0), stop=(k == 7))
                    o = self.rot(st, 'Ao', [128, 512], F32, 4)
                    if c < 4:
                        P.cp(o[:, :ln], ps[:, :ln])
                        P.dma(S['xdT'][c, :, t0:t0 + ln].k(gi), o[:, :ln], q='pool')
                    else:
                        t = self.rot(st, 'Ao2', [128, 512], F32, 2)
                        P.cp(o[:, :ln], ps[:, :ln], e='act')
                        P.tt(t[:, :ln], o[:, :ln], o[:, :ln], ALU.mult)
                        P.ts(t[:, :ln], t[:, :ln], 0.044715, 1.0, ALU.mult, ALU.add)
                        P.tt(t[:, :ln], t[:, :ln], o[:, :ln], ALU.mult)
                        P.act(t[:, :ln], t[:, :ln], AF.Sigmoid, scale=1.5957691216057308)
                        P.tt(o[:, :ln], o[:, :ln], t[:, :ln], ALU.mult)
                        P.dma(S['ggT'][c - 4, :, t0:t0 + ln].k(gi), o[:, :ln], q='pool')
                for ti in range(ln // 128):
                    ta = t0 + ti * 128
                    ps = self.psn()
                    for k in range(8):
                        P.mm(ps[:, 0:128], hT[:, k, ti * 128:(ti + 1) * 128], w[:, k, 640:768], start=(k == 0), stop=(k == 7))
                    o = self.rot(st, 'Av', [128, 128], F32, 3)
                    P.cp(o, ps[:, 0:128])
                    P.dma(S['av_tm'][ta:ta + 128, :].k(gi), o, q='pool')
            qm = P.sb(st, 'qm', [128, 1]); km = P.sb(st, 'km', [128, 1])
            P.reduce(qm, qmax, ALU.max); P.reduce(km, kmax, ALU.max)
            P.tt(qm, qm, km, ALU.mult)
            P.act(qm, qm, AF.Sqrt)
            sk = P.sb(st, 'sk', [128, 8])
            P.dma(sk, self.c_sink[j:j + 1, :].pb(128))
            P.reduce(km, sk, ALU.max)
            P.tt(qm, qm, km, ALU.max)
            P.ts(qm, qm, -1.0, None, ALU.mult)
            P.dma(S['m0'], qm, q='pool')
        P.barrier()

    def stageB_attn(self, l):
        P, j = self.P, l // 2
        NT, N, LC = self.NT, self.N, self.LC
        S = self.scr
        last = l == self.depth - 1
        if 'aoT' not in S:
            self.S('aoT', [512, NT])
        with ExitStack() as st:
            nm0 = P.sb(st, 'nm0', [128, 1])
            P.dma(nm0, S['m0'])
            mprev = P.sb(st, 'mprev', [128, 4, 128]); mnext = P.sb(st, 'mnext', [128, 4, 128])
            P.memset(mprev, 1.0); P.memset(mnext, 1.0)
            P.aselect(mprev, mprev, [[0, 4], [-1, 128]], ALU.is_ge, 0.0, 0, 1)
            P.aselect(mnext, mnext, [[0, 4], [1, 128]], ALU.is_ge, 0.0, 0, -1)
            e64 = P.sb(st, 'e64', [65, 64])
            P.memset(e64, 1.0)
            P.aselect(e64, e64, [[0, 64]], ALU.is_equal, 0.0, -64, 1)
            sk = P.sb(st, 'sk1', [1, 8])
            P.dma(sk, self.c_sink[j:j + 1, :])
            P.act(sk, sk, AF.Exp, bias=nm0[0:1, :])
            crow = P.sb(st, 'crow', [1, 8, 128])
            P.cp(crow, sk[:, :, None].bc([1, 8, 128]))
            kT = P.sb(st, 'akTs', [128, NT])
            P.dma(kT, S['akT'])
            va = P.sb(st, 'vaug', [128, NT // 128, 2, 65])
            P.memset(va, 1.0)
            for g in range(2):
                for n0 in range(0, NT // 128, 8):
                    n1 = min(NT // 128, n0 + 8)
                    P.dma(va[:, n0:n1, g, 0:64], S['av_tm'][n0 * 128:n1 * 128, g * 64:(g + 1) * 64].re("(n p) d -> p n d", p=128))
            nctx = LC // 128
            blocks = []
            if not last:
                blocks += [(b, list(range(nctx)), {}) for b in range(nctx)]
            nlat = N // 128
            for b in range(nlat):
                ch, mk = [], {}
                if b > 0:
                    ch.append(nctx + b - 1); mk[nctx + b - 1] = mprev
                ch.append(nctx + b)
                if b < nlat - 1:
                    ch.append(nctx + b + 1); mk[nctx + b + 1] = mnext
                blocks.append((nctx + b, ch + list(range(nctx)), mk))
            for (qb, chunks, masks) in blocks:
                qa = qb * 128
                qT = self.rot(st, 'aq', [128, 4, 128], F32, 2)
                P.dma(qT, S['aqT'][:, :, qa:qa + 128].re("c p t -> p c t"))
                for g in range(2):
                    pr = slice(g * 64, (g + 1) * 64)
                    po = self.psn()
                    for ci, ck in enumerate(chunks):
                        ps = self.psn()
                        P.mm(ps, kT[pr, ck * 128:(ck + 1) * 128], qT[pr, :, :])
                        pe = self.rot(st, 'ape', [128, 512], F32, 3)
                        P.act(pe, ps, AF.Exp, bias=nm0)
                        if ck in masks:
                            P.tt(pe.re("p (h q) -> p h q", h=4), pe.re("p (h q) -> p h q", h=4), masks[ck], ALU.mult)
                        P.mm(po[0:65, :], va[:, ck, g, :], pe, start=(ci == 0), stop=(ci == len(chunks) - 1))
                    oa = self.rot(st, 'aoa', [65, 512], F32, 2)
                    P.cp(oa, po[0:65, :], e='act')
                    pd = self.psn()
                    P.mm(pd[0:64, :], e64, oa, start=True, stop=False)
                    P.mm(pd[0:64, :], self.ones[0:1, 0:64], crow[:, g * 4:(g + 1) * 4, :], start=False, stop=True)
                    rd = self.rot(st, 'ard', [64, 512], F32, 2)
                    P.recip(rd, pd[0:64, :])
                    P.tt(rd, rd, oa[0:64, :], ALU.mult)
                    P.dma(S['aoT'][g * 256:(g + 1) * 256, qa:qa + 128].re("(h d) t -> d h t", h=4).k(qb),
                          rd.re("d (h t) -> d h t", h=4), q='pool')
        P.barrier()

    def stageB_rglru(self, l):
        P, j = self.P, l // 2
        NT = self.NT
        S = self.scr
        if 'rhf' not in S:
            self.S('rhf', [4, 128, NT]); self.S('rgT', [4, 128, NT])
        with ExitStack() as st:
            cw = P.sb(st, 'rcw', [128, 4, 4]); cb = P.sb(st, 'rcb', [128, 4])
            P.dma(cw, self.d_conv[j]); P.dma(cb, self.d_convb[j])
            br = P.sb(st, 'rbr', [128, 2, 4]); bi = P.sb(st, 'rbi', [128, 2, 4]); lam = P.sb(st, 'rlam', [128, 2, 4])
            P.dma(br, self.d_br[j]); P.dma(bi, self.d_bi[j]); P.dma(lam, self.d_lam[j])
            coef = P.sb(st, 'rcoef', [128, 2, 4]); coef2 = P.sb(st, 'rcoef2', [128, 2, 4])
            P.act(coef, lam, AF.Exp, scale=-1.0)
            P.act(coef, coef, AF.Ln, bias=self.onec)
            P.ts(coef2, coef, -16.0, None, ALU.mult)
            P.ts(coef, coef, -8.0, None, ALU.mult)
            Wr = P.sb(st, 'rWr', [128, 2, 4, 128]); Wi = P.sb(st, 'rWi', [128, 2, 4, 128])
            P.memset(Wr, 0.0); P.memset(Wi, 0.0)
            for z in range(2):
                for c in range(4):
                    for hh in range(2):
                        sl = slice(hh * 64, (hh + 1) * 64)
                        P.dma(Wr[sl, z, c, sl], self.d_wr[j, z, 2 * c + hh])
                        P.dma(Wi[sl, z, c, sl], self.d_wi[j, z, 2 * c + hh])
            hst = P.sb(st, 'rhst', [128, 2, 4])
            P.memset(hst, 0.0)
            for z in range(2):
                order = []
                for seg in (0, 1):
                    gs = [g for g in self.groups if g[0] == seg]
                    order += gs if z == 0 else gs[::-1]
                rv = (lambda v: v) if z == 0 else (lambda v: v[:, ::-1])
                for (seg, t0, ln) in order:
                    a, b = self.segs[seg]
                    cin = self.rot(st, 'rcin', [128, 4, 515], F32, 2)
                    lo, hi = max(a, t0 - 1), min(b, t0 + ln + 2)
                    if lo > t0 - 1 or hi < t0 + ln + 2:
                        P.memset(cin, 0.0)
                    P.dma(cin[:, :, lo - (t0 - 1):hi - (t0 - 1)], S['xdT'][:, :, lo:hi].re("c p t -> p c t"))
                    if z == 1:
                        hf = self.rot(st, 'rhfl', [128, 4, 512], F32, 2)
                        gg = self.rot(st, 'rggl', [128, 4, 512], F32, 2)
                        P.dma(hf[:, :, :ln], S['rhf'][:, :, t0:t0 + ln].re("c p t -> p c t"))
                        P.dma(gg[:, :, :ln], S['ggT'][:, :, t0:t0 + ln].re("c p t -> p c t"))
                    for c in range(4):
                        xc = self.rot(st, 'rxc', [128, 512], F32, 3)
                        P.ts(xc[:, :ln], cin[:, c, 0:ln], cw[:, c, 0:1], cb[:, c:c + 1], ALU.mult, ALU.add)
                        for tap in range(1, 4):
                            P.stt(xc[:, :ln], cin[:, c, tap:tap + ln], cw[:, c, tap:tap + 1], xc[:, :ln], ALU.mult, ALU.add)
                        p_r = self.psn(); p_i = self.psn()
                        P.mm(p_r[:, :ln], Wr[:, z, c, :], xc[:, :ln])
                        P.mm(p_i[:, :ln], Wi[:, z, c, :], xc[:, :ln])
                        r = self.rot(st, 'rr', [128, 512], F32, 2); gi_ = self.rot(st, 'rgi', [128, 512], F32, 2)
                        P.act(r[:, :ln], p_r[:, :ln], AF.Sigmoid, bias=br[:, z, c:c + 1])
                        P.act(gi_[:, :ln], p_i[:, :ln], AF.Sigmoid, bias=bi[:, z, c:c + 1])
                        aa = self.rot(st, 'raa', [128, 512], F32, 2); a2 = self.rot(st, 'ra2', [128, 512], F32, 2)
                        P.act(aa[:, :ln], r[:, :ln], AF.Exp, scale=coef[:, z, c:c + 1])
                        P.act(a2[:, :ln], r[:, :ln], AF.Exp, scale=coef2[:, z, c:c + 1])
                        P.ts(a2[:, :ln], a2[:, :ln], -1.0, 1.0, ALU.mult, ALU.add)
                        P.act(a2[:, :ln], a2[:, :ln], AF.Sqrt)
                        P.tt(a2[:, :ln], a2[:, :ln], gi_[:, :ln], ALU.mult)
                        P.tt(a2[:, :ln], a2[:, :ln], xc[:, :ln], ALU.mult, e='pool')
                        hh_ = self.rot(st, 'rhh', [128, 512], F32, 3)
                        P.scan(rv(hh_[:, :ln]), rv(aa[:, :ln]), rv(a2[:, :ln]), hst[:, z, c:c + 1])
                        e_ = ln - 1 if z == 0 else 0
                        P.cp(hst[:, z, c:c + 1], hh_[:, e_:e_ + 1])
                        if z == 0:
                            P.dma(S['rhf'][c, :, t0:t0 + ln].k('%d_%d' % (t0, c)), hh_[:, :ln], q='pool')
                        else:
                            P.tt(hh_[:, :ln], hh_[:, :ln], hf[:, c, :ln], ALU.add)
                            P.tt(hh_[:, :ln], hh_[:, :ln], gg[:, c, :ln], ALU.mult, e='pool')
                            P.dma(S['rgT'][c, :, t0:t0 + ln].k('%d_%d' % (t0, c)), hh_[:, :ln], q='pool')
        P.barrier()

    def odd_oT(self, st, oT, t0, ln):
        P, S = self.P, self.scr
        a = self.rot(st, 'ooa', [128, 4, 512], F32, 2)
        r = self.rot(st, 'oor', [128, 4, 512], F32, 2)
        P.dma(a[:, :, :ln], S['aoT'][:, t0:t0 + ln].re("(c p) t -> p c t", p=128))
        P.dma(r[:, :, :ln], S['rgT'][:, :, t0:t0 + ln].re("c p t -> p c t"))
        P.cp(oT[:, 0:4, :ln], a[:, :, :ln], e='act')
        P.cp(oT[:, 4:8, :ln], r[:, :, :ln])


def host_maps(inp, nb, depth):
    f = lambda a: np.ascontiguousarray(np.asarray(a, dtype=np.float32))
    n_ev, n_od = (depth + 1) // 2, depth // 2
    sh = {}
    for k in ('w_mod', 'b_mod', 'norm1_w', 'norm2_w', 'moe_router', 'moe_w1', 'moe_w3', 'moe_w2'):
        sh[k] = f(inp[k][:depth])
    sh['final_norm_w'] = f(inp['final_norm_w']).reshape(1, D)
    sh['ev_w_in'] = f(inp['ev_w_in'][:n_ev])
    sh['ev_w_out'] = f(inp['ev_w_out'][:n_ev])
    sh['a_conv'] = f(np.asarray(inp['a_conv_w'])[:n_ev].reshape(n_ev, 4, 12, 128).transpose(0, 3, 2, 1))
    sh['a_log'] = f(np.asarray(inp['a_log'])[:n_ev].reshape(n_ev, 8))
    sh['a_dtb'] = f(np.asarray(inp['a_dt_bias'])[:n_ev].reshape(n_ev, 8))
    sh['a_norm_w'] = f(inp['a_norm_w'][:n_ev])
    sh['b_lbl'] = f(np.asarray(inp['b_lb_logits']).reshape(2, 8, 128).transpose(2, 0, 1))
    sh['b_norm_w'] = f(inp['b_norm_w'][:n_ev])
    if n_od:
        sh['od_w_in'] = f(inp['od_w_in'][:n_od])
        sh['od_w_out'] = f(inp['od_w_out'][:n_od])
        sh['c_sink'] = f(inp['c_sink'][:n_od])
        sh['d_conv'] = f(np.asarray(inp['d_conv_w'])[:n_od].reshape(n_od, 4, 4, 128).transpose(0, 3, 2, 1))
        sh['d_convb'] = f(np.asarray(inp['d_conv_b'])[:n_od].reshape(n_od, 4, 128).transpose(0, 2, 1))
        sh['d_wr'] = f(inp['d_w_r'][:n_od])
        sh['d_wi'] = f(inp['d_w_i'][:n_od])
        for kk, src in (('d_br', 'd_b_r'), ('d_bi', 'd_b_i'), ('d_lam', 'd_lambda')):
            sh[kk] = f(np.asarray(inp[src])[:n_od].reshape(n_od, 2, 4, 128).transpose(0, 3, 1, 2))
    maps = []
    cc = np.asarray(inp['c_ctx'], np.float32).reshape(8, 128).T
    for b in range(nb):
        m = dict(sh)
        m['xin'] = f(np.concatenate([np.asarray(inp['ctx'][b]), np.asarray(inp['x'][b])], axis=0))
        cb = np.asarray(inp['c'][b], np.float32).reshape(8, 128).T
        m['cvec'] = f(np.concatenate([cb, cc], axis=1))
        maps.append(m)
    return maps


_CACHE = {}


def build_net(N, LC, depth):
    key = (N, LC, depth)
    if key not in _CACHE:
        net = Net(N, LC, depth)
        for l in range(depth):
            net.layer(l)
        net.final_norm()
        net.P.barrier()
        _CACHE[key] = net
    return _CACHE[key]


def kernel(**inputs):
    x = np.asarray(inputs['x'])
    B, N, _ = x.shape
    LC = np.asarray(inputs['ctx']).shape[1]
    depth = np.asarray(inputs['w_mod']).shape[0]
    net = build_net(N, LC, depth)
    maps = host_maps(inputs, B, depth)
    res = run_bass_kernel_spmd(net.nc, maps, core_ids=list(range(B)))
    return np.stack([np.asarray(r['out'], dtype=np.float32) for r in res.results], axis=0)
```

```python
import numpy as np
import concourse.bass as bass
import concourse.mybir as mybir
from concourse.bass_utils import run_bass_kernel_spmd
from contextlib import ExitStack

F32 = mybir.dt.float32
BF16 = mybir.dt.bfloat16
I32 = mybir.dt.int32
AF = mybir.ActivationFunctionType
ALU = mybir.AluOpType

D = 1024
EPS = 1e-6
NEG = -1.0e30
ATTACH = True


class V:
    def __init__(self, key, ap):
        self.key = key
        self.ap = ap

    def __getitem__(self, k):
        return V(self.key, self.ap[k])

    def bc(self, shape):
        return V(self.key, self.ap.broadcast_to(list(shape)))

    def re(self, pat, **kw):
        return V(self.key, self.ap.rearrange(pat, **kw))

    def k(self, suffix):
        return V(self.key + ':' + str(suffix), self.ap)

    def pb(self, n):
        return V(self.key, self.ap.partition_broadcast(n))

    def bitcast(self, dt):
        return V(self.key, self.ap.bitcast(dt))


class Prog:
    def __init__(self, nc, es, n_dma_sems=24):
        self.nc = nc
        self.es = es
        self.eng = {'pe': nc.tensor, 'dve': nc.vector, 'act': nc.scalar, 'pool': nc.gpsimd, 'sp': nc.sync}
        self.sem = {k: es.enter_context(nc.semaphore('s_' + k)) for k in self.eng}
        self.cnt = {k: 0 for k in self.eng}
        self.dsem = [es.enter_context(nc.semaphore('d%d' % i)) for i in range(n_dma_sems)]
        self.dval = [0] * n_dma_sems
        self.dnext = 0
        self.seen = {k: {} for k in self.eng}
        self.last_w = {}
        self.reads = {}
        self.nins = 0

    def _need(self, e, deps, attach=False):
        todo = []
        for key, val in deps.items():
            if self.seen[e].get(key, 0) >= val:
                continue
            sem = self.sem[key[1]] if key[0] == 'e' else self.dsem[key[1]]
            todo.append((sem, val))
            self.seen[e][key] = val
        keep = None
        if attach and todo:
            keep = todo.pop()
        for sem, val in todo:
            self.eng[e].wait_ge(sem, val)
            self.nwait = getattr(self, 'nwait', 0) + 1
        return keep

    def _deps(self, R, W):
        deps = {}

        def add(k, v):
            if deps.get(k, 0) < v:
                deps[k] = v
        for b in R:
            if b in self.last_w:
                add(*self.last_w[b])
        for b in W:
            if b in self.last_w:
                add(*self.last_w[b])
            for k, v in self.reads.get(b, {}).items():
                add(k, v)
        return deps

    def _commit(self, R, W, key, val):
        for b in R:
            self.reads.setdefault(b, {})[key] = val
        for b in W:
            self.last_w[b] = (key, val)
            self.reads[b] = {}

    def op(self, e, R, W, fn):
        R = [v.key for v in R if isinstance(v, V)]
        W = [v.key for v in W]
        deps = self._deps(R, W)
        if e == 'pe':
            deps.pop(('e', 'pe'), None)
        keep = self._need(e, deps, attach=ATTACH)
        ins = fn()
        if keep is not None:
            ins._wait_ge(keep[0], keep[1])
        self.cnt[e] += 1
        ins.then_inc(self.sem[e], 1)
        self._commit(R, W, ('e', e), self.cnt[e])
        self.nins += 1
        return ins

    def dma(self, out, in_, q='sp'):
        R, W = [in_.key], [out.key]
        deps = self._deps(R, W)
        i = self.dnext
        self.dnext = (self.dnext + 1) % len(self.dsem)
        if self.dval[i] > 0:
            deps[('d', i)] = max(deps.get(('d', i), 0), self.dval[i])
        keep = self._need(q, deps, attach=ATTACH)
        ins = self.eng[q].dma_start(out=out.ap, in_=in_.ap)
        if keep is not None:
            ins._wait_ge(keep[0], keep[1])
        self.dval[i] += 16
        ins.then_inc(self.dsem[i], 16)
        self._commit(R, W, ('d', i), self.dval[i])
        self.nins += 1

    def barrier(self):
        deps = {('e', k): c for k, c in self.cnt.items() if c > 0}
        for i, v in enumerate(self.dval):
            if v > 0:
                deps[('d', i)] = v
        for e in self.eng:
            d = dict(deps)
            d.pop(('e', e), None)
            self._need(e, d)
        self.last_w = {}
        self.reads = {}

    def sb(self, st, name, shape, dt=F32):
        self.uid = getattr(self, 'uid', 0) + 1
        name = '%s_u%d' % (name, self.uid)
        t = st.enter_context(self.nc.sbuf_tensor(name, list(shape), dt))
        return V(name, t[:])

    def dram(self, name, shape, dt=F32, kind="Internal"):
        t = self.nc.dram_tensor(name, list(shape), dt, kind=kind)
        return V(name, t.ap())

    def act(self, out, in_, func, bias=None, scale=None, accum=None, eng=None):
        kw = {}
        if bias is not None:
            kw['bias'] = bias.ap if isinstance(bias, V) else bias
        if scale is not None:
            kw['scale'] = scale.ap if isinstance(scale, V) else scale
        W = [out]
        if accum is not None:
            kw['accum_out'] = accum.ap
            W.append(accum)
        return self.op('act', [in_, bias, scale], W,
                       lambda: self.nc.scalar.activation(out=out.ap, in_=in_.ap, func=func, **kw))

    def tt(self, out, a, b, op, e='dve'):
        en = self.eng[e]
        return self.op(e, [a, b], [out], lambda: en.tensor_tensor(out=out.ap, in0=a.ap, in1=b.ap, op=op))

    def ts(self, out, a, s1, s2=None, op0=ALU.mult, op1=None, accum=None, e='dve'):
        en = self.eng[e]
        kw = {}
        if op1 is not None:
            kw['op1'] = op1
        W = [out]
        if accum is not None:
            kw['accum_out'] = accum.ap
            W.append(accum)
        g = lambda s: s.ap if isinstance(s, V) else s
        return self.op(e, [a, s1, s2], W,
                       lambda: en.tensor_scalar(out=out.ap, in0=a.ap, scalar1=g(s1), scalar2=g(s2), op0=op0, **kw))

    def stt(self, out, a, s, b, op0, op1):
        g = s.ap if isinstance(s, V) else s
        return self.op('dve', [a, s, b], [out],
                       lambda: self.nc.vector.scalar_tensor_tensor(out=out.ap, in0=a.ap, scalar=g, in1=b.ap, op0=op0, op1=op1))

    def cp(self, out, in_, e='dve'):
        if e == 'act':
            return self.op('act', [in_], [out], lambda: self.nc.scalar.activation(out=out.ap, in_=in_.ap, func=AF.Copy))
        en = self.eng[e]
        return self.op(e, [in_], [out], lambda: en.tensor_copy(out=out.ap, in_=in_.ap))

    def recip(self, out, in_):
        return self.op('dve', [in_], [out], lambda: self.nc.vector.reciprocal(out=out.ap, in_=in_.ap))

    def memset(self, out, val, e='pool'):
        en = self.eng[e]
        return self.op(e, [], [out], lambda: en.memset(out.ap, val))

    def mm(self, out, lhsT, rhs, start=True, stop=True):
        return self.op('pe', [lhsT, rhs], [out],
                       lambda: self.nc.tensor.matmul(out.ap, lhsT=lhsT.ap, rhs=rhs.ap, start=start, stop=stop))

    def tr(self, out, in_, ident):
        return self.op('pe', [in_, ident], [out],
                       lambda: self.nc.tensor.transpose(out=out.ap, in_=in_.ap, identity=ident.ap))

    def scan(self, out, d0, d1, init, op0=ALU.mult, op1=ALU.add):
        g = init.ap if isinstance(init, V) else init
        return self.op('dve', [d0, d1, init], [out],
                       lambda: self.nc.vector.tensor_tensor_scan(out=out.ap, data0=d0.ap, data1=d1.ap, initial=g, op0=op0, op1=op1))

    def reduce(self, out, in_, op, axis=mybir.AxisListType.X):
        return self.op('dve', [in_], [out], lambda: self.nc.vector.tensor_reduce(out=out.ap, in_=in_.ap, axis=axis, op=op))

    def aselect(self, out, in_, pattern, cmp, fill, base, cm):
        return self.op('pool', [in_], [out],
                       lambda: self.nc.gpsimd.affine_select(out=out.ap, in_=in_.ap, pattern=pattern, compare_op=cmp,
                                                            fill=fill, base=base, channel_multiplier=cm))

    def iota(self, out, pattern, base, cm):
        return self.op('pool', [], [out],
                       lambda: self.nc.gpsimd.iota(out.ap, pattern=pattern, base=base, channel_multiplier=cm))


EV_FM = [('qkv', c, c * 128) for c in range(12)] + [('qb', c, 2064 + c * 128) for c in range(4)] + \
        [('fb', c, 3088 + c * 128) for c in range(8)]
EV_TM = [('ga', 1536, 512), ('ab', 2048, 16), ('ib', 2576, 512), ('gb', 4112, 512)]


class Net:
    def __init__(self, N, LC, depth, dbg=()):
        self.N, self.LC, self.NT, self.depth = N, LC, N + LC, depth
        self.dbg = set(dbg)
        self.nc = nc = bass.Bass("TRN2", target_bir_lowering=False)
        self.es = ExitStack()
        self.P = P = Prog(nc, self.es)
        NT = self.NT
        n_ev, n_od = (depth + 1) // 2, depth // 2
        I = lambda name, shape: P.dram(name, shape, F32, kind="ExternalInput")
        self.xin = I("xin", [NT, D])
        self.cvec = I("cvec", [128, 16])
        self.w_mod = I("w_mod", [depth, D, 6 * D])
        self.b_mod = I("b_mod", [depth, 6 * D])
        self.norm1_w = I("norm1_w", [depth, D])
        self.norm2_w = I("norm2_w", [depth, D])
        self.final_norm_w = I("final_norm_w", [1, D])
        self.ev_w_in = I("ev_w_in", [n_ev, D, 4624])
        self.ev_w_out = I("ev_w_out", [n_ev, D, D])
        self.a_conv = I("a_conv", [n_ev, 128, 12, 4])
        self.a_log = I("a_log", [n_ev, 8])
        self.a_dtb = I("a_dtb", [n_ev, 8])
        self.a_norm_w = I("a_norm_w", [n_ev, 128])
        self.b_lbl = I("b_lbl", [128, 2, 8])
        self.b_norm_w = I("b_norm_w", [n_ev, 128])
        if n_od:
            self.od_w_in = I("od_w_in", [n_od, D, 1792])
            self.od_w_out = I("od_w_out", [n_od, D, D])
            self.c_sink = I("c_sink", [n_od, 8])
            self.d_conv = I("d_conv", [n_od, 128, 4, 4])
            self.d_convb = I("d_convb", [n_od, 128, 4])
            self.d_wr = I("d_wr", [n_od, 2, 8, 64, 64])
            self.d_wi = I("d_wi", [n_od, 2, 8, 64, 64])
            self.d_br = I("d_br", [n_od, 128, 2, 4])
            self.d_bi = I("d_bi", [n_od, 128, 2, 4])
            self.d_lam = I("d_lam", [n_od, 128, 2, 4])
        self.moe_router = I("moe_router", [depth, D, 16])
        self.moe_w1 = I("moe_w1", [depth, 16, D, D])
        self.moe_w3 = I("moe_w3", [depth, 16, D, D])
        self.moe_w2 = I("moe_w2", [depth, 16, D, D])
        self.out = P.dram("out", [N, D], F32, kind="ExternalOutput")
        self.scr = {}
        self.xs = self.S("xs", [NT, D])
        self.vecs = self.S("vecs", [depth, 2, 6, D])
        self.groups = []
        for seg, (a, b) in enumerate([(0, LC), (LC, NT)]):
            t = a
            while t < b:
                ln = min(512, b - t)
                self.groups.append((seg, t, ln))
                t += ln
        self.segs = [(0, LC), (LC, NT)]
        self.ps = []
        for i in range(8):
            t = self.es.enter_context(nc.psum_tensor("ps%d" % i, [128, 512], F32))
            self.ps.append(V("ps%d" % i, t[:]))
        self.psi = 0
        self.consts()

    def S(self, name, shape, dt=F32):
        kind = "ExternalOutput" if name in self.dbg else "Internal"
        v = self.P.dram(name, shape, dt, kind=kind)
        self.scr[name] = v
        return v

    def psn(self):
        p = self.ps[self.psi]
        self.psi = (self.psi + 1) % 8
        return p

    def consts(self):
        P, es = self.P, self.es
        self.ident = P.sb(es, "ident", [128, 128])
        P.memset(self.ident, 1.0)
        P.aselect(self.ident, self.ident, [[-1, 128]], ALU.is_equal, 0.0, 0, 1)
        self.identb = P.sb(es, "identb", [128, 128], BF16)
        P.cp(self.identb, self.ident)
        self.ones = P.sb(es, "ones", [128, 128])
        P.memset(self.ones, 1.0)
        self.epsc = P.sb(es, "epsc", [128, 1])
        P.memset(self.epsc, EPS)
        self.onec = P.sb(es, "onec", [128, 1])
        P.memset(self.onec, 1.0)
        cv = P.sb(es, "cv", [128, 16])
        P.dma(cv, self.cvec)
        self.csil = P.sb(es, "csil", [128, 16])
        P.act(self.csil, cv, AF.Silu)
        lbl = P.sb(es, "lbl", [128, 2, 8])
        P.dma(lbl, self.b_lbl)
        self.lb = P.sb(es, "lb", [128, 2, 8])
        self.oml = P.sb(es, "oml", [128, 2, 8])
        P.memset(self.lb, 0.0)
        P.tt(self.lb[:, 1, :], lbl[:, 1, :], lbl[:, 0, :], ALU.subtract)
        P.act(self.lb[:, 1, :], self.lb[:, 1, :], AF.Sigmoid)
        P.ts(self.oml, self.lb, -1.0, 1.0, ALU.mult, ALU.add)

    def vec(self, l, seg, i):
        r = 0 if seg == 1 else 1
        return self.vecs[l, r:r + 1, i, :].pb(128)

    def mod_prep(self, l):
        P = self.P
        with ExitStack() as st:
            wm = [P.sb(st, 'wm%d' % i, [128, 3072]) for i in range(2)]
            modsb = P.sb(st, 'modsb', [2, 6144])
            bm = P.sb(st, 'bm', [2, 6144])
            P.dma(bm, self.b_mod[l:l + 1, :].pb(2))
            cnt = 0
            for half in range(2):
                banks = [self.psn() for _ in range(6)]
                for k in range(8):
                    w = wm[cnt % 2]
                    cnt += 1
                    P.dma(w, self.w_mod[l, k * 128:(k + 1) * 128, half * 3072:(half + 1) * 3072])
                    for j in range(6):
                        P.mm(banks[j][0:2, :], self.csil[:, k:16:8], w[:, j * 512:(j + 1) * 512],
                             start=(k == 0), stop=(k == 7))
                for j in range(6):
                    c0 = half * 3072 + j * 512
                    P.tt(modsb[:, c0:c0 + 512], banks[j][0:2, :], bm[:, c0:c0 + 512], ALU.add)
            n1 = P.sb(st, 'n1', [2, D])
            n2 = P.sb(st, 'n2', [2, D])
            P.dma(n1, self.norm1_w[l:l + 1, :].pb(2))
            P.dma(n2, self.norm2_w[l:l + 1, :].pb(2))
            vv = P.sb(st, 'vv', [2, 6, D])
            m = lambda i: modsb[:, i * D:(i + 1) * D]
            P.stt(vv[:, 0, :], m(1), 1.0, n1, ALU.add, ALU.mult)
            P.cp(vv[:, 1, :], m(0))
            P.cp(vv[:, 2, :], m(2))
            P.stt(vv[:, 3, :], m(4), 1.0, n2, ALU.add, ALU.mult)
            P.cp(vv[:, 4, :], m(3))
            P.cp(vv[:, 5, :], m(5))
            P.dma(self.vecs[l], vv, q='pool')
        P.barrier()

    def norm_mod_T(self, st, src, t0, ln, A, B, hT, tag, hf_out=None):
        P = self.P
        for ti in range(ln // 128):
            xt = self.rot(st, tag + 'xt', [128, D], F32, 2)
            P.dma(xt, src[t0 + ti * 128:t0 + (ti + 1) * 128, :])
            junk = self.rot(st, tag + 'junk', [128, D], F32, 2)
            ss = self.rot(st, tag + 'ss', [128, 1], F32, 2)
            P.act(junk, xt, AF.Square, accum=ss)
            P.act(ss, ss, AF.Sqrt, scale=1.0 / D, bias=self.epsc)
            P.recip(ss, ss)
            P.stt(junk, xt, ss, A, ALU.mult, ALU.mult)
            if hf_out is not None:
                hf = hf_out(ti)
                P.tt(hf, junk, B, ALU.add)
                src_h = hf
            hb = self.rot(st, tag + 'hb', [128, D], BF16, 2)
            P.tt(hb, junk, B, ALU.add)
            pt = self.psn()
            ptb = pt.bitcast(BF16)
            for k in range(8):
                P.tr(ptb[:, k * 128:(k + 1) * 128], hb[:, k * 128:(k + 1) * 128], self.identb)
            P.cp(hT[:, :, ti * 128:(ti + 1) * 128], ptb.re("p (k t) -> p k t", k=8), e='act')

    def rot(self, st, name, shape, dt, n):
        d = st.__dict__.setdefault('_rot', {})
        if name not in d:
            d[name] = [[self.P.sb(st, '%s_%d' % (name, i), shape, dt) for i in range(n)], 0]
        bufs, i = d[name]
        d[name][1] = i + 1
        return bufs[i % n]

    def load_w_bf16(self, dst, src, ncols, q='pool'):
        K = dst.ap.shape[1]
        for k in range(K):
            c = 0
            while c < ncols:
                w = min(2048, ncols - c)
                self.P.dma(dst[:, k, c:c + w], src[k * 128:(k + 1) * 128, c:c + w], q=q)
                c += w

    def stageA_even(self, l):
        P, j = self.P, l // 2
        NT = self.NT
        S = self.scr
        if 'qkvT' not in S:
            self.S('qkvT', [12, 128, NT]); self.S('hqT', [4, 128, NT]); self.S('lfT', [8, 128, NT])
            self.S('hkT', [8, 128, NT]); self.S('tm_ga', [NT, 512]); self.S('tm_gb', [NT, 512])
            self.S('tm_ib', [NT, 512], BF16); self.S('tm_g', [NT, 8]); self.S('tm_bt', [NT, 8])
        src = self.xin if l == 0 else self.xs
        with ExitStack() as st:
            w = P.sb(st, 'wA', [128, 8, 4624], BF16)
            self.load_w_bf16(w, self.ev_w_in[j], 4624)
            AB = {}
            for seg in (0, 1):
                AB[seg] = (P.sb(st, 'A1_%d' % seg, [128, D]), P.sb(st, 'B1_%d' % seg, [128, D]))
                P.dma(AB[seg][0], self.vec(l, seg, 0))
                P.dma(AB[seg][1], self.vec(l, seg, 1))
            al = P.sb(st, 'alog', [128, 8]); dtb = P.sb(st, 'dtb', [128, 8])
            P.dma(al, self.a_log[j:j + 1, :].pb(128))
            P.dma(dtb, self.a_dtb[j:j + 1, :].pb(128))
            negA = P.sb(st, 'negA', [128, 8])
            P.act(negA, al, AF.Exp)
            P.ts(negA, negA, -1.0, None, ALU.mult)
            for gi, (seg, t0, ln) in enumerate(self.groups):
                hT = self.rot(st, 'hT', [128, 8, 512], BF16, 2)
                self.norm_mod_T(st, src, t0, ln, AB[seg][0], AB[seg][1], hT, 'A')
                for kind, c, col0 in EV_FM:
                    ps = self.psn()
                    for k in range(8):
                        P.mm(ps[:, :ln], w[:, k, col0:col0 + 128], hT[:, k, :ln], start=(k == 0), stop=(k == 7))
                    o = self.rot(st, 'Ao', [128, 512], F32, 4)
                    if kind == 'qkv':
                        P.cp(o[:, :ln], ps[:, :ln])
                        P.dma(S['qkvT'][c, :, t0:t0 + ln].k(gi), o[:, :ln], q='pool')
                    elif kind == 'qb':
                        P.act(o[:, :ln], ps[:, :ln], AF.Silu)
                        P.dma(S['hqT'][c, :, t0:t0 + ln].k(gi), o[:, :ln], q='pool')
                    else:
                        o2 = self.rot(st, 'Ao2', [128, 512], F32, 2)
                        P.act(o[:, :ln], ps[:, :ln], AF.Sigmoid)
                        P.ts(o[:, :ln], o[:, :ln], self.oml[:, j, c:c + 1], self.lb[:, j, c:c + 1], ALU.mult, ALU.add)
                        P.act(o2[:, :ln], o[:, :ln], AF.Ln)
                        P.dma(S['lfT'][c, :, t0:t0 + ln].k(gi), o2[:, :ln], q='pool')
                        o3 = self.rot(st, 'Ao3', [128, 512], F32, 2)
                        P.ts(o3[:, :ln], o[:, :ln], -1.0, 1.0, ALU.mult, ALU.add)
                        P.dma(S['hkT'][c, :, t0:t0 + ln].k(gi), o3[:, :ln], q='pool')
                for ti in range(ln // 128):
                    ta = t0 + ti * 128
                    for kind, col0, ncol in EV_TM:
                        ps = self.psn()
                        for k in range(8):
                            P.mm(ps[:, :ncol], hT[:, k, ti * 128:(ti + 1) * 128], w[:, k, col0:col0 + ncol],
                                 start=(k == 0), stop=(k == 7))
                        o = self.rot(st, 'Ao', [128, 512], F32, 4)
                        if kind in ('ga', 'gb'):
                            P.act(o, ps, AF.Silu)
                            P.dma(S['tm_' + kind][ta:ta + 128, :].k(gi), o, q='pool')
                        elif kind == 'ib':
                            ob = self.rot(st, 'Aob', [128, 512], BF16, 2)
                            P.cp(ob, ps)
                            P.dma(S['tm_ib'][ta:ta + 128, :].k(gi), ob, q='pool')
                        else:
                            P.tt(o[:, 0:8], ps[:, 0:8], dtb, ALU.add)
                            P.act(o[:, 0:8], o[:, 0:8], AF.Exp)
                            P.act(o[:, 0:8], o[:, 0:8], AF.Ln, bias=self.onec)
                            P.tt(o[:, 0:8], o[:, 0:8], negA, ALU.mult)
                            P.act(o[:, 8:16], ps[:, 8:16], AF.Sigmoid)
                            P.dma(S['tm_g'][ta:ta + 128, :].k(gi), o[:, 0:8], q='pool')
                            P.dma(S['tm_bt'][ta:ta + 128, :].k(gi), o[:, 8:16], q='pool')
        P.barrier()

    def stageB0_delta(self, l):
        P, j = self.P, l // 2
        NT = self.NT
        S = self.scr
        if 'dqT' not in S:
            self.S('dqT', [4, 128, NT], BF16); self.S('dkT', [4, 128, NT], BF16)
            self.S('dk_tm', [NT, 512], BF16); self.S('dv_tm', [NT, 512], BF16)
        with ExitStack() as st:
            cw = P.sb(st, 'cw', [128, 12, 4])
            P.dma(cw, self.a_conv[j])
            for gi, (seg, t0, ln) in enumerate(self.groups):
                a, b = self.segs[seg]
                cin = self.rot(st, 'cin', [128, 12, 515], F32, 2)
                lo, hi = max(a, t0 - 1), min(b, t0 + ln + 2)
                if lo > t0 - 1 or hi < t0 + ln + 2:
                    P.memset(cin, 0.0)
                P.dma(cin[:, :, lo - (t0 - 1):hi - (t0 - 1)], S['qkvT'][:, :, lo:hi].re("c p t -> p c t"))
                for c in range(12):
                    acc = self.rot(st, 'acc', [128, 512], F32, 3)
                    P.ts(acc[:, :ln], cin[:, c, 0:ln], cw[:, c, 0:1], None, ALU.mult)
                    for tap in range(1, 4):
                        P.stt(acc[:, :ln], cin[:, c, tap:tap + ln], cw[:, c, tap:tap + 1], acc[:, :ln], ALU.mult, ALU.add)
                    sl = self.rot(st, 'sl', [128, 512], F32, 3)
                    P.act(sl[:, :ln], acc[:, :ln], AF.Silu)
                    if c < 8:
                        sq = self.rot(st, 'sq', [128, 512], F32, 2)
                        P.act(sq[:, :ln], sl[:, :ln], AF.Square)
                        ps = self.psn()
                        P.mm(ps[:, :ln], self.ones, sq[:, :ln])
                        P.act(sq[:, :ln], ps[:, :ln], AF.Sqrt, bias=self.epsc)
                        P.recip(sq[:, :ln], sq[:, :ln])
                        qn = self.rot(st, 'qn', [128, 512], BF16, 3)
                        P.stt(qn[:, :ln], sl[:, :ln], (128 ** -0.5) if c < 4 else 1.0, sq[:, :ln], ALU.mult, ALU.mult)
                        if c < 4:
                            P.dma(S['dqT'][c, :, t0:t0 + ln].k(gi), qn[:, :ln], q='pool')
                        else:
                            P.dma(S['dkT'][c - 4, :, t0:t0 + ln].k(gi), qn[:, :ln], q='pool')
                        srcT = qn
                    else:
                        srcT = self.rot(st, 'slb', [128, 512], BF16, 3)
                        P.cp(srcT[:, :ln], sl[:, :ln], e='pool')
                    if c >= 4:
                        dst = S['dk_tm'] if c < 8 else S['dv_tm']
                        h = c % 4
                        ps = self.psn().bitcast(BF16)
                        for ti in range(ln // 128):
                            P.tr(ps[:, ti * 128:(ti + 1) * 128], srcT[:, ti * 128:(ti + 1) * 128], self.identb)
                        tmo = self.rot(st, 'tmo', [128, 512], BF16, 3)
                        P.cp(tmo[:, :ln], ps[:, :ln], e='act')
                        P.dma(dst[t0:t0 + ln, h * 128:(h + 1) * 128].re("(n p) d -> p n d", p=128).k(gi),
                              tmo[:, :ln].re("p (n d) -> p n d", d=128), q='pool')
        P.barrier()

    def chunk_consts(self):
        if hasattr(self, 'tri'):
            return
        P, es = self.P, self.es
        self.tri, self.maskL = [], []
        for z in range(2):
            sgn = 1 if z == 0 else -1
            t = P.sb(es, "tri%d" % z, [64, 64])
            P.memset(t, 1.0)
            P.aselect(t, t, [[sgn, 64]], ALU.is_ge, 0.0, 0, -sgn)
            self.tri.append(t)
            m = P.sb(es, "maskL%d" % z, [64, 4, 64])
            P.memset(m, 0.0)
            P.aselect(m, m, [[0, 4], [-sgn, 64]], ALU.is_gt, NEG, 0, sgn)
            self.maskL.append(m)
        self.ident4 = P.sb(es, "ident4", [64, 4, 64])
        P.memset(self.ident4, 1.0)
        P.aselect(self.ident4, self.ident4, [[0, 4], [-1, 64]], ALU.is_equal, 0.0, 0, 1)

    def chunk_order(self, z):
        out = []
        for a, b in self.segs:
            cs = list(range(a, b, 64))
            out += cs if z == 0 else cs[::-1]
        return out

    def stageB_delta(self, l):
        P = self.P
        S = self.scr
        NT = self.NT
        self.chunk_consts()
        if 'do_0' not in S:
            self.S('do_0', [NT, 512]); self.S('do_1', [NT, 512])
        with ExitStack() as st:
            St = [P.sb(st, 'dS%d' % z, [128, 4, 128]) for z in range(2)]
            self.Sb = [P.sb(st, 'dSb%d' % z, [128, 4, 128], BF16) for z in range(2)]
            for z in range(2):
                P.memset(St[z], 0.0)
                P.memset(self.Sb[z], 0.0)
            if 'ho_0' not in S:
                self.S('ho_0', [NT, 512]); self.S('ho_1', [NT, 512])
            self.hmask, self.hrm = [], []
            for z in range(2):
                sgn = 1 if z == 0 else -1
                m = P.sb(st, "hmask%d" % z, [64, 4, 64])
                P.memset(m, 1.0)
                P.aselect(m, m, [[0, 4], [sgn, 64]], ALU.is_ge, 0.0, 0, -sgn)
                self.hmask.append(m)
                rm = P.sb(st, "hrm%d" % z, [128, 4, 64])
                P.memset(rm, 1.0)
                e0 = 0 if z == 0 else 63
                P.memset(rm[:, :, e0:e0 + 1], 0.0)
                self.hrm.append(rm)
            Sh = [P.sb(st, 'hS%d' % z, [128, 4, 128]) for z in range(2)]
            self.Shb = [P.sb(st, 'hSb%d' % z, [128, 4, 128], BF16) for z in range(2)]
            for z in range(2):
                P.memset(Sh[z], 0.0)
                P.memset(self.Shb[z], 0.0)
            orders = [self.chunk_order(0), self.chunk_order(1)]
            nch = len(orders[0])
            DEP = 1
            hold = {}
            for step in range(nch + DEP):
                for z in range(2):
                    if step < nch:
                        hold[('d', z, step)] = self.delta_b1(st, z, orders[z][step])
                        hold[('h', z, step)] = self.hgrn_b1(st, z, orders[z][step])
                    if step >= DEP:
                        self.delta_b2(st, z, orders[z][step - DEP], St[z], hold.pop(('d', z, step - DEP)))
                        self.hgrn_b2(st, z, orders[z][step - DEP], Sh[z], hold.pop(('h', z, step - DEP)))
        P.barrier()

    def delta_b1(self, st, z, t0):
        P, S = self.P, self.scr
        R = lambda nm, shape, n=2: self.rot(st, 'd%d%s' % (z, nm), shape, F32, n)
        RB = lambda nm, shape, n=2: self.rot(st, 'd%d%s' % (z, nm), shape, BF16, n)
        g4 = R('g4', [64, 4]); bt4 = R('bt4', [64, 4])
        kT = RB('kT', [128, 4, 64]); qT = RB('qT', [128, 4, 64])
        ktm = RB('ktm', [64, 4, 128]); vtm = RB('vtm', [64, 4, 128])
        P.dma(g4, S['tm_g'][t0:t0 + 64, z * 4:(z + 1) * 4])
        P.dma(bt4, S['tm_bt'][t0:t0 + 64, z * 4:(z + 1) * 4])
        P.dma(kT, S['dkT'][:, :, t0:t0 + 64].re("h p t -> p h t"))
        P.dma(qT, S['dqT'][:, :, t0:t0 + 64].re("h p t -> p h t"))
        P.dma(ktm, S['dk_tm'][t0:t0 + 64, :].re("t (h d) -> t h d", h=4))
        P.dma(vtm, S['dv_tm'][t0:t0 + 64, :].re("t (h d) -> t h d", h=4))
        tri, maskL, I4 = self.tri[z], self.maskL[z], self.ident4
        gb = R('gb', [64, 4, 128])
        P.cp(gb, g4[:, :, None].bc([64, 4, 128]), e='pool')
        pc = self.psn()
        P.mm(pc[0:64, 0:4], tri, g4)
        cumc = R('cumc', [64, 4])
        P.cp(cumc, pc[0:64, 0:4])
        pt = self.psn()
        P.mm(pt[:, 0:4], self.ones[0:64, :], g4)
        prow = self.psn()
        for h in range(4):
            P.mm(prow[:, h * 64:(h + 1) * 64], gb[:, h, :], tri)
        prow3 = prow[:, 0:256].re("p (h f) -> p h f", h=4)
        X = R('X', [64, 4, 64])
        P.tt(X, prow3[0:64], cumc[:, :, None].bc([64, 4, 64]), ALU.subtract)
        E = R('E', [64, 4, 64])
        P.stt(E, X, -1.0, maskL, ALU.mult, ALU.add)
        P.act(E, E, AF.Exp)
        ecr = R('ecr', [128, 4, 64])
        P.act(ecr, prow3, AF.Exp)
        qdT = RB('qdT', [128, 4, 64], 3)
        P.tt(qdT, qT, ecr, ALU.mult, e='pool')
        glast = R('glast', [128, 4], 3)
        P.act(glast, pt[:, 0:4], AF.Exp)
        ekd = R('ekd', [64, 4])
        P.tt(ekd, pt[0:64, 0:4], cumc, ALU.subtract)
        P.act(ekd, ekd, AF.Exp)
        kdec = RB('kdec', [64, 4, 128], 3)
        P.tt(kdec, ktm, ekd[:, :, None].bc([64, 4, 128]), ALU.mult, e='pool')
        ec = R('ec', [64, 4])
        P.act(ec, cumc, AF.Exp)
        P.tt(ec, ec, bt4, ALU.mult, e='pool')
        Ru = RB('Ru', [64, 4, 128]); Rw = RB('Rw', [64, 4, 128])
        P.tt(Ru, vtm, bt4[:, :, None].bc([64, 4, 128]), ALU.mult, e='pool')
        P.tt(Rw, ktm, ec[:, :, None].bc([64, 4, 128]), ALU.mult, e='pool')
        pk = self.psn(); pq = self.psn()
        for h in range(4):
            P.mm(pk[0:64, h * 64:(h + 1) * 64], kT[:, h, :], kT[:, h, :])
        for h in range(4):
            P.mm(pq[0:64, h * 64:(h + 1) * 64], qT[:, h, :], kT[:, h, :])
        v3 = lambda p: p[0:64, 0:256].re("p (h f) -> p h f", h=4)
        Pm = RB('Pm', [64, 4, 64]); Qm = RB('Qm', [64, 4, 64])
        T1 = R('T1', [64, 4, 64])
        P.tt(T1, v3(pk), bt4[:, :, None].bc([64, 4, 64]), ALU.mult)
        P.tt(Pm, T1, E, ALU.mult)
        QK = RB('QK', [64, 4, 64])
        P.tt(E, E, I4, ALU.add, e='pool')
        P.tt(QK, v3(pq), E, ALU.mult)
        pn = self.psn().bitcast(BF16); pqt = self.psn().bitcast(BF16)
        for h in range(4):
            P.tr(pn[0:64, h * 64:(h + 1) * 64], Pm[:, h, :], self.identb[0:64, 0:64])
        for h in range(4):
            P.tr(pqt[0:64, h * 64:(h + 1) * 64], QK[:, h, :], self.identb[0:64, 0:64])
        P.cp(Qm, v3(pn), e='act')
        QKT = RB('QKT', [64, 4, 64], 3)
        P.cp(QKT, v3(pqt), e='act')
        Z = RB('Z', [64, 4, 64]); Y = RB('Y', [64, 4, 64])
        P.tt(Z, I4, Pm, ALU.subtract, e='pool')
        P.tt(Y, I4, Qm, ALU.subtract, e='pool')
        for lev in range(1, 6):
            last = lev == 5
            Pn = RB('Pm', [64, 4, 64]); Qn = RB('Qm', [64, 4, 64])
            if not last:
                p1 = self.psn()
                for h in range(4):
                    P.mm(p1[0:64, h * 64:(h + 1) * 64], Qm[:, h, :], Pm[:, h, :])
            p2 = self.psn()
            for h in range(4):
                P.mm(p2[0:64, h * 64:(h + 1) * 64], Pm[:, h, :], Qm[:, h, :])
            if not last:
                P.cp(Pn, v3(p1), e='act')
            P.cp(Qn, v3(p2))
            p3 = self.psn()
            for h in range(4):
                P.mm(p3[0:64, h * 64:(h + 1) * 64], Z[:, h, :], Qn[:, h, :])
            if not last:
                p4 = self.psn()
                for h in range(4):
                    P.mm(p4[0:64, h * 64:(h + 1) * 64], Y[:, h, :], Pn[:, h, :])
            Yn = RB('Y', [64, 4, 64])
            P.tt(Yn, Y, v3(p3), ALU.add)
            if not last:
                Zn = RB('Z', [64, 4, 64])
                P.tt(Zn, Z, v3(p4), ALU.add)
                Z = Zn
            Y, Pm, Qm = Yn, Pn, Qn
        pu = self.psn(); pw = self.psn()
        for h in range(4):
            P.mm(pu[0:64, h * 128:(h + 1) * 128], Y[:, h, :], Ru[:, h, :])
        for h in range(4):
            P.mm(pw[:, h * 64:(h + 1) * 64], Rw[:, h, :], Y[:, h, :])
        u = R('u', [64, 4, 128], 3); wT = RB('wT', [128, 4, 64], 3)
        P.cp(u, pu[0:64, :].re("p (h d) -> p h d", h=4))
        P.cp(wT, pw[:, 0:256].re("p (h f) -> p h f", h=4), e='act')
        return dict(u=u, wT=wT, QKT=QKT, qdT=qdT, kdec=kdec, glast=glast)

    def delta_b2(self, st, z, t0, St, b):
        P, S = self.P, self.scr
        R = lambda nm, shape, n=2: self.rot(st, 'd%d%s' % (z, nm), shape, F32, n)
        Sb = self.Sb[z]
        pw = self.psn()
        for h in range(4):
            P.mm(pw[0:64, h * 128:(h + 1) * 128], b['wT'][:, h, :], Sb[:, h, :])
        vn = self.rot(st, 'd%dvn' % z, [64, 4, 128], BF16, 2)
        P.tt(vn, b['u'], pw[0:64, :].re("p (h d) -> p h d", h=4), ALU.subtract)
        po = self.psn()
        for h in range(4):
            P.mm(po[0:64, h * 128:(h + 1) * 128], b['qdT'][:, h, :], Sb[:, h, :], start=True, stop=False)
            P.mm(po[0:64, h * 128:(h + 1) * 128], b['QKT'][:, h, :], vn[:, h, :], start=False, stop=True)
        pS = self.psn()
        for h in range(4):
            P.mm(pS[:, h * 128:(h + 1) * 128], b['kdec'][:, h, :], vn[:, h, :])
        o = R('o', [64, 512])
        P.cp(o, po[0:64, :], e='act')
        P.dma(S['do_%d' % z][t0:t0 + 64, :].k(t0), o, q='pool')
        P.tt(St, St, b['glast'][:, :, None].bc([128, 4, 128]), ALU.mult)
        P.tt(St, St, pS.re("p (h d) -> p h d", h=4), ALU.add)
        P.cp(Sb, St, e='act')

    def stageB_hgrn(self, l):
        P = self.P
        S = self.scr
        NT = self.NT
        self.chunk_consts()
        if 'ho_0' not in S:
            self.S('ho_0', [NT, 512]); self.S('ho_1', [NT, 512])
        with ExitStack() as st:
            self.hmask, self.hrm = [], []
            for z in range(2):
                sgn = 1 if z == 0 else -1
                m = P.sb(st, "hmask%d" % z, [64, 4, 64])
                P.memset(m, 1.0)
                P.aselect(m, m, [[0, 4], [sgn, 64]], ALU.is_ge, 0.0, 0, -sgn)
                self.hmask.append(m)
                rm = P.sb(st, "hrm%d" % z, [128, 4, 64])
                P.memset(rm, 1.0)
                e0 = 0 if z == 0 else 63
                P.memset(rm[:, :, e0:e0 + 1], 0.0)
                self.hrm.append(rm)
            St = [P.sb(st, 'hS%d' % z, [128, 4, 128]) for z in range(2)]
            self.Sb = [P.sb(st, 'hSb%d' % z, [128, 4, 128], BF16) for z in range(2)]
            for z in range(2):
                P.memset(St[z], 0.0)
                P.memset(self.Sb[z], 0.0)
            orders = [self.chunk_order(0), self.chunk_order(1)]
            nch = len(orders[0])
            DEP = 1
            hold = {}
            for step in range(nch + DEP):
                for z in range(2):
                    if step < nch:
                        hold[(z, step)] = self.hgrn_b1(st, z, orders[z][step])
                    if step >= DEP:
                        self.hgrn_b2(st, z, orders[z][step - DEP], St[z], hold.pop((z, step - DEP)))
        P.barrier()

    def hgrn_b1(self, st, z, t0):
        P, S = self.P, self.scr
        R = lambda nm, shape, n=2: self.rot(st, 'h%d%s' % (z, nm), shape, F32, n)
        lf = R('lf', [128, 4, 64]); hk = R('hk', [128, 4, 64]); q = R('q', [128, 4, 64])
        RB = lambda nm, shape, n=2: self.rot(st, 'h%d%s' % (z, nm), shape, BF16, n)
        vtm = RB('vtm', [64, 4, 128], 3)
        P.dma(lf, S['lfT'][z * 4:(z + 1) * 4, :, t0:t0 + 64].re("h p t -> p h t"))
        P.dma(hk, S['hkT'][z * 4:(z + 1) * 4, :, t0:t0 + 64].re("h p t -> p h t"))
        P.dma(q, S['hqT'][:, :, t0:t0 + 64].re("h p t -> p h t"))
        P.dma(vtm, S['tm_ib'][t0:t0 + 64, :].re("t (h d) -> t h d", h=4))
        b = R('b', [128, 4, 64])
        fl = lambda v: v.re("p h t -> p (h t)")
        rv = (lambda v: v) if z == 0 else (lambda v: v[:, ::-1])
        P.scan(rv(fl(b)), rv(fl(self.hrm[z])), rv(fl(lf)), 0.0)
        last = 63 if z == 0 else 0
        db = R('db', [128, 4, 64])
        P.tt(db, b, b[:, :, 32:33].bc([128, 4, 64]), ALU.subtract)
        eq = R('eq', [128, 4, 64]); ek = R('ek', [128, 4, 64])
        P.act(eq, db, AF.Exp)
        P.act(ek, db, AF.Exp, scale=-1.0)
        eqb = RB('eqb', [128, 4, 64]); ekb = RB('ekb', [128, 4, 64])
        P.tt(eqb, eq, q, ALU.mult)
        P.tt(ekb, ek, hk, ALU.mult, e='pool')
        pa = self.psn()
        for h in range(4):
            P.mm(pa[0:64, h * 64:(h + 1) * 64], ekb[:, h, :], eqb[:, h, :])
        attT = RB('attT', [64, 4, 64], 3)
        att0 = R('att0', [64, 4, 64])
        P.ts(att0, pa[0:64, 0:256].re("p (h f) -> p h f", h=4), 1.0e30, -1.0e30, ALU.min, ALU.max)
        P.tt(attT, att0, self.hmask[z], ALU.mult)
        eb = R('eb', [128, 4, 64])
        P.act(eb, b, AF.Exp)
        qeT = RB('qeT', [128, 4, 64], 3)
        P.tt(qeT, eb, q, ALU.mult)
        kd = R('kd', [128, 4, 64])
        P.tt(kd, b, b[:, :, last:last + 1].bc([128, 4, 64]), ALU.subtract)
        P.act(kd, kd, AF.Exp, scale=-1.0)
        kdb = RB('kdb', [128, 4, 64])
        P.tt(kdb, kd, hk, ALU.mult, e='pool')
        pk = self.psn().bitcast(BF16)
        for h in range(4):
            P.tr(pk[0:64, h * 128:(h + 1) * 128], kdb[:, h, :], self.identb)
        kdtm = RB('kdtm', [64, 4, 128], 3)
        P.cp(kdtm, pk[0:64, 0:512].re("p (h d) -> p h d", h=4), e='act')
        ebl = R('ebl', [128, 4, 1], 3)
        P.act(ebl, b[:, :, last:last + 1], AF.Exp)
        return dict(attT=attT, qeT=qeT, kdtm=kdtm, ebl=ebl, vtm=vtm)

    def hgrn_b2(self, st, z, t0, St, b):
        P, S = self.P, self.scr
        R = lambda nm, shape, n=2: self.rot(st, 'h%d%s' % (z, nm), shape, F32, n)
        po = self.psn()
        for h in range(4):
            P.mm(po[0:64, h * 128:(h + 1) * 128], b['attT'][:, h, :], b['vtm'][:, h, :], start=True, stop=False)
            P.mm(po[0:64, h * 128:(h + 1) * 128], b['qeT'][:, h, :], self.Shb[z][:, h, :], start=False, stop=True)
        pS = self.psn()
        for h in range(4):
            P.mm(pS[:, h * 128:(h + 1) * 128], b['kdtm'][:, h, :], b['vtm'][:, h, :])
        o = R('o', [64, 512])
        P.cp(o, po[0:64, :], e='act')
        P.dma(S['ho_%d' % z][t0:t0 + 64, :].k(t0), o, q='pool')
        P.tt(St, St, b['ebl'].bc([128, 4, 128]), ALU.mult)
        P.tt(St, St, pS.re("p (h d) -> p h d", h=4), ALU.add)
        P.cp(self.Shb[z], St, e='act')

    def stageC(self, l, even):
        P, S = self.P, self.scr
        NT, j = self.NT, l // 2
        last = l == self.depth - 1
        if 'h2tm' not in S:
            self.S('h2tm', [NT, D], BF16); self.S('affT', [16, NT]); self.S('posT', [16, NT])
        src = self.xin if l == 0 else self.xs
        with ExitStack() as st:
            wo = P.sb(st, 'wo', [128, 8, D], BF16)
            self.load_w_bf16(wo, (self.ev_w_out if even else self.od_w_out)[j], D)
            rt = P.sb(st, 'rt', [128, 8, 16])
            P.dma(rt, self.moe_router[l].re("(k p) e -> p k e", p=128))
            vt = {}
            for seg in (0, 1):
                vt[seg] = [P.sb(st, 'C%d_%d' % (i, seg), [128, D]) for i in (2, 3, 4)]
                for t, i in zip(vt[seg], (2, 3, 4)):
                    P.dma(t, self.vec(l, seg, i))
            if even:
                nwa = P.sb(st, 'nwa', [128, 128]); nwb = P.sb(st, 'nwb', [128, 128])
                P.dma(nwa, self.a_norm_w[j:j + 1, :].pb(128))
                P.dma(nwb, self.b_norm_w[j:j + 1, :].pb(128))
            for gi, (seg, t0, ln) in enumerate(self.groups):
                if last and seg == 0:
                    continue
                oT = self.rot(st, 'oT', [128, 8, 512], BF16, 2)
                G1, A2, B2 = vt[seg]
                nt = ln // 128
                if even:
                    for ti in range(nt):
                        ta = t0 + ti * 128
                        oc = self.rot(st, 'oc', [128, D], BF16, 2)
                        for mi, (pre, gname, nw) in enumerate((('do', 'tm_ga', nwa), ('ho', 'tm_gb', nwb))):
                            o0 = self.rot(st, 'o0', [128, 4, 128], F32, 2)
                            o1 = self.rot(st, 'o1', [128, 4, 128], F32, 2)
                            gt = self.rot(st, 'gt', [128, 4, 128], F32, 2)
                            P.dma(o0, S[pre + '_0'][ta:ta + 128, :].re("t (h d) -> t h d", h=4))
                            P.dma(o1, S[pre + '_1'][ta:ta + 128, :].re("t (h d) -> t h d", h=4))
                            P.dma(gt, S[gname][ta:ta + 128, :].re("t (h d) -> t h d", h=4))
                            P.tt(o0, o0, o1, ALU.add)
                            P.tt(o1, o0, o0, ALU.mult, e='pool')
                            ss = self.rot(st, 'ss4', [128, 4], F32, 2)
                            P.reduce(ss, o1, ALU.add)
                            P.act(ss, ss, AF.Sqrt, scale=1.0 / 128, bias=self.epsc)
                            P.recip(ss, ss)
                            P.tt(o0, o0, ss[:, :, None].bc([128, 4, 128]), ALU.mult)
                            P.tt(o0, o0, nw[:, None, :].bc([128, 4, 128]), ALU.mult, e='pool')
                            P.tt(oc[:, mi * 512:(mi + 1) * 512].re("t (h d) -> t h d", h=4), o0, gt, ALU.mult)
                        pt = self.psn()
                        ptb = pt.bitcast(BF16)
                        for k in range(8):
                            P.tr(ptb[:, k * 128:(k + 1) * 128], oc[:, k * 128:(k + 1) * 128], self.identb)
                        P.cp(oT[:, :, ti * 128:(ti + 1) * 128], ptb.re("p (k t) -> p k t", k=8), e='act')
                else:
                    self.odd_oT(st, oT, t0, ln)
                affs = self.rot(st, 'affs', [16, 512], F32, 2)
                for ti in range(nt):
                    ta = t0 + ti * 128
                    xt = self.rot(st, 'Cxt', [128, D], F32, 2)
                    P.dma(xt, src[ta:ta + 128, :])
                    xn = self.rot(st, 'Cxn', [128, D], F32, 2)
                    for half in range(2):
                        ps = self.psn()
                        for k in range(8):
                            P.mm(ps, oT[:, k, ti * 128:(ti + 1) * 128], wo[:, k, half * 512:(half + 1) * 512],
                                 start=(k == 0), stop=(k == 7))
                        hs = slice(half * 512, (half + 1) * 512)
                        P.tt(xn[:, hs], ps, G1[:, hs], ALU.mult)
                        P.tt(xn[:, hs], xn[:, hs], xt[:, hs], ALU.add, e='pool')
                    P.dma(self.xs[ta:ta + 128, :].k(gi), xn, q='pool')
                    junk = self.rot(st, 'Cjunk', [128, D], F32, 2)
                    ss = self.rot(st, 'Css', [128, 1], F32, 2)
                    P.act(junk, xn, AF.Square, accum=ss)
                    P.act(ss, ss, AF.Sqrt, scale=1.0 / D, bias=self.epsc)
                    P.recip(ss, ss)
                    h2 = self.rot(st, 'Ch2', [128, D], F32, 2)
                    P.stt(h2, xn, ss, A2, ALU.mult, ALU.mult)
                    P.tt(h2, h2, B2, ALU.add, e='pool')
                    pa = self.psn(); pb = self.psn()
                    for k in range(8):
                        pp = pa if k < 4 else pb
                        P.tr(pp[:, (k % 4) * 128:(k % 4 + 1) * 128], h2[:, k * 128:(k + 1) * 128], self.ident)
                    h2T = self.rot(st, 'Ch2T', [128, 8, 128], F32, 2)
                    P.cp(h2T[:, 0:4, :], pa.re("p (k t) -> p k t", k=4), e='act')
                    P.cp(h2T[:, 4:8, :], pb.re("p (k t) -> p k t", k=4))
                    h2b = self.rot(st, 'Ch2b', [128, D], BF16, 2)
                    P.cp(h2b, h2, e='pool')
                    P.dma(S['h2tm'][ta:ta + 128, :].k(gi), h2b, q='pool')
                    pl = self.psn()
                    for k in range(8):
                        P.mm(pl[:, 0:16], h2T[:, k, :], rt[:, k, :], start=(k == 0), stop=(k == 7))
                    mx = self.rot(st, 'Cmx', [128, 1], F32, 2)
                    P.reduce(mx, pl[:, 0:16], ALU.max)
                    P.ts(mx, mx, -1.0, None, ALU.mult)
                    ex = self.rot(st, 'Cex', [128, 16], F32, 2)
                    sm = self.rot(st, 'Csm', [128, 1], F32, 2)
                    P.act(ex, pl[:, 0:16], AF.Exp, bias=mx, accum=sm)
                    P.recip(sm, sm)
                    P.ts(ex, ex, sm, None, ALU.mult)
                    pT = self.psn()
                    P.tr(pT[0:16, 0:128], ex, self.ident)
                    P.cp(affs[:, ti * 128:(ti + 1) * 128], pT[0:16, 0:128], e='act')
                P.dma(S['affT'][:, t0:t0 + ln].k(gi), affs[:, :ln], q='pool')
        P.barrier()

    def stage_topk(self, l):
        P, S = self.P, self.scr
        last = l == self.depth - 1
        with ExitStack() as st:
            for seg, (a, b) in enumerate(self.segs):
                if last and seg == 0:
                    continue
                n = b - a
                kk = max(1, 2 * n // 16)
                af = P.sb(st, 'af%d' % seg, [16, n])
                jk = P.sb(st, 'jk%d' % seg, [16, n])
                P.dma(af, S['affT'][:, a:b])
                lo = P.sb(st, 'lo%d' % seg, [16, 1]); hi = P.sb(st, 'hi%d' % seg, [16, 1])
                mid = P.sb(st, 'mid%d' % seg, [16, 1]); cnt = P.sb(st, 'cnt%d' % seg, [16, 1])
                fl = P.sb(st, 'fl%d' % seg, [16, 1]); d1 = P.sb(st, 'd1%d' % seg, [16, 1]); d2 = P.sb(st, 'd2%d' % seg, [16, 1])
                P.memset(lo, 0.0, e='dve'); P.memset(hi, 2.0, e='dve')
                for it in range(36):
                    P.ts(mid, lo, hi, 0.5, ALU.add, ALU.mult)
                    P.ts(jk, af, mid, None, ALU.is_ge, ALU.add, accum=cnt)
                    P.ts(fl, cnt, float(kk), None, ALU.is_ge)
                    P.tt(d1, mid, lo, ALU.subtract)
                    P.tt(d2, hi, mid, ALU.subtract)
                    P.stt(lo, d1, fl, lo, ALU.mult, ALU.add)
                    P.stt(hi, d2, fl, mid, ALU.mult, ALU.add)
                on = P.sb(st, 'on%d' % seg, [16, n])
                P.memset(on, 1.0)
                P.ts(jk, af, lo, None, ALU.is_ge)
                P.scan(af, on, jk, 0.0)
                P.tt(af, af, jk, ALU.mult)
                P.ts(af, af, -1.0, None, ALU.add)
                P.dma(S['posT'][:, a:b], af, q='pool')
        P.barrier()

    def stage_moe_dense(self, l):
        P, S = self.P, self.scr
        last = l == self.depth - 1
        if 'wbf' not in S:
            self.S('wbf', [16, 3, D, D], BF16)
        for e in range(16):
            for i, wsrc in enumerate((self.moe_w1, self.moe_w3, self.moe_w2)):
                P.dma(S['wbf'][e, i].k('%d_%d' % (e, i)), wsrc[l, e], q='pool')
        with ExitStack() as st:
            sel = P.sb(st, 'sel', [16, 16, 128])
            P.memset(sel, 1.0)
            P.aselect(sel, sel, [[-1, 16], [0, 128]], ALU.is_equal, 0.0, 0, 1)
            G2 = {}
            for seg in (0, 1):
                G2[seg] = P.sb(st, 'G2_%d' % seg, [128, D])
                P.dma(G2[seg], self.vec(l, seg, 5))
            acc = P.sb(st, 'macc', [128, 8, D])
            hb = P.sb(st, 'mhb', [128, 8, 1024], BF16)
            gT = P.sb(st, 'mgT', [16, 1024])
            hid = P.sb(st, 'mhid', [128, 8, 512], BF16)
            blocks = []
            for seg, (a, b) in enumerate(self.segs):
                if last and seg == 0:
                    continue
                t = a
                while t < b:
                    bl = min(1024, b - t)
                    blocks.append((seg, t, bl))
                    t += bl
            for bi, (seg, t0, bl) in enumerate(blocks):
                P.memset(acc, 0.0)
                P.dma(hb[:, :, :bl], S['h2T'][:, :, t0:t0 + bl].re("k p t -> p k t"))
                P.dma(gT[:, :bl], S['gateT'][:, t0:t0 + bl])
                for e in range(16):
                    ws = []
                    for i in range(3):
                        wt = self.rot(st, 'mw%d' % i, [128, 8, D], BF16, 2)
                        P.dma(wt, S['wbf'][e, i].re("(k p) f -> p k f", p=128).k('%d_%d' % (e, i)))
                        ws.append(wt)
                    w1, w3, w2 = ws
                    for s0 in range(0, bl, 512):
                        ln = min(512, bl - s0)
                        pg = self.psn()
                        P.mm(pg[:, :ln], sel[:, e, :], gT[:, s0:s0 + ln])
                        gbc = self.rot(st, 'mgbc', [128, 512], F32, 2)
                        P.cp(gbc[:, :ln], pg[:, :ln], e='act')
                        for fc in range(8):
                            p1 = self.psn(); p3 = self.psn()
                            for k in range(8):
                                P.mm(p1[:, :ln], w1[:, k, fc * 128:(fc + 1) * 128], hb[:, k, s0:s0 + ln], start=(k == 0), stop=(k == 7))
                            for k in range(8):
                                P.mm(p3[:, :ln], w3[:, k, fc * 128:(fc + 1) * 128], hb[:, k, s0:s0 + ln], start=(k == 0), stop=(k == 7))
                            sg = self.rot(st, 'msg', [128, 512], F32, 2)
                            P.act(sg[:, :ln], p1[:, :ln], AF.Silu)
                            P.tt(sg[:, :ln], sg[:, :ln], p3[:, :ln], ALU.mult)
                            P.tt(hid[:, fc, :ln], sg[:, :ln], gbc[:, :ln], ALU.mult, e='pool')
                        for ti in range(ln // 128):
                            at = (s0 + ti * 128) // 128
                            for half in range(2):
                                po = self.psn()
                                for fc in range(8):
                                    P.mm(po, hid[:, fc, ti * 128:(ti + 1) * 128], w2[:, fc, half * 512:(half + 1) * 512],
                                         start=(fc == 0), stop=(fc == 7))
                                hs = slice(half * 512, (half + 1) * 512)
                                P.tt(acc[:, at, hs], acc[:, at, hs], po, ALU.add)
                for ti in range(bl // 128):
                    ta = t0 + ti * 128
                    xt = self.rot(st, 'mxt', [128, D], F32, 2)
                    P.dma(xt, self.xs[ta:ta + 128, :].k('m%d' % bi))
                    P.tt(acc[:, ti, :], acc[:, ti, :], G2[seg], ALU.mult)
                    P.tt(xt, xt, acc[:, ti, :], ALU.add, e='pool')
                    P.dma(self.xs[ta:ta + 128, :].k('m%d' % bi), xt, q='pool')
        P.barrier()

    def idma(self, fn, R, W):
        P = self.P
        R = [v.key for v in R]; W = [v.key for v in W]
        deps = P._deps(R, W)
        i = P.dnext
        P.dnext = (P.dnext + 1) % len(P.dsem)
        if P.dval[i] > 0:
            deps[('d', i)] = max(deps.get(('d', i), 0), P.dval[i])
        keep = P._need('pool', deps, attach=ATTACH)
        ins = fn()
        if keep is not None:
            ins._wait_ge(keep[0], keep[1])
        P.dval[i] += 16
        ins.then_inc(P.dsem[i], 16)
        P._commit(R, W, ('d', i), P.dval[i])
        P.nins += 1

    def stage_moe_gather(self, l):
        P, S, nc = self.P, self.scr, self.nc
        last = l == self.depth - 1
        NT = self.NT
        nch = NT // 128
        with ExitStack() as st:
            G2 = {}
            for seg in (0, 1):
                G2[seg] = P.sb(st, 'G2_%d' % seg, [128, D])
                P.dma(G2[seg], self.vec(l, seg, 5))
            ptm = P.sb(st, 'ptm', [128, nch, 16])
            tg = P.sb(st, 'tg', [128, nch, 16, 4], BF16)
            atm = P.sb(st, 'atm', [128, nch, 16])
            for c0 in range(0, NT, 512):
                ln = min(512, NT - c0)
                for nm, dst in (('posT', ptm), ('affT', atm)):
                    t = self.rot(st, 'mld', [16, 512], F32, 2)
                    P.dma(t[:, :ln], S[nm][:, c0:c0 + ln])
                    ps = self.psn()
                    for i in range(ln // 128):
                        P.tr(ps[:, i * 16:(i + 1) * 16], t[:, i * 128:(i + 1) * 128], self.ident[0:16, 0:16])
                    P.cp(dst[:, c0 // 128:c0 // 128 + ln // 128, :], ps[:, 0:(ln // 128) * 16].re("p (c e) -> p c e", e=16))
            ti_ = P.sb(st, 'mti', [128, nch], I32)
            P.iota(ti_, [[0, nch]], 0, 1)
            P.cp(tg[:, :, :, 0], ti_[:, :, None].bc([128, nch, 16]))
            P.iota(ti_, [[128, nch]], 0, 0)
            P.cp(tg[:, :, :, 1], ti_[:, :, None].bc([128, nch, 16]))
            P.cp(tg[:, :, :, 2], atm)
            ahi = P.sb(st, 'ahi', [128, nch, 16])
            P.cp(ahi, tg[:, :, :, 2])
            P.tt(ahi, atm, ahi, ALU.subtract)
            P.cp(tg[:, :, :, 3], ahi)
            capmax = max(1, 2 * self.N // 16)
            ii = P.sb(st, 'mii', [128, capmax], I32)
            P.iota(ii, [[1, capmax]], 0, 0)
            iof = P.sb(st, 'miof', [128, capmax])
            P.cp(iof, ii)
            xT = P.sb(st, 'mxT', [128, 8, capmax], BF16)
            hid = P.sb(st, 'mhid', [128, 8, capmax], BF16)
            rows = P.sb(st, 'mrows', [4, capmax])
            def loadw(e):
                ws = []
                for i, wsrc in enumerate((self.moe_w1, self.moe_w3, self.moe_w2)):
                    wt = self.rot(st, 'mw%d' % i, [128, 8, D], BF16, 2)
                    P.dma(wt, wsrc[l, e].re("(k p) f -> p k f", p=128), q='pool')
                    ws.append(wt)
                return ws
            nxt = loadw(0)
            for e in range(16):
                w1, w3, w2 = nxt
                if e < 15:
                    nxt = loadw(e + 1)
                for seg, (a, b) in enumerate(self.segs):
                    if last and seg == 0:
                        continue
                    n = b - a
                    cap = max(1, 2 * n // 16)
                    halves = [(h0, min(512, cap - h0)) for h0 in range(0, cap, 512)]
                    tiles = [(s0, min(128, cap - s0)) for s0 in range(0, cap, 128)]
                    pr = [self.psn() for _ in halves]
                    c_lo, c_hi = a // 128, b // 128
                    for c in range(c_lo, c_hi):
                        oh = self.rot(st, 'moh', [128, capmax], BF16, 3)
                        P.ts(oh[:, :cap], iof[:, :cap], ptm[:, c, e:e + 1], None, ALU.is_equal)
                        for hi_, (h0, hl) in enumerate(halves):
                            P.mm(pr[hi_][0:4, :hl], tg[:, c, e, :], oh[:, h0:h0 + hl], start=(c == c_lo), stop=(c == c_hi - 1))
                    for hi_, (h0, hl) in enumerate(halves):
                        P.cp(rows[:, h0:h0 + hl], pr[hi_][0:4, :hl], e='act')
                    idxs, gcols = [], []
                    for (s0, ns) in tiles:
                        pc = self.psn()
                        P.mm(pc[0:ns, 0:4], rows[:, s0:s0 + ns], self.ident[0:4, 0:4])
                        cf = self.rot(st, 'mcf', [128, 4], F32, 10)
                        P.cp(cf[0:ns], pc[0:ns, 0:4])
                        ix = self.rot(st, 'mix', [128, 1], I32, 10)
                        P.tt(cf[0:ns, 0:1], cf[0:ns, 0:1], cf[0:ns, 1:2], ALU.add)
                        P.cp(ix[0:ns], cf[0:ns, 0:1])
                        P.tt(cf[0:ns, 2:3], cf[0:ns, 2:3], cf[0:ns, 3:4], ALU.add)
                        idxs.append(ix); gcols.append(cf)
                    for ti, (s0, ns) in enumerate(tiles):
                        xg = self.rot(st, 'mxg', [128, D], BF16, 3)
                        ix = idxs[ti]
                        self.idma(lambda: nc.gpsimd.indirect_dma_start(
                            out=xg.ap[0:ns], out_offset=None, in_=S['h2tm'].ap,
                            in_offset=bass.IndirectOffsetOnAxis(ap=ix.ap[0:ns, 0:1], axis=0)), [S['h2tm'], ix], [xg])
                        pt = self.psn()
                        ptb = pt.bitcast(BF16)
                        for k in range(8):
                            P.tr(ptb[:, k * 128:k * 128 + ns], xg[0:ns, k * 128:(k + 1) * 128], self.identb[0:ns, 0:ns])
                        P.cp(xT[:, :, s0:s0 + ns], ptb.re("p (k t) -> p k t", k=8)[:, :, 0:ns], e='act')
                    for fc in range(8):
                        for (h0, hl) in halves:
                            p1 = self.psn(); p3 = self.psn()
                            for k in range(8):
                                P.mm(p1[:, :hl], w1[:, k, fc * 128:(fc + 1) * 128], xT[:, k, h0:h0 + hl], start=(k == 0), stop=(k == 7))
                            for k in range(8):
                                P.mm(p3[:, :hl], w3[:, k, fc * 128:(fc + 1) * 128], xT[:, k, h0:h0 + hl], start=(k == 0), stop=(k == 7))
                            sg = self.rot(st, 'msg', [128, 512], F32, 2)
                            P.act(sg[:, :hl], p1[:, :hl], AF.Silu)
                            P.tt(hid[:, fc, h0:h0 + hl], sg[:, :hl], p3[:, :hl], ALU.mult)
                    for ti, (s0, ns) in enumerate(tiles):
                        y = self.rot(st, 'my', [128, D], F32, 2)
                        for half in range(2):
                            po = self.psn()
                            for fc in range(8):
                                P.mm(po[0:ns, :], hid[:, fc, s0:s0 + ns], w2[:, fc, half * 512:(half + 1) * 512],
                                     start=(fc == 0), stop=(fc == 7))
                            hs = slice(half * 512, (half + 1) * 512)
                            P.stt(y[0:ns, hs], po[0:ns, :], gcols[ti][0:ns, 2:3], G2[seg][0:ns, hs], ALU.mult, ALU.mult)
                        ix = idxs[ti]
                        self.idma(lambda: nc.gpsimd.indirect_dma_start(
                            out=self.xs.ap, out_offset=bass.IndirectOffsetOnAxis(ap=ix.ap[0:ns, 0:1], axis=0),
                            in_=y.ap[0:ns], in_offset=None, compute_op=ALU.add), [y, ix], [self.xs])
        P.barrier()

    def final_norm(self):
        P = self.P
        with ExitStack() as st:
            fw = P.sb(st, 'fw', [128, D])
            P.dma(fw, self.final_norm_w.pb(128))
            for ti in range(self.N // 128):
                ta = self.LC + ti * 128
                xt = self.rot(st, 'Fxt', [128, D], F32, 3)
                P.dma(xt, self.xs[ta:ta + 128, :])
                junk = self.rot(st, 'Fjunk', [128, D], F32, 2)
                ss = self.rot(st, 'Fss', [128, 1], F32, 2)
                P.act(junk, xt, AF.Square, accum=ss)
                P.act(ss, ss, AF.Sqrt, scale=1.0 / D, bias=self.epsc)
                P.recip(ss, ss)
                P.stt(junk, xt, ss, fw, ALU.mult, ALU.mult)
                P.dma(self.out[ti * 128:(ti + 1) * 128, :].k(ti), junk, q='pool')
        P.barrier()

    def layer(self, l):
        self.mod_prep(l)
        if l % 2 == 0:
            self.stageA_even(l)
            self.stageB0_delta(l)
            self.stageB_delta(l)
            self.stageC(l, True)
        else:
            self.stageA_odd(l)
            self.stageB_attn(l)
            self.stageB_rglru(l)
            self.stageC(l, False)
        self.stage_topk(l)
        self.stage_moe_gather(l)

    def rope_tables(self):
        P, S = self.P, self.scr
        if 'ropeC' in S:
            return
        N = self.N
        self.S('ropeC', [128, N]); self.S('ropeS', [128, N])
        import math
        with ExitStack() as st:
            pi_ = P.sb(st, 'r_pi', [128, 1], I32)
            P.iota(pi_, [[0, 1]], 0, 1)
            t1 = P.sb(st, 'r_t1', [128, 1], I32); t2 = P.sb(st, 'r_t2', [128, 1], I32)
            P.ts(t1, pi_, 4, 4, ALU.arith_shift_right, ALU.logical_shift_left)
            P.tt(t1, pi_, t1, ALU.subtract)
            f16 = P.sb(st, 'r_f16', [128, 1])
            P.cp(f16, t1)
            inv = P.sb(st, 'r_inv', [128, 1])
            P.act(inv, f16, AF.Exp, scale=-math.log(10000.0) / 16.0)
            P.ts(inv, inv, 1.0 / (2 * math.pi), None, ALU.mult)
            P.ts(t2, pi_, 5, 1, ALU.arith_shift_right, ALU.bitwise_and)
            selc = P.sb(st, 'r_sel', [128, 1])
            P.cp(selc, t2)
            CW = min(N, 2048)
            ri = P.sb(st, 'r_ri', [128, CW], I32); ci = P.sb(st, 'r_ci', [128, CW], I32)
            rf = P.sb(st, 'r_rf', [128, CW]); cf = P.sb(st, 'r_cf', [128, CW])
            ys = {nm: P.sb(st, 'r_y' + nm, [128, CW]) for nm in ('ropeS', 'ropeC')}
            yi = P.sb(st, 'r_yi', [128, CW], I32)
            tm = P.sb(st, 'r_tm', [128, CW])
            for c0 in range(0, N, CW):
                P.iota(ri, [[1, CW // 64], [0, 64]], c0 // 64, 0)
                P.iota(ci, [[0, CW // 64], [1, 64]], 0, 0)
                P.cp(rf, ri); P.cp(cf, ci)
                P.tt(cf, cf, rf, ALU.subtract)
                P.stt(rf, cf, selc, rf, ALU.mult, ALU.add)
                P.ts(rf, rf, inv, None, ALU.mult)
                for name, off in (('ropeS', 0.0), ('ropeC', 0.25)):
                    y = ys[name]
                    P.ts(y, rf, off, None, ALU.add)
                    P.cp(yi, y)
                    P.cp(tm, yi)
                    P.tt(y, y, tm, ALU.subtract)
                    P.ts(tm, y, 0.5, None, ALU.is_gt)
                    P.tt(y, y, tm, ALU.subtract)
                    P.ts(tm, y, -0.5, None, ALU.is_lt)
                    P.tt(y, y, tm, ALU.add)
                    P.act(y, y, AF.Sin, scale=2 * math.pi)
                    P.dma(S[name][:, c0:c0 + CW].k(c0), y, q='pool')
        P.barrier()

    def stageA_odd(self, l):
        P, j = self.P, l // 2
        NT, N, LC = self.NT, self.N, self.LC
        S = self.scr
        self.rope_tables()
        if 'aqT' not in S:
            self.S('aqT', [4, 128, NT], BF16); self.S('akT', [128, NT], BF16); self.S('av_tm', [NT, 128], BF16)
            self.S('xdT', [4, 128, NT]); self.S('ggT', [4, 128, NT]); self.S('m0', [128, 1])
        src = self.xs
        with ExitStack() as st:
            w = P.sb(st, 'wAo', [128, 8, 1792], BF16)
            for k in range(8):
                rows = self.od_w_in[j, k * 128:(k + 1) * 128, :]
                for g in range(2):
                    P.dma(w[:, k, 0:512].re("p (i g d) -> p g i d", i=4, g=2)[:, g], rows[:, g * 256:(g + 1) * 256].re("p (i d) -> p i d", i=4), q='pool')
                P.dma(w[:, k, 512:1792], rows[:, 512:1792], q='pool')
            AB = {}
            for seg in (0, 1):
                AB[seg] = (P.sb(st, 'A1_%d' % seg, [128, D]), P.sb(st, 'B1_%d' % seg, [128, D]))
                P.dma(AB[seg][0], self.vec(l, seg, 0))
                P.dma(AB[seg][1], self.vec(l, seg, 1))
            piT = P.sb(st, 'piT', [128, 128])
            P.memset(piT, 0.0)
            pv = piT.re("p (b h j) -> p b h j", b=4, h=2)
            m1 = P.sb(st, 'pm1', [128, 4, 16]); p1 = P.sb(st, 'pp1', [128, 4, 16])
            P.memset(m1, -1.0); P.memset(p1, 1.0)
            P.aselect(pv[:, :, 0, :], m1, [[-32, 4], [-1, 16]], ALU.is_equal, 0.0, -16, 1)
            P.aselect(pv[:, :, 1, :], p1, [[-32, 4], [-1, 16]], ALU.is_equal, 0.0, 0, 1)
            qmax = P.sb(st, 'qmax', [128, 512]); kmax = P.sb(st, 'kmax', [128, 512])
            P.memset(qmax, 0.0); P.memset(kmax, 0.0)
            for gi, (seg, t0, ln) in enumerate(self.groups):
                hT = self.rot(st, 'hT', [128, 8, 512], BF16, 2)
                self.norm_mod_T(st, src, t0, ln, AB[seg][0], AB[seg][1], hT, 'A')
                if seg == 1:
                    rc = self.rot(st, 'rC', [128, 512], F32, 2); rs = self.rot(st, 'rS', [128, 512], F32, 2)
                    P.dma(rc[:, :ln], S['ropeC'][:, t0 - LC:t0 - LC + ln])
                    P.dma(rs[:, :ln], S['ropeS'][:, t0 - LC:t0 - LC + ln])
                for c in range(5):
                    ps = self.psn()
                    col0 = c * 128
                    for k in range(8):
                        P.mm(ps[:, :ln], w[:, k, col0:col0 + 128], hT[:, k, :ln], start=(k == 0), stop=(k == 7))
                    x0 = self.rot(st, 'ox0', [128, 512], F32, 3)
                    if c < 4:
                        P.act(x0[:, :ln], ps[:, :ln], AF.Copy, scale=0.125)
                    else:
                        P.cp(x0[:, :ln], ps[:, :ln])
                    if seg == 1:
                        pr = self.psn()
                        P.mm(pr[:, :ln], piT, x0[:, :ln])
                        x1 = self.rot(st, 'ox1', [128, 512], F32, 2)
                        P.tt(x1[:, :ln], pr[:, :ln], rs[:, :ln], ALU.mult)
                        P.tt(x0[:, :ln], x0[:, :ln], rc[:, :ln], ALU.mult, e='pool')
                        P.tt(x0[:, :ln], x0[:, :ln], x1[:, :ln], ALU.add)
                    dst = S['aqT'][c, :, t0:t0 + ln] if c < 4 else S['akT'][:, t0:t0 + ln]
                    xb = self.rot(st, 'oxb', [128, 512], BF16, 3)
                    P.cp(xb[:, :ln], x0[:, :ln], e='pool')
                    P.dma(dst.k(gi), xb[:, :ln], q='pool')
                    sq = self.rot(st, 'osq', [128, 512], F32, 2)
                    P.act(sq[:, :ln], x0[:, :ln], AF.Square)
                    pn = self.psn()
                    P.mm(pn[:, :ln], self.ones, sq[:, :ln])
                    mx = qmax if c < 4 else kmax
                    P.tt(mx[:, :ln], mx[:, :ln], pn[:, :ln], ALU.max)
                for c in range(8):
                    ps = self.psn()
                    col0 = 768 + c * 128
                    for k in range(8):
                        P.mm(ps[:, :ln], w[:, k, col0:col0 + 128], hT[:, k, :ln], start=(k == 0), stop=(k == 7))
                    o = self.rot(st, 'Ao', [128, 512], F32, 4)
                    if c < 4:
                        P.cp(o[:, :ln], ps[:, :ln])
                        P.dma(S['xdT'][c, :, t0:t0 + ln].k(gi), o[:, :ln], q='pool')
                    else:
                        t = self.rot(st, 'Ao2', [128, 512], F32, 2)
                        P.cp(o[:, :ln], ps[:, :ln], e='act')
                        P.tt(t[:, :ln], o[:, :ln], o[:, :ln], ALU.mult)
                        P.ts(t[:, :ln], t[:, :ln], 0.044715, 1.0, ALU.mult, ALU.add)
                        P.tt(t[:, :ln], t[:, :ln], o[:, :ln], ALU.mult)
                        P.act(t[:, :ln], t[:, :ln], AF.Sigmoid, scale=1.5957691216057308)
                        P.tt(o[:, :ln], o[:, :ln], t[:, :ln], ALU.mult)
                        P.dma(S['ggT'][c - 4, :, t0:t0 + ln].k(gi), o[:, :ln], q='pool')
                for ti in range(ln // 128):
                    ta = t0 + ti * 128
                    ps = self.psn()
                    for k in range(8):
                        P.mm(ps[:, 0:128], hT[:, k, ti * 128:(ti + 1) * 128], w[:, k, 640:768], start=(k == 0), stop=(k == 7))
                    o = self.rot(st, 'Av', [128, 128], BF16, 3)
                    P.cp(o, ps[:, 0:128])
                    P.dma(S['av_tm'][ta:ta + 128, :].k(gi), o, q='pool')
            qm = P.sb(st, 'qm', [128, 1]); km = P.sb(st, 'km', [128, 1])
            P.reduce(qm, qmax, ALU.max); P.reduce(km, kmax, ALU.max)
            P.tt(qm, qm, km, ALU.mult)
            P.act(qm, qm, AF.Sqrt)
            sk = P.sb(st, 'sk', [128, 8])
            P.dma(sk, self.c_sink[j:j + 1, :].pb(128))
            P.reduce(km, sk, ALU.max)
            P.tt(qm, qm, km, ALU.max)
            P.ts(qm, qm, -1.0, None, ALU.mult)
            P.dma(S['m0'], qm, q='pool')
        P.barrier()

    def stageB_attn(self, l):
        P, j = self.P, l // 2
        NT, N, LC = self.NT, self.N, self.LC
        S = self.scr
        last = l == self.depth - 1
        if 'aoT' not in S:
            self.S('aoT', [512, NT])
        with ExitStack() as st:
            nm0 = P.sb(st, 'nm0', [128, 1])
            P.dma(nm0, S['m0'])
            mprev = P.sb(st, 'mprev', [128, 4, 128]); mnext = P.sb(st, 'mnext', [128, 4, 128])
            P.memset(mprev, 1.0); P.memset(mnext, 1.0)
            P.aselect(mprev, mprev, [[0, 4], [-1, 128]], ALU.is_ge, 0.0, 0, 1)
            P.aselect(mnext, mnext, [[0, 4], [1, 128]], ALU.is_ge, 0.0, 0, -1)
            e64 = P.sb(st, 'e64', [65, 64])
            P.memset(e64, 1.0)
            P.aselect(e64, e64, [[0, 64]], ALU.is_equal, 0.0, -64, 1)
            sk = P.sb(st, 'sk1', [1, 8])
            P.dma(sk, self.c_sink[j:j + 1, :])
            P.act(sk, sk, AF.Exp, bias=nm0[0:1, :])
            crow = P.sb(st, 'crow', [1, 8, 128])
            P.cp(crow, sk[:, :, None].bc([1, 8, 128]))
            kT = P.sb(st, 'akTs', [128, NT], BF16)
            P.dma(kT, S['akT'])
            va = P.sb(st, 'vaug', [128, NT // 128, 2, 65], BF16)
            P.memset(va, 1.0)
            for g in range(2):
                for n0 in range(0, NT // 128, 8):
                    n1 = min(NT // 128, n0 + 8)
                    P.dma(va[:, n0:n1, g, 0:64], S['av_tm'][n0 * 128:n1 * 128, g * 64:(g + 1) * 64].re("(n p) d -> p n d", p=128))
            nctx = LC // 128
            blocks = []
            if not last:
                blocks += [(b, list(range(nctx)), {}) for b in range(nctx)]
            nlat = N // 128
            for b in range(nlat):
                ch, mk = [], {}
                if b > 0:
                    ch.append(nctx + b - 1); mk[nctx + b - 1] = mprev
                ch.append(nctx + b)
                if b < nlat - 1:
                    ch.append(nctx + b + 1); mk[nctx + b + 1] = mnext
                blocks.append((nctx + b, ch + list(range(nctx)), mk))
            for (qb, chunks, masks) in blocks:
                qa = qb * 128
                qT = self.rot(st, 'aq', [128, 4, 128], BF16, 2)
                P.dma(qT, S['aqT'][:, :, qa:qa + 128].re("c p t -> p c t"))
                for g in range(2):
                    pr = slice(g * 64, (g + 1) * 64)
                    po = self.psn()
                    for ci, ck in enumerate(chunks):
                        ps = self.psn()
                        P.mm(ps, kT[pr, ck * 128:(ck + 1) * 128], qT[pr, :, :])
                        pe = self.rot(st, 'ape', [128, 512], BF16, 3)
                        P.act(pe, ps, AF.Exp, bias=nm0)
                        if ck in masks:
                            P.tt(pe.re("p (h q) -> p h q", h=4), pe.re("p (h q) -> p h q", h=4), masks[ck], ALU.mult)
                        P.mm(po[0:65, :], va[:, ck, g, :], pe, start=(ci == 0), stop=(ci == len(chunks) - 1))
                    oa = self.rot(st, 'aoa', [65, 512], F32, 2)
                    P.cp(oa, po[0:65, :], e='act')
                    pd = self.psn()
                    P.mm(pd[0:64, :], e64, oa, start=True, stop=False)
                    P.mm(pd[0:64, :], self.ones[0:1, 0:64], crow[:, g * 4:(g + 1) * 4, :], start=False, stop=True)
                    rd = self.rot(st, 'ard', [64, 512], F32, 2)
                    P.recip(rd, pd[0:64, :])
                    P.tt(rd, rd, oa[0:64, :], ALU.mult)
                    P.dma(S['aoT'][g * 256:(g + 1) * 256, qa:qa + 128].re("(h d) t -> d h t", h=4).k(qb),
                          rd.re("d (h t) -> d h t", h=4), q='pool')
        P.barrier()

    def stageB_rglru(self, l):
        P, j = self.P, l // 2
        NT = self.NT
        S = self.scr
        if 'rhf' not in S:
            self.S('rhf', [4, 128, NT]); self.S('rgT', [4, 128, NT])
        with ExitStack() as st:
            cw = P.sb(st, 'rcw', [128, 4, 4]); cb = P.sb(st, 'rcb', [128, 4])
            P.dma(cw, self.d_conv[j]); P.dma(cb, self.d_convb[j])
            br = P.sb(st, 'rbr', [128, 2, 4]); bi = P.sb(st, 'rbi', [128, 2, 4]); lam = P.sb(st, 'rlam', [128, 2, 4])
            P.dma(br, self.d_br[j]); P.dma(bi, self.d_bi[j]); P.dma(lam, self.d_lam[j])
            coef = P.sb(st, 'rcoef', [128, 2, 4]); coef2 = P.sb(st, 'rcoef2', [128, 2, 4])
            P.act(coef, lam, AF.Exp, scale=-1.0)
            P.act(coef, coef, AF.Ln, bias=self.onec)
            P.ts(coef2, coef, -16.0, None, ALU.mult)
            P.ts(coef, coef, -8.0, None, ALU.mult)
            Wr = P.sb(st, 'rWr', [128, 2, 4, 128]); Wi = P.sb(st, 'rWi', [128, 2, 4, 128])
            P.memset(Wr, 0.0); P.memset(Wi, 0.0)
            for z in range(2):
                for c in range(4):
                    for hh in range(2):
                        sl = slice(hh * 64, (hh + 1) * 64)
                        P.dma(Wr[sl, z, c, sl], self.d_wr[j, z, 2 * c + hh])
                        P.dma(Wi[sl, z, c, sl], self.d_wi[j, z, 2 * c + hh])
            hst = P.sb(st, 'rhst', [128, 2, 4])
            P.memset(hst, 0.0)
            for z in range(2):
                order = []
                for seg in (0, 1):
                    gs = [g for g in self.groups if g[0] == seg]
                    order += gs if z == 0 else gs[::-1]
                rv = (lambda v: v) if z == 0 else (lambda v: v[:, ::-1])
                for (seg, t0, ln) in order:
                    a, b = self.segs[seg]
                    cin = self.rot(st, 'rcin', [128, 4, 515], F32, 2)
                    lo, hi = max(a, t0 - 1), min(b, t0 + ln + 2)
                    if lo > t0 - 1 or hi < t0 + ln + 2:
                        P.memset(cin, 0.0)
                    P.dma(cin[:, :, lo - (t0 - 1):hi - (t0 - 1)], S['xdT'][:, :, lo:hi].re("c p t -> p c t"))
                    if z == 1:
                        hf = self.rot(st, 'rhfl', [128, 4, 512], F32, 2)
                        gg = self.rot(st, 'rggl', [128, 4, 512], F32, 2)
                        P.dma(hf[:, :, :ln], S['rhf'][:, :, t0:t0 + ln].re("c p t -> p c t"))
                        P.dma(gg[:, :, :ln], S['ggT'][:, :, t0:t0 + ln].re("c p t -> p c t"))
                    for c in range(4):
                        xc = self.rot(st, 'rxc', [128, 512], F32, 3)
                        P.ts(xc[:, :ln], cin[:, c, 0:ln], cw[:, c, 0:1], cb[:, c:c + 1], ALU.mult, ALU.add)
                        for tap in range(1, 4):
                            P.stt(xc[:, :ln], cin[:, c, tap:tap + ln], cw[:, c, tap:tap + 1], xc[:, :ln], ALU.mult, ALU.add)
                        p_r = self.psn(); p_i = self.psn()
                        P.mm(p_r[:, :ln], Wr[:, z, c, :], xc[:, :ln])
                        P.mm(p_i[:, :ln], Wi[:, z, c, :], xc[:, :ln])
                        r = self.rot(st, 'rr', [128, 512], F32, 2); gi_ = self.rot(st, 'rgi', [128, 512], F32, 2)
                        P.act(r[:, :ln], p_r[:, :ln], AF.Sigmoid, bias=br[:, z, c:c + 1])
                        P.act(gi_[:, :ln], p_i[:, :ln], AF.Sigmoid, bias=bi[:, z, c:c + 1])
                        aa = self.rot(st, 'raa', [128, 512], F32, 2); a2 = self.rot(st, 'ra2', [128, 512], F32, 2)
                        P.act(aa[:, :ln], r[:, :ln], AF.Exp, scale=coef[:, z, c:c + 1])
                        P.act(a2[:, :ln], r[:, :ln], AF.Exp, scale=coef2[:, z, c:c + 1])
                        P.ts(a2[:, :ln], a2[:, :ln], -1.0, 1.0, ALU.mult, ALU.add)
                        P.act(a2[:, :ln], a2[:, :ln], AF.Sqrt)
                        P.tt(a2[:, :ln], a2[:, :ln], gi_[:, :ln], ALU.mult)
                        P.tt(a2[:, :ln], a2[:, :ln], xc[:, :ln], ALU.mult, e='pool')
                        hh_ = self.rot(st, 'rhh', [128, 512], F32, 3)
                        P.scan(rv(hh_[:, :ln]), rv(aa[:, :ln]), rv(a2[:, :ln]), hst[:, z, c:c + 1])
                        e_ = ln - 1 if z == 0 else 0
                        P.cp(hst[:, z, c:c + 1], hh_[:, e_:e_ + 1])
                        if z == 0:
                            P.dma(S['rhf'][c, :, t0:t0 + ln].k('%d_%d' % (t0, c)), hh_[:, :ln], q='pool')
                        else:
                            P.tt(hh_[:, :ln], hh_[:, :ln], hf[:, c, :ln], ALU.add)
                            P.tt(hh_[:, :ln], hh_[:, :ln], gg[:, c, :ln], ALU.mult, e='pool')
                            P.dma(S['rgT'][c, :, t0:t0 + ln].k('%d_%d' % (t0, c)), hh_[:, :ln], q='pool')
        P.barrier()

    def odd_oT(self, st, oT, t0, ln):
        P, S = self.P, self.scr
        a = self.rot(st, 'ooa', [128, 4, 512], F32, 2)
        r = self.rot(st, 'oor', [128, 4, 512], F32, 2)
        P.dma(a[:, :, :ln], S['aoT'][:, t0:t0 + ln].re("(c p) t -> p c t", p=128))
        P.dma(r[:, :, :ln], S['rgT'][:, :, t0:t0 + ln].re("c p t -> p c t"))
        P.cp(oT[:, 0:4, :ln], a[:, :, :ln], e='act')
        P.cp(oT[:, 4:8, :ln], r[:, :, :ln])


def host_maps(inp, nb, depth):
    f = lambda a: np.ascontiguousarray(np.asarray(a, dtype=np.float32))
    n_ev, n_od = (depth + 1) // 2, depth // 2
    sh = {}
    for k in ('w_mod', 'b_mod', 'norm1_w', 'norm2_w', 'moe_router', 'moe_w1', 'moe_w3', 'moe_w2'):
        sh[k] = f(inp[k][:depth])
    sh['final_norm_w'] = f(inp['final_norm_w']).reshape(1, D)
    sh['ev_w_in'] = f(inp['ev_w_in'][:n_ev])
    sh['ev_w_out'] = f(inp['ev_w_out'][:n_ev])
    sh['a_conv'] = f(np.asarray(inp['a_conv_w'])[:n_ev].reshape(n_ev, 4, 12, 128).transpose(0, 3, 2, 1))
    sh['a_log'] = f(np.asarray(inp['a_log'])[:n_ev].reshape(n_ev, 8))
    sh['a_dtb'] = f(np.asarray(inp['a_dt_bias'])[:n_ev].reshape(n_ev, 8))
    sh['a_norm_w'] = f(inp['a_norm_w'][:n_ev])
    sh['b_lbl'] = f(np.asarray(inp['b_lb_logits']).reshape(2, 8, 128).transpose(2, 0, 1))
    sh['b_norm_w'] = f(inp['b_norm_w'][:n_ev])
    if n_od:
        sh['od_w_in'] = f(inp['od_w_in'][:n_od])
        sh['od_w_out'] = f(inp['od_w_out'][:n_od])
        sh['c_sink'] = f(inp['c_sink'][:n_od])
        sh['d_conv'] = f(np.asarray(inp['d_conv_w'])[:n_od].reshape(n_od, 4, 4, 128).transpose(0, 3, 2, 1))
        sh['d_convb'] = f(np.asarray(inp['d_conv_b'])[:n_od].reshape(n_od, 4, 128).transpose(0, 2, 1))
        sh['d_wr'] = f(inp['d_w_r'][:n_od])
        sh['d_wi'] = f(inp['d_w_i'][:n_od])
        for kk, src in (('d_br', 'd_b_r'), ('d_bi', 'd_b_i'), ('d_lam', 'd_lambda')):
            sh[kk] = f(np.asarray(inp[src])[:n_od].reshape(n_od, 2, 4, 128).transpose(0, 3, 1, 2))
    maps = []
    cc = np.asarray(inp['c_ctx'], np.float32).reshape(8, 128).T
    for b in range(nb):
        m = dict(sh)
        m['xin'] = f(np.concatenate([np.asarray(inp['ctx'][b]), np.asarray(inp['x'][b])], axis=0))
        cb = np.asarray(inp['c'][b], np.float32).reshape(8, 128).T
        m['cvec'] = f(np.concatenate([cb, cc], axis=1))
        maps.append(m)
    return maps


_CACHE = {}


def build_net(N, LC, depth):
    key = (N, LC, depth)
    if key not in _CACHE:
        net = Net(N, LC, depth)
        for l in range(depth):
            net.layer(l)
        net.final_norm()
        net.P.barrier()
        _CACHE[key] = net
    return _CACHE[key]


def kernel(**inputs):
    x = np.asarray(inputs['x'])
    B, N, _ = x.shape
    LC = np.asarray(inputs['ctx']).shape[1]
    depth = np.asarray(inputs['w_mod']).shape[0]
    net = build_net(N, LC, depth)
    maps = host_maps(inputs, B, depth)
    res = run_bass_kernel_spmd(net.nc, maps, core_ids=list(range(B)))
    return np.stack([np.asarray(r['out'], dtype=np.float32) for r in res.results], axis=0)
```
